# Optimizing a Trainium2 kernel written in Bass

```python
import jax, jax.numpy as jnp
from jax import lax
import numpy as np

D_MODEL = 1024
BATCH = 16
SEQ = 2048
DEPTH = 4

POOL_WIDTH = D_MODEL
N_POOL_GROUPS = 4
POOL_WINDOWS = (2, 4, 8, 16)
POOL_GROUP = POOL_WIDTH // N_POOL_GROUPS
RWKV_WIDTH = D_MODEL
HEAD_SIZE = 64
N_HEADS = RWKV_WIDTH // HEAD_SIZE
D_DECAY_LORA = 64
D_AAA_LORA = 64
D_MV_LORA = 32
N_BRANCHES = 2
NORM_EPS = 1e-6
LNX_EPS = 1e-5 * HEAD_SIZE
SHIFT_WIDTH = 3 * RWKV_WIDTH + D_DECAY_LORA + D_AAA_LORA
IN_SPLITS = (POOL_WIDTH, POOL_WIDTH, SHIFT_WIDTH, RWKV_WIDTH, N_BRANCHES * D_MODEL)
IN_WIDTH = 2 * POOL_WIDTH + SHIFT_WIDTH + RWKV_WIDTH + N_BRANCHES * D_MODEL

kernel_name = 'pool_rwkv7_gated_hybrid'


def rms_norm(x, g):
    xf = x.astype(jnp.float32)
    y = xf * lax.rsqrt(jnp.mean(xf * xf, axis=-1, keepdims=True) + NORM_EPS)
    return (y * g.astype(jnp.float32)).astype(x.dtype)


def split_cols(z, widths):
    points = [int(p) for p in np.cumsum(widths)[:-1]]
    return jnp.split(z, points, axis=-1)


def token_shift(z, mu):
    z_prev = jnp.pad(z, ((0, 0), (1, 0), (0, 0)))[:, :-1]
    return z + mu * (z_prev - z)


def pool_mixer(u, lin, scale):
    b, s, _ = u.shape
    groups = u.astype(jnp.float32).reshape(b, s, N_POOL_GROUPS, POOL_GROUP)
    csum = jnp.cumsum(groups, axis=1)
    pos = jnp.arange(1, s + 1, dtype=jnp.float32)
    pooled = []
    for gi, w in enumerate(POOL_WINDOWS):
        c = csum[:, :, gi]
        c_lag = jnp.pad(c, ((0, 0), (w, 0), (0, 0)))[:, :s]
        count = jnp.minimum(pos, float(w))[None, :, None]
        pooled.append((c - c_lag) / count)
    pooled = jnp.stack(pooled, axis=2)
    mixed = jnp.einsum('bsgc,gcd->bsgd', (pooled - groups).astype(u.dtype), lin)
    return mixed.reshape(b, s, POOL_WIDTH) * scale


def rwkv7_scan(r, decay, k, v, a_vec, b_vec):
    b, s, hh, n = r.shape
    xs = tuple(jnp.moveaxis(t, 1, 0) for t in (r, decay, k, v, a_vec, b_vec))
    state0 = jnp.zeros((b, hh, n, n), jnp.float32)

    def step(state, inp):
        r_t, w_t, k_t, v_t, a_t, b_t = inp
        sa = jnp.einsum('bhvk,bhk->bhv', state, a_t)
        state = (state * w_t[:, :, None, :] + sa[..., None] * b_t[:, :, None, :]
                 + v_t[..., None] * k_t[:, :, None, :])
        y_t = jnp.einsum('bhvk,bhk->bhv', state, r_t)
        return state, y_t

    _, ys = lax.scan(step, state0, xs)
    return jnp.moveaxis(ys, 0, 1)


def heads(z):
    b, s, _ = z.shape
    return z.astype(jnp.float32).reshape(b, s, N_HEADS, HEAD_SIZE)


def setup_inputs(seed: int = 0) -> dict:
    key = jax.random.key(seed)
    ks = jax.random.split(key, 24)
    nrm = lambda kk, shape, sc: sc * jax.random.normal(kk, shape, jnp.float32)
    x = nrm(ks[0], (BATCH, SEQ, D_MODEL), 1.0)
    norm_g = 1.0 + nrm(ks[1], (DEPTH, D_MODEL), 0.05)
    w_in = nrm(ks[2], (DEPTH, D_MODEL, IN_WIDTH), D_MODEL ** -0.5)
    pool_lin = nrm(ks[3], (DEPTH, N_POOL_GROUPS, POOL_GROUP, POOL_GROUP), POOL_GROUP ** -0.5)
    pool_scale = 1.0 + nrm(ks[4], (DEPTH, POOL_WIDTH), 0.1)
    w_pool_proj = nrm(ks[5], (DEPTH, POOL_WIDTH, D_MODEL), POOL_WIDTH ** -0.5)
    mu_shift = jax.random.uniform(ks[6], (DEPTH, SHIFT_WIDTH), jnp.float32)
    decay_w0 = jax.random.uniform(ks[7], (DEPTH, RWKV_WIDTH), jnp.float32, -6.0, -1.0)
    decay_w2 = nrm(ks[8], (DEPTH, D_DECAY_LORA, RWKV_WIDTH), 0.5 * D_DECAY_LORA ** -0.5)
    a0 = nrm(ks[9], (DEPTH, RWKV_WIDTH), 0.5)
    a2 = nrm(ks[10], (DEPTH, D_AAA_LORA, RWKV_WIDTH), 0.5 * D_AAA_LORA ** -0.5)
    vres_v0 = nrm(ks[11], (DEPTH - 1, RWKV_WIDTH), 0.5)
    vres_v1 = nrm(ks[12], (DEPTH - 1, RWKV_WIDTH, D_MV_LORA), RWKV_WIDTH ** -0.5)
    vres_v2 = nrm(ks[13], (DEPTH - 1, D_MV_LORA, RWKV_WIDTH), 0.5 * D_MV_LORA ** -0.5)
    k_k = 0.85 + nrm(ks[14], (DEPTH, RWKV_WIDTH), 0.05)
    k_a = 1.0 + nrm(ks[15], (DEPTH, RWKV_WIDTH), 0.05)
    r_k = nrm(ks[16], (DEPTH, N_HEADS, HEAD_SIZE), 0.1)
    lnx_w = 1.0 + nrm(ks[17], (DEPTH, RWKV_WIDTH), 0.05)
    lnx_b = nrm(ks[18], (DEPTH, RWKV_WIDTH), 0.01)
    w_rwkv_proj = nrm(ks[19], (DEPTH, RWKV_WIDTH, D_MODEL), RWKV_WIDTH ** -0.5)
    w_out = nrm(ks[20], (DEPTH, D_MODEL, D_MODEL), D_MODEL ** -0.5)
    final_g = 1.0 + nrm(ks[21], (D_MODEL,), 0.05)
    return {'x': x, 'norm_g': norm_g, 'w_in': w_in, 'pool_lin': pool_lin, 'pool_scale': pool_scale,
            'w_pool_proj': w_pool_proj, 'mu_shift': mu_shift, 'decay_w0': decay_w0, 'decay_w2': decay_w2,
            'a0': a0, 'a2': a2, 'vres_v0': vres_v0, 'vres_v1': vres_v1, 'vres_v2': vres_v2,
            'k_k': k_k, 'k_a': k_a, 'r_k': r_k, 'lnx_w': lnx_w, 'lnx_b': lnx_b,
            'w_rwkv_proj': w_rwkv_proj, 'w_out': w_out, 'final_g': final_g}


def reference(x, norm_g, w_in, pool_lin, pool_scale, w_pool_proj, mu_shift, decay_w0, decay_w2,
              a0, a2, vres_v0, vres_v1, vres_v2, k_k, k_a, r_k, lnx_w, lnx_b,
              w_rwkv_proj, w_out, final_g):
    b, s, _ = x.shape
    v_first = None
    for i in range(DEPTH):
        h = rms_norm(x, norm_g[i])
        proj = jnp.einsum('bsd,de->bse', h, w_in[i])
        pool_in, pool_gate, rkvwa, rwkv_gate, merge_logits = split_cols(proj, IN_SPLITS)

        y_pool = pool_mixer(pool_in, pool_lin[i], pool_scale[i]) * jax.nn.silu(pool_gate)
        y_pool = jnp.einsum('bsp,pd->bsd', y_pool, w_pool_proj[i])

        rkvwa = token_shift(rkvwa, mu_shift[i])
        r, k, v, w_dn, a_dn = split_cols(
            rkvwa, (RWKV_WIDTH, RWKV_WIDTH, RWKV_WIDTH, D_DECAY_LORA, D_AAA_LORA))
        w_dn = w_dn.astype(jnp.float32)
        decay_logit = -jax.nn.softplus(-(decay_w0[i] + jnp.tanh(w_dn) @ decay_w2[i])) - 0.5
        decay = jnp.exp(-jnp.exp(decay_logit.astype(jnp.float32)))
        a = jax.nn.sigmoid((a0[i] + a_dn @ a2[i]).astype(jnp.float32))
        if i == 0:
            v_first = v
        else:
            mix = jax.nn.sigmoid(vres_v0[i - 1] + (v @ vres_v1[i - 1]) @ vres_v2[i - 1])
            v = v + (v_first - v) * mix
        kk = heads(k * k_k[i])
        kk = kk / jnp.maximum(jnp.linalg.norm(kk, axis=-1, keepdims=True), 1e-12)
        k = k.astype(jnp.float32) * (1.0 + (a - 1.0) * k_a[i])
        rh, kh, vh, ah = heads(r), heads(k), heads(v), heads(a)
        y = rwkv7_scan(rh, heads(decay), kh, vh, -kk, kk * ah)
        mu = jnp.mean(y, axis=-1, keepdims=True)
        var = jnp.mean(jnp.square(y - mu), axis=-1, keepdims=True)
        y = (y - mu) * lax.rsqrt(var + LNX_EPS)
        y = y.reshape(b, s, RWKV_WIDTH) * lnx_w[i] + lnx_b[i]
        bonus = jnp.sum(rh * kh * r_k[i], axis=-1, keepdims=True) * vh
        y = (y + bonus.reshape(b, s, RWKV_WIDTH)).astype(x.dtype)
        y_rwkv = jnp.einsum('bsc,cd->bsd', y * jax.nn.silu(rwkv_gate), w_rwkv_proj[i])

        g_pool, g_rwkv = split_cols(merge_logits, (D_MODEL, D_MODEL))
        merged = jax.nn.sigmoid(g_pool) * y_pool + jax.nn.sigmoid(g_rwkv) * y_rwkv
        x = x + jnp.einsum('bsd,de->bse', merged, w_out[i])
    return rms_norm(x, final_g)
```

```python
import numpy as np
from contextlib import ExitStack
import concourse.bass as bass
import concourse.mybir as mybir
from concourse.bass_utils import run_bass_kernel_spmd

F32 = mybir.dt.float32
BF16 = mybir.dt.bfloat16
AF = mybir.ActivationFunctionType
ALU = mybir.AluOpType
AX = mybir.AxisListType

D = 1024
SEQ = 2048
NSEQ = 2
DEPTH = 4
T = 512
NTC = 4
IN_W = 8320
NWL = 106
NORM_EPS = 1e-6
LNX_EPS = 1e-5 * 64
DECAY_C = 0.6065306597126334
POOL_WINDOWS = (2, 4, 8, 16)
NPAR = 8


class Buf:
    __slots__ = ("name", "lw", "rd", "const")

    def __init__(self, name, const=False):
        self.name = name
        self.lw = None
        self.rd = []
        self.const = const


class Sched:
    ENGS = ("pe", "act", "dve", "pool", "sp")

    def __init__(self, nc, stack, n_dsem=32, strict=("act", "dve", "pool")):
        self.nc = nc
        self.sem = {e: stack.enter_context(nc.semaphore("s_" + e)) for e in self.ENGS}
        self.dsem = [stack.enter_context(nc.semaphore("d%d" % i)) for i in range(n_dsem)]
        self.dval = [0] * n_dsem
        self.dnext = 0
        self.cnt = {e: 0 for e in self.ENGS}
        self.prog = {e: [] for e in self.ENGS}
        self.seen = {e: {} for e in self.ENGS}
        self.strict = set(strict)
        self.nwait = 0
        self.ninst = 0

    @staticmethod
    def _flat(x):
        out = []
        for b in x:
            if isinstance(b, (list, tuple)):
                out.extend(Sched._flat(b))
            elif b is not None:
                out.append(b)
        return out

    def _deps(self, eng, reads, writes):
        deps = set()
        for b in reads:
            if b.lw is not None:
                deps.add(b.lw)
        for b in writes:
            if b.lw is not None:
                deps.add(b.lw)
            for t in b.rd:
                deps.add(t)
        need = {}
        for (kind, ident, val) in deps:
            if kind == "e" and ident == eng and eng not in self.strict:
                continue
            key = (kind, ident)
            if self.seen[eng].get(key, 0) >= val:
                continue
            if need.get(key, 0) < val:
                need[key] = val
        for key, val in need.items():
            self.seen[eng][key] = val
            sem = self.sem[key[1]] if key[0] == "e" else self.dsem[key[1]]
            self.prog[eng].append(("w", sem, val))
            self.nwait += 1

    def _commit(self, tok, reads, writes):
        for b in writes:
            b.lw = tok
            b.rd = []
        for b in reads:
            if b.const or b in writes:
                continue
            b.rd.append(tok)
            if len(b.rd) > 96:
                b.rd = b.rd[-96:]

    def op(self, eng, fn, reads=(), writes=()):
        reads, writes = self._flat(reads), self._flat(writes)
        self._deps(eng, reads, writes)
        self.cnt[eng] += 1
        n = self.cnt[eng]
        self.prog[eng].append(("i", fn, self.sem[eng], 1))
        self._commit(("e", eng, n), reads, writes)
        self.ninst += 1

    def dma(self, q, out, in_, reads=(), writes=(), **kw):
        reads, writes = self._flat(reads), self._flat(writes)
        k = self.dnext
        self.dnext = (self.dnext + 1) % len(self.dsem)
        if self.dval[k] > 0 and self.seen[q].get(("d", k), 0) < self.dval[k]:
            self.seen[q][("d", k)] = self.dval[k]
            self.prog[q].append(("w", self.dsem[k], self.dval[k]))
        self._deps(q, reads, writes)
        self.dval[k] += 16
        v = self.dval[k]
        self.prog[q].append(("i", lambda e: e.dma_start(out=out, in_=in_, **kw), self.dsem[k], 16))
        self._commit(("d", k, v), reads, writes)
        self.ninst += 1

    def barrier(self):
        for e in self.ENGS:
            for p in self.ENGS:
                if p == e or self.cnt[p] == 0:
                    continue
                if self.seen[e].get(("e", p), 0) < self.cnt[p]:
                    self.seen[e][("e", p)] = self.cnt[p]
                    self.prog[e].append(("w", self.sem[p], self.cnt[p]))
            for k, v in enumerate(self.dval):
                if v > 0 and self.seen[e].get(("d", k), 0) < v:
                    self.seen[e][("d", k)] = v
                    self.prog[e].append(("w", self.dsem[k], v))

    def finish(self, q="sp"):
        for k, v in enumerate(self.dval):
            if v > 0:
                self.prog[q].append(("w", self.dsem[k], v))

    def run(self, block):
        def body(prog):
            def f(e):
                for it in prog:
                    if it[0] == "w":
                        e.wait_ge(it[1], it[2])
                    else:
                        it[1](e).then_inc(it[2], it[3])
            return f
        block.tensor(body(self.prog["pe"]))
        block.scalar(body(self.prog["act"]))
        block.vector(body(self.prog["dve"]))
        block.gpsimd(body(self.prog["pool"]))
        block.sync(body(self.prog["sp"]))

    def mm(self, out, lhsT, rhs, start, stop, R, W):
        self.op("pe", lambda e: e.matmul(out, lhsT, rhs, start=start, stop=stop), R, W)

    def tr(self, out, in_, ident, R, W):
        self.op("pe", lambda e: e.transpose(out, in_, ident), R, W)

    def act(self, out, in_, func, R, W, bias=None, scale=None, accum_out=None):
        kw = {}
        if bias is not None:
            kw["bias"] = bias
        if scale is not None:
            kw["scale"] = scale
        if accum_out is not None:
            kw["accum_out"] = accum_out
        self.op("act", lambda e: e.activation(out=out, in_=in_, func=func, **kw), R, W)

    def tt(self, eng, out, in0, in1, op, R, W):
        self.op(eng, lambda e: e.tensor_tensor(out=out, in0=in0, in1=in1, op=op), R, W)

    def ts(self, eng, out, in0, s1, op0, R, W, s2=None, op1=None):
        if op1 is None:
            self.op(eng, lambda e: e.tensor_scalar(out=out, in0=in0, scalar1=s1, scalar2=None, op0=op0), R, W)
        else:
            self.op(eng, lambda e: e.tensor_scalar(out=out, in0=in0, scalar1=s1, scalar2=s2, op0=op0, op1=op1), R, W)

    def stt(self, out, in0, scalar, in1, op0, op1, R, W):
        self.op("dve", lambda e: e.scalar_tensor_tensor(out=out, in0=in0, scalar=scalar, in1=in1, op0=op0, op1=op1), R, W)

    def copy(self, eng, out, in_, R, W):
        if eng == "act":
            self.op("act", lambda e: e.activation(out=out, in_=in_, func=AF.Copy), R, W)
        else:
            self.op(eng, lambda e: e.tensor_copy(out=out, in_=in_), R, W)

    def memset(self, eng, ap, val, W):
        self.op(eng, lambda e: e.memset(ap, val), (), W)


def build_program(L0, L1, NT, final_norm):
    NL = L1 - L0
    NTOK = NSEQ * SEQ
    nc = bass.Bass("TRN2", target_bir_lowering=False, dynamic_dma_scratch_size=2048)
    dt_in = lambda name, shape: nc.dram_tensor(name, shape, F32, kind="ExternalInput").ap()
    x_in = dt_in("x", [NTOK, D])
    w_in = dt_in("w_in", [NL, D, IN_W])
    pool_lin = dt_in("pool_lin", [NL, 4, 256, 256])
    w_pool_proj = dt_in("w_pool_proj", [NL, D, D])
    mu_shift = dt_in("mu_shift", [NL, 3200])
    decay_w2 = dt_in("decay_w2", [NL, 64, D])
    a2 = dt_in("a2", [NL, 64, D])
    vres_v1 = dt_in("vres_v1", [NL, D, 32])
    vres_v2 = dt_in("vres_v2", [NL, 32, D])
    w_rwkv_proj = dt_in("w_rwkv_proj", [NL, D, D])
    w_out = dt_in("w_out", [NL, D, D])
    norm_g = dt_in("norm_g", [NL, D])
    lnx_w = dt_in("lnx_w", [NL, D])
    lnx_b = dt_in("lnx_b", [NL, D])
    final_g = dt_in("final_g", [1, D])
    pp_in = dt_in("pp", [NL, 128, NPAR * 8])
    out_d = nc.dram_tensor("out", [NTOK, D], F32, kind="ExternalOutput").ap()
    NVT = NSEQ * NT
    if L0 == 0 and L1 < DEPTH:
        vfd = nc.dram_tensor("vfirst_out", [NVT, 128, 8 * T], F32, kind="ExternalOutput").ap()
    elif L0 == 0:
        vfd = nc.dram_tensor("vfirst_scr", [NVT, 128, 8 * T], F32, kind="Internal").ap()
    else:
        vfd = dt_in("vfirst_in", [NVT, 128, 8 * T])
    xbufs = [nc.dram_tensor("xbuf%d" % i, [NTOK, D], F32, kind="Internal").ap() for i in range(2)] if NL > 1 else []
    WLd = nc.dram_tensor("wl_scr", [NL, NWL, 128, 1024], BF16, kind="Internal").ap()
    WRd = nc.dram_tensor("wr_scr", [NL, 4, 128, 4096], BF16, kind="Internal").ap()
    B_vfd = Buf("vfd")
    B_xb = [Buf("xb0"), Buf("xb1")]
    B_WLd = [[Buf("wld") for _ in range(NWL)] for _ in range(NL)]
    B_WRd = [[Buf("wrd") for _ in range(4)] for _ in range(NL)]

    with ExitStack() as st:
        S = Sched(nc, st)

        def mk(stack, name, shape, dt, nb=0):
            t = stack.enter_context(nc.sbuf_tensor(name, shape, dt))
            if nb:
                return t, [Buf("%s%d" % (name, i)) for i in range(nb)]
            return t, Buf(name)

        with ExitStack() as pst:
            STG = [mk(pst, "stg%d" % i, [128, 8, 512], F32) for i in range(2)]
            CB = [mk(pst, "cb%d" % i, [128, 4, 1024], BF16) for i in range(2)]
            CB2 = [mk(pst, "cbp%d" % i, [128, 4, 1024], BF16) for i in range(2)]
            MU, B_MU = mk(pst, "mu_bc", [128, 3200], F32)
            MU1, B_MU1 = mk(pst, "mu1_bc", [128, 3200], F32)
            cast_engs = ["pool", "dve", "act"]
            ce = [0]

            def cast(out, in_, R, W):
                e = cast_engs[ce[0] % 3]
                ce[0] += 1
                S.copy(e, out, in_, R, W)

            blk = [0]
            for li in range(NL):
                S.dma("sp", MU[:], mu_shift[li:li + 1, :].to_broadcast([128, 3200]), (), [B_MU])
                S.ts("pool", MU1[:], MU[:], -1.0, ALU.mult, [B_MU], [B_MU1], s2=1.0, op1=ALU.add)
                segs = []
                for c0 in list(range(0, 5248, 512)):
                    segs.append((w_in[li], c0, min(512, 5248 - c0), "L", c0 // 128))
                for c0 in range(6272, 8320, 512):
                    segs.append((w_in[li], c0, 512, "L", c0 // 128))
                segs.append((w_in[li], 5248, 512, "R", 0))
                segs.append((w_in[li], 5760, 512, "R", 1))
                for c0 in (0, 512):
                    segs.append((w_pool_proj[li], c0, 512, "L", 90 + c0 // 128))
                for c0 in (0, 512):
                    segs.append((w_rwkv_proj[li], c0, 512, "L", 98 + c0 // 128))
                segs.append((w_out[li], 0, 512, "R", 2))
                segs.append((w_out[li], 512, 512, "R", 3))
                for (src, c0, ncol, kind, base) in segs:
                    b = blk[0] % 2
                    blk[0] += 1
                    stg, B_stg = STG[b]
                    cb, B_cb = CB[b]
                    cb2, B_cb2 = CB2[b]
                    S.dma("sp", stg[:, :, 0:ncol], src[:, c0:c0 + ncol].rearrange("(kc kp) c -> kp kc c", kp=128),
                          (), [B_stg])
                    if kind == "R":
                        cbr = cb[:].rearrange("p a f -> p (a f)").rearrange("p (kc j) -> p kc j", j=512)
                        for kc in range(8):
                            cast(cbr[:, kc, :], stg[:, kc, :], [B_stg], [B_cb])
                        S.dma("act", WRd[li, base], cb[:].rearrange("p a f -> p (a f)"), [B_cb], [B_WRd[li][base]])
                        continue
                    nm = ncol // 128
                    for mi in range(nm):
                        m = base + mi
                        o = cb[:, mi, :].rearrange("p (kc j) -> p kc j", j=128)
                        i_ = stg[:, :, mi * 128:(mi + 1) * 128]
                        is_shift = (base < 90) and (16 <= m <= 40)
                        if is_shift:
                            mc = (m - 16) * 128
                            S.tt("pool" if mi % 2 == 0 else "dve", o, i_,
                                 MU1[:, mc:mc + 128].unsqueeze(1).to_broadcast([128, 8, 128]), ALU.mult,
                                 [B_stg, B_MU1], [B_cb])
                            o2 = cb2[:, mi, :].rearrange("p (kc j) -> p kc j", j=128)
                            S.tt("dve" if mi % 2 == 0 else "pool", o2, i_,
                                 MU[:, mc:mc + 128].unsqueeze(1).to_broadcast([128, 8, 128]), ALU.mult,
                                 [B_stg, B_MU], [B_cb2])
                            S.dma("act", WLd[li, 65 + m - 16], cb2[:, mi, :], [B_cb2], [B_WLd[li][65 + m - 16]])
                        else:
                            cast(o, i_, [B_stg], [B_cb])
                        S.dma("act", WLd[li, m], cb[:, mi, :], [B_cb], [B_WLd[li][m]])
        S.barrier()
        try:
            _chk("prepass")
            _main(locals())
        except _Stop:
            pass
        S.finish("sp")
        print("program: %d instructions, %d waits" % (S.ninst, S.nwait), {e: S.cnt[e] for e in S.ENGS}, flush=True)
        with nc.Block() as block:
            S.run(block)
    return nc


def _main(env):
    globals_ = env
    nc = env["nc"]; st = env["st"]; S = env["S"]; mk = env["mk"]
    NL = env["NL"]; L0 = env["L0"]; L1 = env["L1"]; NT = env["NT"]; final_norm = env["final_norm"]
    x_in = env["x_in"]; out_d = env["out_d"]; vfd = env["vfd"]; xbufs = env["xbufs"]
    WLd = env["WLd"]; WRd = env["WRd"]; B_vfd = env["B_vfd"]; B_xb = env["B_xb"]; B_WLd = env["B_WLd"]; B_WRd = env["B_WRd"]
    pool_lin = env["pool_lin"]; decay_w2 = env["decay_w2"]; a2 = env["a2"]; vres_v1 = env["vres_v1"]; vres_v2 = env["vres_v2"]
    norm_g = env["norm_g"]; lnx_w = env["lnx_w"]; lnx_b = env["lnx_b"]; final_g = env["final_g"]; pp_in = env["pp_in"]
    if True:
        IDENT, B_IDENT = mk(st, "ident", [128, 128], BF16)
        MASKT, B_MASKT = mk(st, "maskt", [128, 512], BF16)
        MASKL, B_MASKL = mk(st, "maskl", [128, 128], BF16)
        BONES, B_BONES = mk(st, "bones", [128, 128], BF16)
        E2, B_E2 = mk(st, "e2", [128, 2], BF16)
        ONESF, B_ONESF = mk(st, "onesf", [128, 128], F32)
        CORR, B_CORR = mk(st, "corr", [128, 4, 16], F32)
        EPSC, B_EPSC = mk(st, "epsc", [128, 2], F32)
        for b_ in (B_IDENT, B_MASKT, B_MASKL, B_BONES, B_E2, B_ONESF, B_CORR, B_EPSC):
            b_.const = True
        G_BC, B_G = mk(st, "g_bc", [128, D], F32)
        LNW, B_LNW = mk(st, "lnw_bc", [128, D], F32)
        LNB, B_LNB = mk(st, "lnb_bc", [128, D], F32)
        FG, B_FG = mk(st, "fg_bc", [128, D], F32)
        PP, B_PP = mk(st, "pp_sb", [128, NPAR, 8], F32)
        PL, B_PL = mk(st, "pl", [128, 4, 2, 256], BF16)
        DAZ, B_DA = mk(st, "daz", [128, 2, D], BF16)
        V1W, B_V1W = mk(st, "v1w", [128, 8, 128], BF16)
        V2W, B_V2W = mk(st, "v2w", [128, D], BF16)
        HT, B_HT = mk(st, "ht", [128, 8, T + 1], BF16)
        MP, B_MP = mk(st, "mp", [128, 8, T], BF16)
        YGT, B_YGT = mk(st, "ygt", [128, 8, T], BF16)
        YPI, B_YPI = YGT, B_YGT
        XIN = [mk(st, "xin%d" % i, [128, D], F32) for i in range(2)]
        HN, B_HN = mk(st, "hn", [128, D], BF16)
        JUNK, B_JUNK = HN, B_HN
        STAT, B_STAT = mk(st, "stat", [128, 8], F32)
        NRL = 6
        WLB = [mk(st, "wlb%d" % i, [128, 1024], BF16) for i in range(NRL)]
        WRB = [mk(st, "wrb%d" % i, [128, 4096], BF16) for i in range(2)]
        NTMP = 10
        TMPALL, B_TMP = mk(st, "tmpall", [128, NTMP, T + 16], F32, nb=NTMP)
        TMP = [(TMPALL[:, i, 0:T], B_TMP[i]) for i in range(NTMP)]
        UG, B_UG = TMPALL[:, 0:2, :], [B_TMP[0], B_TMP[1]]
        SA, B_SA = TMPALL[:, 2:4, :], [B_TMP[2], B_TMP[3]]
        SBb, B_SBb = TMPALL[:, 4:6, :], [B_TMP[4], B_TMP[5]]
        SG, B_SG = TMPALL[:, 6:8, 0:T], [B_TMP[6], B_TMP[7]]
        SGM, B_SGM = TMP[8]
        CTMP, B_CTMP = TMP[9]
        DG, B_DG = mk(st, "dg", [128, 2, T], BF16)
        HALO_P, B_HALOP = mk(st, "halop", [128, 8, 16], F32)
        VF, B_VF = mk(st, "vf", [128, 8, T], F32, nb=8)
        STGS, B_STGS = VF[:].rearrange("p c t -> p (c t)")[:, 0:2048], B_VF[0:4]
        VB, B_VB = mk(st, "vb", [128, 8, T], BF16, nb=8)
        VT, B_VT = mk(st, "vt", [128, NTC, D], BF16, nb=NTC)
        TWA, B_TWA = mk(st, "twa", [128, T], BF16)
        P1, B_P1 = mk(st, "p1", [128, T], BF16)
        BTZ, B_BT = mk(st, "btz", [128, 2, 4, T], BF16, nb=4)
        KTZ, B_KT = mk(st, "ktz", [128, 2, 4, T], BF16, nb=4)
        AR, B_AR = mk(st, "ar", [128, 4, NTC, 256], BF16, nb=4)
        BTT, B_BTT = mk(st, "btt", [128, NTC, 512], BF16, nb=NTC)
        KTT, B_KTT = mk(st, "ktt", [128, NTC, 512], BF16, nb=NTC)
        RKB, B_RKB = mk(st, "rkb", [128, T], BF16)
        EWC, B_EWC = mk(st, "ewc", [128, 4, NTC], F32)
        BDOT, B_BDOT = mk(st, "bdot", [128, NTC, 16], F32)
        SC, B_SC = mk(st, "sc", [128, 8, 512], BF16, nb=8)
        PK = [mk(st, "pk%d" % i, [128, 2, 512], BF16, nb=2) for i in range(2)]
        QK = [mk(st, "qk%d" % i, [128, 2, 512], BF16, nb=2) for i in range(2)]
        SK, B_SK = mk(st, "sk", [128, 2, 512], BF16, nb=2)
        XB, B_XB = mk(st, "xbv", [128, 512], BF16)
        UB, B_UB = mk(st, "ubv", [128, 512], BF16)
        HS, B_HS = mk(st, "hs", [128, 8, 64], F32, nb=2)
        HBZ, B_HB = mk(st, "hbz", [128, 8, 128], BF16, nb=2)
        YA, B_YA = TMP[0]
        YB, B_YB = TMP[1]
        SGT, B_SGT = TMP[2]
        YG, B_YG = mk(st, "yg", [128, 512], BF16)
        ST8, B_ST8 = mk(st, "st8", [128, 6, 8], F32)
        MG, B_MG = VB, B_VB
        PSB = []
        for i in range(6):
            t_ = st.enter_context(nc.psum_tensor("ps%d" % i, [128, 512], F32))
            PSB.append((t_, Buf("ps%d" % i)))
        PTB = []
        for i in range(2):
            t_ = st.enter_context(nc.psum_tensor("pt%d" % i, [128, 1024], BF16))
            PTB.append((t_, Buf("pt%d" % i)))
        ring = [0, 0, 0, 0]

        def ps():
            r = PSB[ring[0] % 5]
            ring[0] += 1
            return r

        PBD, B_PBD = PSB[5]

        def pt():
            r = PTB[ring[1] % 2]
            ring[1] += 1
            return r

        def tmp():
            r = TMP[ring[3] % NTMP]
            ring[3] += 1
            return r

        def tri(dst_ap, cmp_op, pattern_step, cm):
            S.memset("pool", CTMP[:, 0:128], 1.0, [B_CTMP])
            S.op("pool", lambda e: e.affine_select(out=CTMP[:, 0:128], in_=CTMP[:, 0:128], pattern=[[pattern_step, 128]],
                                                   compare_op=cmp_op, fill=0.0, base=0, channel_multiplier=cm),
                 [B_CTMP], [B_CTMP])
            S.copy("pool", dst_ap, CTMP[:, 0:128], [B_CTMP], [B_IDENT])

        tri(IDENT[:], ALU.is_equal, -1, 1)
        tri(MASKL[:], ALU.is_gt, -1, 1)
        tri(MASKT[:, 0:128], ALU.is_gt, 1, -1)
        tri(MASKT[:, 128:256], ALU.is_ge, 1, -1)
        tri(MASKT[:, 256:384], ALU.is_gt, 1, -1)
        tri(MASKT[:, 384:512], ALU.is_ge, 1, -1)
        S.memset("pool", BONES[:], 0.0, [B_IDENT])
        S.memset("pool", BONES[0:64, 0:64], 1.0, [B_IDENT])
        S.memset("pool", BONES[64:128, 64:128], 1.0, [B_IDENT])
        S.memset("pool", E2[:], 0.0, [B_IDENT])
        S.memset("pool", E2[0:64, 0:1], 1.0, [B_IDENT])
        S.memset("pool", E2[64:128, 1:2], 1.0, [B_IDENT])
        S.memset("pool", ONESF[:], 1.0, [B_IDENT])
        S.memset("pool", CORR[:], 1.0, [B_IDENT])
        for g, w in enumerate(POOL_WINDOWS):
            for t_ in range(w - 1):
                S.memset("pool", CORR[:, g, t_:t_ + 1], float(w) / float(t_ + 1), [B_IDENT])
        S.memset("pool", BTZ[:], 0.0, B_BT)
        S.memset("pool", KTZ[:], 0.0, B_KT)
        S.memset("pool", HBZ[:], 0.0, B_HB)
        S.memset("pool", DAZ[:], 0.0, [B_DA])
        S.memset("pool", V1W[:], 0.0, [B_V1W])
        S.memset("pool", V2W[:], 0.0, [B_V2W])
        S.memset("pool", EPSC[:, 0:1], NORM_EPS, [B_IDENT])
        S.memset("pool", EPSC[:, 1:2], LNX_EPS, [B_IDENT])
        S.dma("sp", FG[:], final_g.to_broadcast([128, D]), (), [B_FG])
        _chk("consts")

        def wl(li, idx):
            t_, b_ = WLB[ring[2] % NRL]
            ring[2] += 1
            S.dma("sp", t_[:], WLd[li, idx], [B_WLd[li][idx]], [b_])
            return t_, b_

        wr_i = [0]

        def wr(li, idx):
            t_, b_ = WRB[wr_i[0] % 2]
            wr_i[0] += 1
            S.dma("sp", t_[:], WRd[li, idx], [B_WRd[li][idx]], [b_])
            return t_, b_

        def inproj(li, m, shift):
            pt_, pb_ = ps()
            w_, wb_ = wl(li, m)
            for kc in range(8):
                S.mm(pt_[:], w_[:, kc * 128:(kc + 1) * 128], HT[:, kc, 1:T + 1], kc == 0, (kc == 7 and not shift),
                     [wb_, B_HT], [pb_])
            if shift:
                w2, wb2 = wl(li, 65 + m - 16)
                for kc in range(8):
                    S.mm(pt_[:], w2[:, kc * 128:(kc + 1) * 128], HT[:, kc, 0:T], False, kc == 7,
                         [wb2, B_HT], [pb_])
            return pt_, pb_

        for li in range(NL):
            L = L0 + li
            last = (L == DEPTH - 1) and final_norm
            x_src, B_xs = (x_in, None) if li == 0 else (xbufs[(li - 1) % 2], B_xb[(li - 1) % 2])
            x_dst, B_xd = (out_d, None) if li == NL - 1 else (xbufs[li % 2], B_xb[li % 2])
            Rxs = [B_xs] if B_xs is not None else []
            Wxd = [B_xd] if B_xd is not None else []
            S.dma("sp", G_BC[:], norm_g[li:li + 1, :].to_broadcast([128, D]), (), [B_G])
            S.dma("sp", LNW[:], lnx_w[li:li + 1, :].to_broadcast([128, D]), (), [B_LNW])
            S.dma("sp", LNB[:], lnx_b[li:li + 1, :].to_broadcast([128, D]), (), [B_LNB])
            S.dma("sp", PP[:].rearrange("p k c -> p (k c)"), pp_in[li], (), [B_PP])
            S.ts("pool", PP[:, 7, :], PP[:, 5, :], -1.0, ALU.mult, [B_PP], [B_PP], s2=1.0, op1=ALU.add)
            S.dma("sp", STGS[:].rearrange("p (g kc d) -> p g kc d", g=4, kc=2),
                  pool_lin[li].rearrange("g (kc kp) d -> kp g kc d", kp=128), (), [B_STGS])
            S.copy("pool", PL[:].rearrange("p g kc d -> p (g kc d)"), STGS[:], [B_STGS], [B_PL])
            S.dma("sp", STGS[0:64, 0:D], decay_w2[li], [], [B_STGS])
            S.dma("sp", STGS[64:128, 0:D], a2[li], [], [B_STGS])
            S.copy("pool", DAZ[0:64, 0, :], STGS[0:64, 0:D], [B_STGS], [B_DA])
            S.copy("pool", DAZ[64:128, 1, :], STGS[64:128, 0:D], [B_STGS], [B_DA])
            if L > 0:
                S.dma("sp", STGS[:, 0:256].rearrange("p (kc j) -> p kc j", j=32),
                      vres_v1[li].rearrange("(kc kp) j -> kp kc j", kp=128), [], [B_STGS])
                S.copy("pool", V1W[:, :, 0:32], STGS[:, 0:256].rearrange("p (kc j) -> p kc j", j=32), [B_STGS], [B_V1W])
                S.dma("sp", STGS[0:32, 0:D], vres_v2[li], [], [B_STGS])
                S.copy("pool", V2W[0:32, :], STGS[0:32, 0:D], [B_STGS], [B_V2W])
            ppc = lambda k, c: PP[:, k, c:c + 1]
            _chk("params")

            for s in range(NSEQ):
                for ti in range(NT):
                    row0 = s * SEQ + ti * T
                    vt_idx = s * NT + ti
                    first = (ti == 0)
                    if first:
                        S.memset("pool", HT[:, :, 0:1], 0.0, [B_HT])
                    else:
                        S.copy("pool", HT[:, :, 0:1], HT[:, :, T:T + 1], [B_HT], [B_HT])
                    for tg in range(4):
                        xi, B_xi = XIN[tg % 2]
                        S.dma("sp", xi[:], x_src[row0 + tg * 128: row0 + (tg + 1) * 128, :], Rxs, [B_xi])
                        S.act(JUNK[:], xi[:], AF.Square, [B_xi], [B_JUNK, B_STAT], accum_out=STAT[:, 0:1])
                        S.act(STAT[:, 1:2], STAT[:, 0:1], AF.Sqrt, [B_STAT, B_EPSC], [B_STAT], bias=EPSC[:, 0:1], scale=1.0 / D)
                        S.op("dve", lambda e: e.reciprocal(out=STAT[:, 2:3], in_=STAT[:, 1:2]), [B_STAT], [B_STAT])
                        S.stt(HN[:], xi[:], STAT[:, 2:3], G_BC[:], ALU.mult, ALU.mult, [B_xi, B_STAT, B_G], [B_HN])
                        ptt, ptb = pt()
                        for kc in range(8):
                            S.tr(ptt[:, kc * 128:(kc + 1) * 128], HN[:, kc * 128:(kc + 1) * 128], IDENT[:],
                                 [B_HN, B_IDENT], [ptb])
                        S.copy("act", HT[:, :, 1 + tg * 128: 1 + (tg + 1) * 128],
                               ptt[:].rearrange("p (kc j) -> p kc j", j=128), [ptb], [B_HT])

                    _chk("N")
                    if first:
                        S.memset("pool", HALO_P[:], 0.0, [B_HALOP])
                    for g, w in enumerate(POOL_WINDOWS):
                        for j in range(2):
                            p_, pb_ = inproj(li, 2 * g + j, False)
                            S.copy("act", UG[:, j, 16:16 + T], p_[:], [pb_], [B_UG])
                        for j in range(2):
                            p_, pb_ = inproj(li, 8 + 2 * g + j, False)
                            S.act(SG[:, j, :], p_[:], AF.Silu, [pb_], [B_SG])
                        S.copy("pool", UG[:, :, 0:16], HALO_P[:, 2 * g:2 * g + 2, :], [B_HALOP], [B_UG])
                        S.copy("pool", HALO_P[:, 2 * g:2 * g + 2, :], UG[:, :, T:T + 16], [B_UG], [B_HALOP])
                        NJ = T + 16
                        S.tt("pool", SA[:, :, 1:NJ], UG[:, :, 1:NJ], UG[:, :, 0:NJ - 1], ALU.add, [B_UG], [B_SA])
                        cur, B_cur = SA, B_SA
                        if w >= 4:
                            S.tt("pool", SBb[:, :, 3:NJ], SA[:, :, 3:NJ], SA[:, :, 1:NJ - 2], ALU.add, [B_SA], [B_SBb])
                            cur, B_cur = SBb, B_SBb
                        if w >= 8:
                            S.tt("pool", SA[:, :, 7:NJ], SBb[:, :, 7:NJ], SBb[:, :, 3:NJ - 4], ALU.add, [B_SBb], [B_SA])
                            cur, B_cur = SA, B_SA
                        if w >= 16:
                            S.tt("pool", SBb[:, :, 15:NJ], SA[:, :, 15:NJ], SA[:, :, 7:NJ - 8], ALU.add, [B_SA], [B_SBb])
                            cur, B_cur = SBb, B_SBb
                        if first:
                            S.tt("pool", cur[:, :, 16:32], cur[:, :, 16:32],
                                 CORR[:, g, :].unsqueeze(1).to_broadcast([128, 2, 16]), ALU.mult,
                                 [B_cur, B_CORR], [B_cur])
                        S.stt(DG[:], cur[:, :, 16:16 + T], 1.0 / w, UG[:, :, 16:16 + T], ALU.mult, ALU.subtract,
                              [B_cur, B_UG], [B_DG])
                        for mo in range(2):
                            p_, pb_ = ps()
                            for kc in range(2):
                                S.mm(p_[:], PL[:, g, kc, mo * 128:(mo + 1) * 128], DG[:, kc, :], kc == 0, kc == 1,
                                     [B_PL, B_DG], [pb_])
                            S.stt(YPI[:, 2 * g + mo, :], p_[:], ppc(0, 2 * g + mo), SG[:, mo, :], ALU.mult, ALU.mult,
                                  [pb_, B_PP, B_SG], [B_YPI])
                    for m in range(8):
                        py_, pyb_ = ps()
                        w_, wb_ = wl(li, 90 + m)
                        for kc in range(8):
                            S.mm(py_[:], w_[:, kc * 128:(kc + 1) * 128], YPI[:, kc, :], kc == 0, kc == 7,
                                 [wb_, B_YPI], [pyb_])
                        pg_, pgb_ = inproj(li, 49 + m, False)
                        S.act(SGM[:], pg_[:], AF.Sigmoid, [pgb_], [B_SGM])
                        S.tt("dve", MP[:, m, :], py_[:], SGM[:], ALU.mult, [pyb_, B_SGM], [B_MP])

                    _chk("P")
                    p_, pb_ = inproj(li, 40, True)
                    S.act(TWA[0:64, :], p_[0:64, :], AF.Tanh, [pb_], [B_TWA])
                    S.copy("act", TWA[64:128, :], p_[64:128, :], [pb_], [B_TWA])
                    for c in range(8):
                        p_, pb_ = inproj(li, 32 + c, True)
                        S.copy("act", VF[:, c, :], p_[:], [pb_], [B_VF[c]])
                    if L == 0:
                        S.dma("act", vfd[vt_idx], VF[:].rearrange("p c t -> p (c t)"), B_VF, [B_vfd])
                    else:
                        for c in range(8):
                            S.copy("pool", VB[:, c, :], VF[:, c, :], [B_VF[c]], [B_VB[c]])
                        p1_, p1b_ = ps()
                        for kc in range(8):
                            S.mm(p1_[:], V1W[:, kc, :], VB[:, kc, :], kc == 0, kc == 7, [B_V1W, B_VB[kc]], [p1b_])
                        S.copy("act", P1[:], p1_[:], [p1b_], [B_P1])
                        for c in range(8):
                            p_, pb_ = ps()
                            S.mm(p_[:], V2W[:, c * 128:(c + 1) * 128], P1[:], True, True, [B_V2W, B_P1], [pb_])
                            mx, B_mx = tmp()
                            S.act(mx[:], p_[:], AF.Sigmoid, [pb_, B_PP], [B_mx], bias=ppc(3, c))
                            vf1, B_vf1 = tmp()
                            S.dma("sp", vf1[:], vfd[vt_idx, :, c * T:(c + 1) * T], [B_vfd], [B_vf1])
                            S.tt("pool", vf1[:], vf1[:], VF[:, c, :], ALU.subtract, [B_vf1, B_VF[c]], [B_vf1])
                            S.tt("pool", vf1[:], vf1[:], mx[:], ALU.mult, [B_vf1, B_mx], [B_vf1])
                            S.tt("dve", VF[:, c, :], VF[:, c, :], vf1[:], ALU.add, [B_VF[c], B_vf1], [B_VF[c]])
                    for c in range(8):
                        S.copy("pool", VB[:, c, :], VF[:, c, :], [B_VF[c]], [B_VB[c]])
                    for tc in range(NTC):
                        ptt, ptb = pt()
                        for c in range(8):
                            S.tr(ptt[:, c * 128:(c + 1) * 128], VB[:, c, tc * 128:(tc + 1) * 128], IDENT[:],
                                 [B_VB[c], B_IDENT], [ptb])
                        S.copy("act", VT[:, tc, :], ptt[:], [ptb], [B_VT[tc]])

                    _chk("R0")
                    for half in range(2):
                        for cl in range(4):
                            c = half * 4 + cl
                            pr_, prb_ = inproj(li, 16 + c, True)
                            pk_, pkb_ = inproj(li, 24 + c, True)
                            pd_, pdb_ = ps()
                            S.mm(pd_[:], DAZ[:, 0, c * 128:(c + 1) * 128], TWA[:], True, True, [B_DA, B_TWA], [pdb_])
                            sgd, B_sgd = tmp()
                            S.act(sgd[:], pd_[:], AF.Sigmoid, [pdb_, B_PP], [B_sgd], bias=ppc(1, c))
                            pa_, pab_ = ps()
                            S.mm(pa_[:], DAZ[:, 1, c * 128:(c + 1) * 128], TWA[:], True, True, [B_DA, B_TWA], [pab_])
                            ag, B_ag = tmp()
                            S.act(ag[:], pa_[:], AF.Sigmoid, [pab_, B_PP], [B_ag], bias=ppc(2, c))
                            cs, B_cs = tmp()
                            for tc in range(NTC):
                                S.op("dve", lambda e, cs=cs, sgd=sgd, tc=tc: e.tensor_tensor_scan(
                                    out=cs[:, tc * 128:(tc + 1) * 128], data0=ONESF[:], data1=sgd[:, tc * 128:(tc + 1) * 128],
                                    initial=0.0, op0=ALU.mult, op1=ALU.add), [B_ONESF, B_sgd], [B_cs])
                            csp, B_csp = tmp()
                            S.tt("pool", csp[:], cs[:], sgd[:], ALU.subtract, [B_cs, B_sgd], [B_csp])
                            ew, B_ew = tmp()
                            S.act(ew[:], cs[:], AF.Exp, [B_cs], [B_ew], scale=-DECAY_C)
                            ewi, B_ewi = tmp()
                            S.act(ewi[:], cs[:], AF.Exp, [B_cs], [B_ewi], scale=DECAY_C)
                            ewp, B_ewp = tmp()
                            S.act(ewp[:], csp[:], AF.Exp, [B_csp], [B_ewp], scale=-DECAY_C)
                            S.copy("pool", EWC[:, cl, :], ew[:].rearrange("p (tc j) -> p tc j", j=128)[:, :, 127],
                                   [B_ew], [B_EWC])
                            kkr, B_kkr = tmp()
                            S.ts("dve", kkr[:], pk_[:], ppc(4, c), ALU.mult, [pkb_, B_PP], [B_kkr])
                            S.act(RKB[:], kkr[:], AF.Square, [B_kkr], [B_RKB])
                            pn_, pnb_ = ps()
                            S.mm(pn_[:], BONES[:], RKB[:], True, True, [B_BONES, B_RKB], [pnb_])
                            nrm, B_nrm = tmp()
                            S.act(nrm[:], pn_[:], AF.Sqrt, [pnb_], [B_nrm])
                            S.ts("pool", nrm[:], nrm[:], 1e-12, ALU.max, [B_nrm], [B_nrm])
                            S.op("dve", lambda e, nrm=nrm: e.reciprocal(out=nrm[:], in_=nrm[:]), [B_nrm], [B_nrm])
                            S.tt("pool", kkr[:], kkr[:], nrm[:], ALU.mult, [B_kkr, B_nrm], [B_kkr])
                            S.ts("pool", nrm[:], ag[:], ppc(5, c), ALU.mult, [B_ag, B_PP], [B_nrm], s2=ppc(7, c), op1=ALU.add)
                            kp, B_kp = csp, B_csp
                            S.tt("dve", kp[:], pk_[:], nrm[:], ALU.mult, [pkb_, B_nrm], [B_kp])
                            S.stt(RKB[:], pr_[:], ppc(6, c), kp[:], ALU.mult, ALU.mult, [prb_, B_PP, B_kp], [B_RKB])
                            for tc in range(NTC):
                                S.mm(PBD[:, tc * 16 + 2 * c: tc * 16 + 2 * c + 2], RKB[:, tc * 128:(tc + 1) * 128], E2[:],
                                     True, True, [B_RKB, B_E2], [B_PBD])
                            arv = AR[:, cl].rearrange("p tc (two j) -> p tc two j", two=2)
                            S.tt("dve", arv[:, :, 1, :], pr_[:].rearrange("p (tc j) -> p tc j", j=128),
                                 ew[:].rearrange("p (tc j) -> p tc j", j=128), ALU.mult, [prb_, B_ew], [B_AR[cl]])
                            S.stt(arv[:, :, 0, :], kkr[:].rearrange("p (tc j) -> p tc j", j=128), -1.0,
                                  ewp[:].rearrange("p (tc j) -> p tc j", j=128), ALU.mult, ALU.mult,
                                  [B_kkr, B_ewp], [B_AR[cl]])
                            S.tt("pool", ag[:], ag[:], kkr[:], ALU.mult, [B_ag, B_kkr], [B_ag])
                            for hh_ in range(2):
                                Pq = slice(hh_ * 64, (hh_ + 1) * 64)
                                S.tt("pool", BTZ[Pq, hh_, cl, :], ag[Pq, :], ewi[Pq, :], ALU.mult, [B_ag, B_ewi], [B_BT[cl]])
                                S.tt("pool", KTZ[Pq, hh_, cl, :], kp[Pq, :], ewi[Pq, :], ALU.mult, [B_kp, B_ewi], [B_KT[cl]])
                        for tc in range(NTC):
                            for (srcz, Bsrc, dst, Bdst) in ((BTZ, B_BT, BTT, B_BTT), (KTZ, B_KT, KTT, B_KTT)):
                                pz_, pzb_ = ps()
                                for cl in range(4):
                                    for hh_ in range(2):
                                        S.mm(pz_[:, cl * 128:(cl + 1) * 128], srcz[:, hh_, cl, tc * 128:(tc + 1) * 128], IDENT[:],
                                             hh_ == 0, hh_ == 1, [Bsrc[cl], B_IDENT], [pzb_])
                                S.copy("act", dst[:, tc, :], pz_[:], [pzb_], [Bdst[tc]])
                        S.copy("act", BDOT[:, :, half * 8:(half + 1) * 8],
                               PBD[:, 0:NTC * 16].rearrange("p (tc h) -> p tc h", h=16)[:, :, half * 8:(half + 1) * 8],
                               [B_PBD], [B_BDOT])
                        _chk("R1")
                        if first and True:
                            S.memset("pool", HS[:, half * 4:(half + 1) * 4, :], 0.0, [B_HS[half]])
                            S.memset("pool", HBZ[:, half * 4:(half + 1) * 4, :], 0.0, [B_HB[half]])
                        _chk("S00")
                        wg_, wgb_ = wr(li, half)
                        _chk("S01")
                        for tc in range(NTC):
                            tcs = slice(tc * 128, (tc + 1) * 128)
                            for q in range(2):
                                pq_, pqb_ = ps()
                                for hq in range(4):
                                    h = q * 4 + hq
                                    cl, hh = h // 2, h % 2
                                    Ph = slice(hh * 64, (hh + 1) * 64)
                                    psc_, pscb_ = ps()
                                    S.mm(psc_[:, 0:256], BTZ[:, hh, cl, tcs], AR[:, cl, tc, :], True, True,
                                         [B_BT[cl], B_AR[cl]], [pscb_])
                                    _chk("cnt")
                                    S.mm(psc_[:, 256:512], KTZ[:, hh, cl, tcs], AR[:, cl, tc, :], True, True,
                                         [B_KT[cl], B_AR[cl]], [pscb_])
                                    _chk("cnt")
                                    _chk("S02")
                                    S.tt("dve", SC[:, h, :], psc_[:], MASKT[:], ALU.mult, [pscb_, B_MASKT], [B_SC[h]])
                                    _chk("S03")
                                    _chk("cnt")
                                    S.mm(pq_[:, hq * 128:(hq + 1) * 128], AR[:, cl, tc, 0:128], BTZ[:, hh, cl, tcs], True, True,
                                         [B_AR[cl], B_BT[cl]], [pqb_])
                                    _chk("cnt")
                                _chk("S04")
                                S.tt("dve", QK[0][0][:, q, :].rearrange("p (h j) -> p h j", j=128),
                                     pq_[:].rearrange("p (h j) -> p h j", j=128),
                                     MASKL[:].unsqueeze(1).to_broadcast([128, 4, 128]), ALU.mult,
                                     [pqb_, B_MASKL], [QK[0][1][q]])
                            _chk("S0")
                            for q in range(2):
                                hs4 = range(q * 4, q * 4 + 4)
                                Bsc4 = [B_SC[h] for h in hs4]
                                P0 = SC[:, q * 4:(q + 1) * 4, 0:128]
                                S.tt("dve", SK[:, q, :].rearrange("p (h j) -> p h j", j=128), P0,
                                     IDENT[:].unsqueeze(1).to_broadcast([128, 4, 128]), ALU.add, Bsc4 + [B_IDENT], [B_SK[q]])
                                for k in range(7):
                                    cur, nxt = k % 2, (k + 1) % 2
                                    Qk, BQk = QK[cur][0], QK[cur][1][q]

                                    def Pk_ap(hq, k=k, cur=cur, q=q):
                                        if k == 0:
                                            return SC[:, q * 4 + hq, 0:128]
                                        return PK[cur][0][:, q, hq * 128:(hq + 1) * 128]
                                    BPk = Bsc4 if k == 0 else [PK[cur][1][q]]
                                    if k >= 1:
                                        pS_, pSb_ = ps()
                                        for hq in range(4):
                                            S.mm(pS_[:, hq * 128:(hq + 1) * 128], Qk[:, q, hq * 128:(hq + 1) * 128],
                                                 SK[:, q, hq * 128:(hq + 1) * 128], True, True, [BQk, B_SK[q]], [pSb_])
                                        S.tt("dve", SK[:, q, :], pS_[:], SK[:, q, :], ALU.add, [pSb_, B_SK[q]], [B_SK[q]])
                                    if k <= 5:
                                        pQ_, pQb_ = ps()
                                        for hq in range(4):
                                            S.mm(pQ_[:, hq * 128:(hq + 1) * 128], Pk_ap(hq), Qk[:, q, hq * 128:(hq + 1) * 128],
                                                 True, True, BPk + [BQk], [pQb_])
                                        S.copy("act", QK[nxt][0][:, q, :], pQ_[:], [pQb_], [QK[nxt][1][q]])
                                    if k <= 4:
                                        pP_, pPb_ = ps()
                                        for hq in range(4):
                                            S.mm(pP_[:, hq * 128:(hq + 1) * 128], Qk[:, q, hq * 128:(hq + 1) * 128], Pk_ap(hq),
                                                 True, True, BPk + [BQk], [pPb_])
                                        S.copy("act", PK[nxt][0][:, q, :], pP_[:], [pPb_], [PK[nxt][1][q]])
                            _chk("S1")
                            pgt_, pgtb_ = ps()
                            for kc in range(8):
                                S.mm(pgt_[:], HT[:, kc, 1 + tc * 128: 1 + (tc + 1) * 128], wg_[:, kc * 512:(kc + 1) * 512],
                                     kc == 0, kc == 7, [B_HT, wgb_], [pgtb_])
                            S.act(SGT[:], pgt_[:], AF.Silu, [pgtb_], [B_SGT])
                            _chk("S2")
                            px_, pxb_ = ps()
                            for cl in range(4):
                                c = half * 4 + cl
                                S.mm(px_[:, cl * 128:(cl + 1) * 128], AR[:, cl, tc, 0:128], HBZ[:, c, :], True, False,
                                     [B_AR[cl], B_HB[half]], [pxb_])
                                for hh in range(2):
                                    h = cl * 2 + hh
                                    S.mm(px_[:, h * 64:(h + 1) * 64], SC[:, h, 256:384], VT[:, tc, (c * 2 + hh) * 64:(c * 2 + hh + 1) * 64],
                                         False, hh == 1, [B_SC[h], B_VT[tc]], [pxb_])
                            S.copy("act", XB[:], px_[:], [pxb_], [B_XB])
                            _chk("S3")
                            pu_, pub_ = ps()
                            for h in range(8):
                                q, hq = h // 4, h % 4
                                S.mm(pu_[:, h * 64:(h + 1) * 64], SK[:, q, hq * 128:(hq + 1) * 128], XB[:, h * 64:(h + 1) * 64],
                                     True, True, [B_SK[q], B_XB], [pub_])
                            S.copy("act", UB[:], pu_[:], [pub_], [B_UB])
                            _chk("S4")
                            py_, pyb_ = ps()
                            for cl in range(4):
                                c = half * 4 + cl
                                S.mm(py_[:, cl * 128:(cl + 1) * 128], AR[:, cl, tc, 128:256], HBZ[:, c, :], True, False,
                                     [B_AR[cl], B_HB[half]], [pyb_])
                                for hh in range(2):
                                    h = cl * 2 + hh
                                    vcol = slice((c * 2 + hh) * 64, (c * 2 + hh + 1) * 64)
                                    S.mm(py_[:, h * 64:(h + 1) * 64], SC[:, h, 128:256], UB[:, h * 64:(h + 1) * 64], False, False,
                                         [B_SC[h], B_UB], [pyb_])
                                    S.mm(py_[:, h * 64:(h + 1) * 64], SC[:, h, 384:512], VT[:, tc, vcol], False, hh == 1,
                                         [B_SC[h], B_VT[tc]], [pyb_])
                            _chk("S5")
                            ph_, phb_ = ps()
                            for cl in range(4):
                                c = half * 4 + cl
                                pc = slice(cl * 128, (cl + 1) * 128)
                                S.mm(ph_[:, pc], BTT[:, tc, pc], UB[:, pc], True, False, [B_BTT[tc], B_UB], [phb_])
                                S.mm(ph_[:, pc], KTT[:, tc, pc], VT[:, tc, c * 128:(c + 1) * 128], False, True,
                                     [B_KTT[tc], B_VT[tc]], [phb_])
                            _chk("S6")
                            for hh in range(2):
                                Ph = slice(hh * 64, (hh + 1) * 64)
                                hsv = HS[Ph, half * 4:(half + 1) * 4, :]
                                phv = ph_[Ph, :].rearrange("p (c x) -> p c x", x=128)[:, :, hh * 64:(hh + 1) * 64]
                                S.tt("dve", hsv, phv, hsv, ALU.add, [phb_, B_HS[half]], [B_HS[half]])
                                S.tt("dve", hsv, hsv, EWC[Ph, :, tc].unsqueeze(2).to_broadcast([64, 4, 64]), ALU.mult,
                                     [B_HS[half], B_EWC], [B_HS[half]])
                                S.copy("pool", HBZ[Ph, half * 4:(half + 1) * 4, hh * 64:(hh + 1) * 64], hsv,
                                       [B_HS[half]], [B_HB[half]])
                            _chk("S7")
                            y3 = py_[:].rearrange("p (h v) -> p h v", v=64)
                            S.op("dve", lambda e, y3=y3: e.tensor_reduce(out=ST8[:, 0, :], in_=y3, axis=AX.X, op=ALU.add),
                                 [pyb_], [B_ST8])
                            S.act(YA[:], py_[:], AF.Square, [pyb_], [B_YA])
                            S.op("dve", lambda e: e.tensor_reduce(out=ST8[:, 1, :], in_=YA[:].rearrange("p (h v) -> p h v", v=64),
                                                                 axis=AX.X, op=ALU.add), [B_YA], [B_ST8])
                            S.ts("dve", ST8[:, 2, :], ST8[:, 0, :], 1.0 / 64, ALU.mult, [B_ST8], [B_ST8])
                            S.tt("dve", ST8[:, 3, :], ST8[:, 2, :], ST8[:, 2, :], ALU.mult, [B_ST8], [B_ST8])
                            S.stt(ST8[:, 4, :], ST8[:, 1, :], 1.0 / 64, ST8[:, 3, :], ALU.mult, ALU.subtract, [B_ST8], [B_ST8])
                            S.act(ST8[:, 5, :], ST8[:, 4, :], AF.Sqrt, [B_ST8, B_EPSC], [B_ST8], bias=EPSC[:, 1:2])
                            S.op("dve", lambda e: e.reciprocal(out=ST8[:, 5, :], in_=ST8[:, 5, :]), [B_ST8], [B_ST8])
                            ya3 = YA[:].rearrange("p (h v) -> p h v", v=64)
                            yb3 = YB[:].rearrange("p (h v) -> p h v", v=64)
                            S.tt("dve", ya3, y3, ST8[:, 2, :].unsqueeze(2).to_broadcast([128, 8, 64]), ALU.subtract,
                                 [pyb_, B_ST8], [B_YA])
                            S.tt("dve", ya3, ya3, ST8[:, 5, :].unsqueeze(2).to_broadcast([128, 8, 64]), ALU.mult,
                                 [B_YA, B_ST8], [B_YA])
                            hc = slice(half * 512, (half + 1) * 512)
                            S.tt("pool", YA[:], YA[:], LNW[:, hc], ALU.mult, [B_YA, B_LNW], [B_YA])
                            S.tt("pool", YA[:], YA[:], LNB[:, hc], ALU.add, [B_YA, B_LNB], [B_YA])
                            S.tt("dve", yb3, VT[:, tc, hc].rearrange("p (h v) -> p h v", v=64),
                                 BDOT[:, tc, half * 8:(half + 1) * 8].unsqueeze(2).to_broadcast([128, 8, 64]), ALU.mult,
                                 [B_VT[tc], B_BDOT], [B_YB])
                            S.tt("pool", YA[:], YA[:], YB[:], ALU.add, [B_YA, B_YB], [B_YA])
                            S.tt("pool", YG[:], YA[:], SGT[:], ALU.mult, [B_YA, B_SGT], [B_YG])
                            ptt, ptb = pt()
                            for cl in range(4):
                                S.tr(ptt[:, cl * 128:(cl + 1) * 128], YG[:, cl * 128:(cl + 1) * 128], IDENT[:],
                                     [B_YG, B_IDENT], [ptb])
                            S.copy("act", YGT[:, half * 4:(half + 1) * 4, tcs],
                                   ptt[:, 0:512].rearrange("p (c j) -> p c j", j=128), [ptb], [B_YGT])

                    _chk("S")
                    for m in range(8):
                        py_, pyb_ = ps()
                        w_, wb_ = wl(li, 98 + m)
                        for kc in range(8):
                            S.mm(py_[:], w_[:, kc * 128:(kc + 1) * 128], YGT[:, kc, :], kc == 0, kc == 7,
                                 [wb_, B_YGT], [pyb_])
                        pg_, pgb_ = inproj(li, 57 + m, False)
                        S.act(SGM[:], pg_[:], AF.Sigmoid, [pgb_], [B_SGM])
                        t1, B_t1 = TMP[7]
                        S.tt("dve", t1[:], py_[:], SGM[:], ALU.mult, [pyb_, B_SGM], [B_t1])
                        S.tt("pool", MG[:, m, :], t1[:], MP[:, m, :], ALU.add, [B_t1, B_MP], [B_MG[m]])
                    _chk("O1")
                    wo = [wr(li, 2), wr(li, 3)]
                    for tg in range(4):
                        xi, B_xi = XIN[tg % 2]
                        rows = slice(row0 + tg * 128, row0 + (tg + 1) * 128)
                        S.dma("sp", xi[:], x_src[rows, :], Rxs, [B_xi])
                        for hf in range(2):
                            po_, pob_ = ps()
                            for kc in range(8):
                                S.mm(po_[:], MG[:, kc, tg * 128:(tg + 1) * 128], wo[hf][0][:, kc * 512:(kc + 1) * 512],
                                     kc == 0, kc == 7, [B_MG, wo[hf][1]], [pob_])
                            S.tt("dve", xi[:, hf * 512:(hf + 1) * 512], po_[:], xi[:, hf * 512:(hf + 1) * 512], ALU.add,
                                 [pob_, B_xi], [B_xi])
                        if last:
                            S.act(JUNK[:], xi[:], AF.Square, [B_xi], [B_JUNK, B_STAT], accum_out=STAT[:, 4:5])
                            S.act(STAT[:, 5:6], STAT[:, 4:5], AF.Sqrt, [B_STAT, B_EPSC], [B_STAT], bias=EPSC[:, 0:1], scale=1.0 / D)
                            S.op("dve", lambda e: e.reciprocal(out=STAT[:, 6:7], in_=STAT[:, 5:6]), [B_STAT], [B_STAT])
                            S.stt(xi[:], xi[:], STAT[:, 6:7], FG[:], ALU.mult, ALU.mult, [B_xi, B_STAT, B_FG], [B_xi])
                        _chk("O2")
                        S.dma("act", x_dst[rows, :], xi[:], [B_xi], Wxd)
                        _chk("O3")
                    _chk("T")


def _pack_pp(inp, L0, L1):
    nl = L1 - L0
    pp = np.zeros((nl, 128, NPAR, 8), np.float32)
    for li in range(nl):
        L = L0 + li
        vecs = [inp["pool_scale"][L], inp["decay_w0"][L], inp["a0"][L],
                inp["vres_v0"][L - 1] if L > 0 else None, inp["k_k"][L], inp["k_a"][L],
                np.asarray(inp["r_k"][L]).reshape(-1)]
        for k, v in enumerate(vecs):
            if v is None:
                continue
            pp[li, :, k, :] = np.asarray(v, np.float32).reshape(8, 128).T
    return pp.reshape(nl, 128, NPAR * 8)


def _layer_inputs(inp, L0, L1):
    f = lambda a: np.ascontiguousarray(np.asarray(a, np.float32))
    nl = L1 - L0
    v1 = np.zeros((nl, D, 32), np.float32)
    v2 = np.zeros((nl, 32, D), np.float32)
    for li in range(nl):
        L = L0 + li
        if L > 0:
            v1[li] = inp["vres_v1"][L - 1]
            v2[li] = inp["vres_v2"][L - 1]
    return {
        "w_in": f(inp["w_in"][L0:L1]), "pool_lin": f(inp["pool_lin"][L0:L1]),
        "w_pool_proj": f(inp["w_pool_proj"][L0:L1]), "mu_shift": f(inp["mu_shift"][L0:L1]),
        "decay_w2": f(inp["decay_w2"][L0:L1]), "a2": f(inp["a2"][L0:L1]),
        "vres_v1": v1, "vres_v2": v2,
        "w_rwkv_proj": f(inp["w_rwkv_proj"][L0:L1]), "w_out": f(inp["w_out"][L0:L1]),
        "norm_g": f(inp["norm_g"][L0:L1]), "lnx_w": f(inp["lnx_w"][L0:L1]), "lnx_b": f(inp["lnx_b"][L0:L1]),
        "final_g": f(inp["final_g"]).reshape(1, D), "pp": _pack_pp(inp, L0, L1),
    }


_PROG_CACHE = {}
STOP = None


class _Stop(Exception):
    pass


_CNT = [0]


_TILE = [0]


def _chk(name):
    if name == "T":
        _TILE[0] += 1
    if STOP == name or STOP == "%s@%d" % (name, _TILE[0]):
        raise _Stop()
    if name == "cnt" and STOP is not None and STOP.startswith("cnt"):
        _CNT[0] += 1
        if _CNT[0] >= int(STOP[3:]):
            raise _Stop()


def run_layers(inp, xs, L0, L1, NT, vfirst=None, cores=None):
    key = (L0, L1, NT)
    if key not in _PROG_CACHE:
        _PROG_CACHE[key] = build_program(L0, L1, NT, True)
    nc = _PROG_CACHE[key]
    shared = _layer_inputs(inp, L0, L1)
    in_maps = []
    for i, xc in enumerate(xs):
        m = dict(shared)
        m["x"] = np.ascontiguousarray(xc)
        if L0 > 0:
            m["vfirst_in"] = vfirst[i]
        in_maps.append(m)
    cores = list(range(len(xs))) if cores is None else cores
    res = run_bass_kernel_spmd(nc, in_maps, core_ids=cores)
    outs = [r["out"] for r in res.results]
    vf = [r["vfirst_out"] for r in res.results] if (L0 == 0 and L1 < DEPTH) else None
    return outs, vf


FUSED = True


def kernel(**inputs):
    x = np.asarray(inputs["x"], np.float32)
    xs = [np.ascontiguousarray(x[2 * i:2 * i + 2].reshape(NSEQ * SEQ, D)) for i in range(8)]
    if FUSED:
        outs, _ = run_layers(inputs, xs, 0, DEPTH, SEQ // T)
    else:
        outs, vf = run_layers(inputs, xs, 0, 1, SEQ // T)
        for L in range(1, DEPTH):
            outs, _ = run_layers(inputs, outs, L, L + 1, SEQ // T, vfirst=vf)
    return np.stack([o.reshape(NSEQ, SEQ, D) for o in outs], 0).reshape(16, SEQ, D).astype(np.float32)
```

```python
import numpy as np
from contextlib import ExitStack
import concourse.bass as bass
import concourse.mybir as mybir
from concourse.bass_utils import run_bass_kernel_spmd

F32 = mybir.dt.float32
BF16 = mybir.dt.bfloat16
AF = mybir.ActivationFunctionType
ALU = mybir.AluOpType
AX = mybir.AxisListType

D = 1024
SEQ = 2048
NSEQ = 2
DEPTH = 4
T = 512
NTC = 4
IN_W = 8320
NWL = 106
NORM_EPS = 1e-6
LNX_EPS = 1e-5 * 64
DECAY_C = 0.6065306597126334
POOL_WINDOWS = (2, 4, 8, 16)
NPAR = 8


class Buf:
    __slots__ = ("name", "lw", "rd", "const")

    def __init__(self, name, const=False):
        self.name = name
        self.lw = None
        self.rd = []
        self.const = const


class Sched:
    ENGS = ("pe", "act", "dve", "pool", "sp")

    def __init__(self, nc, stack, n_dsem=32, strict=("act", "dve", "pool")):
        self.nc = nc
        self.sem = {e: stack.enter_context(nc.semaphore("s_" + e)) for e in self.ENGS}
        self.dsem = [stack.enter_context(nc.semaphore("d%d" % i)) for i in range(n_dsem)]
        self.dval = [0] * n_dsem
        self.dnext = 0
        self.cnt = {e: 0 for e in self.ENGS}
        self.prog = {e: [] for e in self.ENGS}
        self.seen = {e: {} for e in self.ENGS}
        self.strict = set(strict)
        self.nwait = 0
        self.ninst = 0

    @staticmethod
    def _flat(x):
        out = []
        for b in x:
            if isinstance(b, (list, tuple)):
                out.extend(Sched._flat(b))
            elif b is not None:
                out.append(b)
        return out

    def _deps(self, eng, reads, writes):
        deps = set()
        for b in reads:
            if b.lw is not None:
                deps.add(b.lw)
        for b in writes:
            if b.lw is not None:
                deps.add(b.lw)
            for t in b.rd:
                deps.add(t)
        need = {}
        for (kind, ident, val) in deps:
            if kind == "e" and ident == eng and eng not in self.strict:
                continue
            key = (kind, ident)
            if self.seen[eng].get(key, 0) >= val:
                continue
            if need.get(key, 0) < val:
                need[key] = val
        for key, val in need.items():
            self.seen[eng][key] = val
            sem = self.sem[key[1]] if key[0] == "e" else self.dsem[key[1]]
            self.prog[eng].append(("w", sem, val))
            self.nwait += 1

    def _commit(self, tok, reads, writes):
        for b in writes:
            b.lw = tok
            b.rd = []
        for b in reads:
            if b.const or b in writes:
                continue
            b.rd.append(tok)
            if len(b.rd) > 400:
                b.rd = b.rd[-400:]

    def op(self, eng, fn, reads=(), writes=()):
        reads, writes = self._flat(reads), self._flat(writes)
        self._deps(eng, reads, writes)
        self.cnt[eng] += 1
        n = self.cnt[eng]
        self.prog[eng].append(("i", fn, self.sem[eng], 1))
        self._commit(("e", eng, n), reads, writes)
        self.ninst += 1

    def dma(self, q, out, in_, reads=(), writes=(), **kw):
        reads, writes = self._flat(reads), self._flat(writes)
        k = self.dnext
        self.dnext = (self.dnext + 1) % len(self.dsem)
        if self.dval[k] > 0 and self.seen[q].get(("d", k), 0) < self.dval[k]:
            self.seen[q][("d", k)] = self.dval[k]
            self.prog[q].append(("w", self.dsem[k], self.dval[k]))
        self._deps(q, reads, writes)
        self.dval[k] += 16
        v = self.dval[k]
        self.prog[q].append(("i", lambda e: e.dma_start(out=out, in_=in_, **kw), self.dsem[k], 16))
        self._commit(("d", k, v), reads, writes)
        self.ninst += 1

    def barrier(self):
        for e in self.ENGS:
            for p in self.ENGS:
                if p == e or self.cnt[p] == 0:
                    continue
                if self.seen[e].get(("e", p), 0) < self.cnt[p]:
                    self.seen[e][("e", p)] = self.cnt[p]
                    self.prog[e].append(("w", self.sem[p], self.cnt[p]))
            for k, v in enumerate(self.dval):
                if v > 0 and self.seen[e].get(("d", k), 0) < v:
                    self.seen[e][("d", k)] = v
                    self.prog[e].append(("w", self.dsem[k], v))

    def finish(self, q="sp"):
        for k, v in enumerate(self.dval):
            if v > 0:
                self.prog[q].append(("w", self.dsem[k], v))

    def run(self, block):
        def body(prog):
            def f(e):
                for it in prog:
                    if it[0] == "w":
                        e.wait_ge(it[1], it[2])
                    else:
                        it[1](e).then_inc(it[2], it[3])
            return f
        block.tensor(body(self.prog["pe"]))
        block.scalar(body(self.prog["act"]))
        block.vector(body(self.prog["dve"]))
        block.gpsimd(body(self.prog["pool"]))
        block.sync(body(self.prog["sp"]))

    def mm(self, out, lhsT, rhs, start, stop, R, W):
        self.op("pe", lambda e: e.matmul(out, lhsT, rhs, start=start, stop=stop), R, W)

    def tr(self, out, in_, ident, R, W):
        self.op("pe", lambda e: e.transpose(out, in_, ident), R, W)

    def act(self, out, in_, func, R, W, bias=None, scale=None, accum_out=None):
        kw = {}
        if bias is not None:
            kw["bias"] = bias
        if scale is not None:
            kw["scale"] = scale
        if accum_out is not None:
            kw["accum_out"] = accum_out
        self.op("act", lambda e: e.activation(out=out, in_=in_, func=func, **kw), R, W)

    def tt(self, eng, out, in0, in1, op, R, W):
        self.op(eng, lambda e: e.tensor_tensor(out=out, in0=in0, in1=in1, op=op), R, W)

    def ts(self, eng, out, in0, s1, op0, R, W, s2=None, op1=None):
        if op1 is None:
            self.op(eng, lambda e: e.tensor_scalar(out=out, in0=in0, scalar1=s1, scalar2=None, op0=op0), R, W)
        else:
            self.op(eng, lambda e: e.tensor_scalar(out=out, in0=in0, scalar1=s1, scalar2=s2, op0=op0, op1=op1), R, W)

    def stt(self, out, in0, scalar, in1, op0, op1, R, W):
        self.op("dve", lambda e: e.scalar_tensor_tensor(out=out, in0=in0, scalar=scalar, in1=in1, op0=op0, op1=op1), R, W)

    def copy(self, eng, out, in_, R, W):
        if eng == "act":
            self.op("act", lambda e: e.activation(out=out, in_=in_, func=AF.Copy), R, W)
        else:
            self.op(eng, lambda e: e.tensor_copy(out=out, in_=in_), R, W)

    def memset(self, eng, ap, val, W):
        self.op(eng, lambda e: e.memset(ap, val), (), W)


def build_program(L0, L1, NT, final_norm):
    NL = L1 - L0
    NTOK = NSEQ * SEQ
    nc = bass.Bass("TRN2", target_bir_lowering=False, dynamic_dma_scratch_size=2048)
    dt_in = lambda name, shape: nc.dram_tensor(name, shape, F32, kind="ExternalInput").ap()
    x_in = dt_in("x", [NTOK, D])
    w_in = dt_in("w_in", [NL, D, IN_W])
    pool_lin = dt_in("pool_lin", [NL, 4, 256, 256])
    w_pool_proj = dt_in("w_pool_proj", [NL, D, D])
    mu_shift = dt_in("mu_shift", [NL, 3200])
    decay_w2 = dt_in("decay_w2", [NL, 64, D])
    a2 = dt_in("a2", [NL, 64, D])
    vres_v1 = dt_in("vres_v1", [NL, D, 32])
    vres_v2 = dt_in("vres_v2", [NL, 32, D])
    w_rwkv_proj = dt_in("w_rwkv_proj", [NL, D, D])
    w_out = dt_in("w_out", [NL, D, D])
    norm_g = dt_in("norm_g", [NL, D])
    lnx_w = dt_in("lnx_w", [NL, D])
    lnx_b = dt_in("lnx_b", [NL, D])
    final_g = dt_in("final_g", [1, D])
    pp_in = dt_in("pp", [NL, 128, NPAR * 8])
    out_d = nc.dram_tensor("out", [NTOK, D], F32, kind="ExternalOutput").ap()
    NVT = NSEQ * NT
    if L0 == 0 and L1 < DEPTH:
        vfd = nc.dram_tensor("vfirst_out", [NVT, 128, 8 * T], F32, kind="ExternalOutput").ap()
    elif L0 == 0:
        vfd = nc.dram_tensor("vfirst_scr", [NVT, 128, 8 * T], F32, kind="Internal").ap()
    else:
        vfd = dt_in("vfirst_in", [NVT, 128, 8 * T])
    xbufs = [nc.dram_tensor("xbuf%d" % i, [NTOK, D], F32, kind="Internal").ap() for i in range(2)] if NL > 1 else []
    WLd = nc.dram_tensor("wl_scr", [NL, NWL, 128, 1024], BF16, kind="Internal").ap()
    WRd = nc.dram_tensor("wr_scr", [NL, 4, 128, 4096], BF16, kind="Internal").ap()
    B_vfd = Buf("vfd")
    B_xb = [Buf("xb0"), Buf("xb1")]
    B_WLd = [[Buf("wld") for _ in range(NWL)] for _ in range(NL)]
    B_WRd = [[Buf("wrd") for _ in range(4)] for _ in range(NL)]

    with ExitStack() as st:
        S = Sched(nc, st)

        def mk(stack, name, shape, dt, nb=0):
            t = stack.enter_context(nc.sbuf_tensor(name, shape, dt))
            if nb:
                return t, [Buf("%s%d" % (name, i)) for i in range(nb)]
            return t, Buf(name)

        with ExitStack() as pst:
            STG = [mk(pst, "stg%d" % i, [128, 8, 512], F32) for i in range(2)]
            CB = [mk(pst, "cb%d" % i, [128, 4, 1024], BF16) for i in range(2)]
            CB2 = [mk(pst, "cbp%d" % i, [128, 4, 1024], BF16) for i in range(2)]
            MU, B_MU = mk(pst, "mu_bc", [128, 3200], F32)
            MU1, B_MU1 = mk(pst, "mu1_bc", [128, 3200], F32)
            cast_engs = ["pool", "dve", "act"]
            ce = [0]

            def cast(out, in_, R, W):
                e = cast_engs[ce[0] % 3]
                ce[0] += 1
                S.copy(e, out, in_, R, W)

            blk = [0]
            for li in range(NL):
                S.dma("sp", MU[:], mu_shift[li:li + 1, :].to_broadcast([128, 3200]), (), [B_MU])
                S.ts("pool", MU1[:], MU[:], -1.0, ALU.mult, [B_MU], [B_MU1], s2=1.0, op1=ALU.add)
                segs = []
                for c0 in list(range(0, 5248, 512)):
                    segs.append((w_in[li], c0, min(512, 5248 - c0), "L", c0 // 128))
                for c0 in range(6272, 8320, 512):
                    segs.append((w_in[li], c0, 512, "L", c0 // 128))
                segs.append((w_in[li], 5248, 512, "R", 0))
                segs.append((w_in[li], 5760, 512, "R", 1))
                for c0 in (0, 512):
                    segs.append((w_pool_proj[li], c0, 512, "L", 90 + c0 // 128))
                for c0 in (0, 512):
                    segs.append((w_rwkv_proj[li], c0, 512, "L", 98 + c0 // 128))
                segs.append((w_out[li], 0, 512, "R", 2))
                segs.append((w_out[li], 512, 512, "R", 3))
                for (src, c0, ncol, kind, base) in segs:
                    b = blk[0] % 2
                    blk[0] += 1
                    stg, B_stg = STG[b]
                    cb, B_cb = CB[b]
                    cb2, B_cb2 = CB2[b]
                    S.dma("sp", stg[:, :, 0:ncol], src[:, c0:c0 + ncol].rearrange("(kc kp) c -> kp kc c", kp=128),
                          (), [B_stg])
                    if kind == "R":
                        cbr = cb[:].rearrange("p a f -> p (a f)").rearrange("p (kc j) -> p kc j", j=512)
                        for kc in range(8):
                            cast(cbr[:, kc, :], stg[:, kc, :], [B_stg], [B_cb])
                        S.dma("act", WRd[li, base], cb[:].rearrange("p a f -> p (a f)"), [B_cb], [B_WRd[li][base]])
                        continue
                    nm = ncol // 128
                    for mi in range(nm):
                        m = base + mi
                        o = cb[:, mi, :].rearrange("p (kc j) -> p kc j", j=128)
                        i_ = stg[:, :, mi * 128:(mi + 1) * 128]
                        is_shift = (base < 90) and (16 <= m <= 40)
                        if is_shift:
                            mc = (m - 16) * 128
                            S.tt("pool" if mi % 2 == 0 else "dve", o, i_,
                                 MU1[:, mc:mc + 128].unsqueeze(1).to_broadcast([128, 8, 128]), ALU.mult,
                                 [B_stg, B_MU1], [B_cb])
                            o2 = cb2[:, mi, :].rearrange("p (kc j) -> p kc j", j=128)
                            S.tt("dve" if mi % 2 == 0 else "pool", o2, i_,
                                 MU[:, mc:mc + 128].unsqueeze(1).to_broadcast([128, 8, 128]), ALU.mult,
                                 [B_stg, B_MU], [B_cb2])
                            S.dma("act", WLd[li, 65 + m - 16], cb2[:, mi, :], [B_cb2], [B_WLd[li][65 + m - 16]])
                        else:
                            cast(o, i_, [B_stg], [B_cb])
                        S.dma("act", WLd[li, m], cb[:, mi, :], [B_cb], [B_WLd[li][m]])
        S.barrier()
        try:
            _chk("prepass")
            _main(locals())
        except _Stop:
            pass
        S.finish("sp")
        print("program: %d instructions, %d waits" % (S.ninst, S.nwait), {e: S.cnt[e] for e in S.ENGS}, flush=True)
        with nc.Block() as block:
            S.run(block)
    return nc


def _main(env):
    globals_ = env
    nc = env["nc"]; st = env["st"]; S = env["S"]; mk = env["mk"]
    NL = env["NL"]; L0 = env["L0"]; L1 = env["L1"]; NT = env["NT"]; final_norm = env["final_norm"]
    x_in = env["x_in"]; out_d = env["out_d"]; vfd = env["vfd"]; xbufs = env["xbufs"]
    WLd = env["WLd"]; WRd = env["WRd"]; B_vfd = env["B_vfd"]; B_xb = env["B_xb"]; B_WLd = env["B_WLd"]; B_WRd = env["B_WRd"]
    pool_lin = env["pool_lin"]; decay_w2 = env["decay_w2"]; a2 = env["a2"]; vres_v1 = env["vres_v1"]; vres_v2 = env["vres_v2"]
    norm_g = env["norm_g"]; lnx_w = env["lnx_w"]; lnx_b = env["lnx_b"]; final_g = env["final_g"]; pp_in = env["pp_in"]
    if True:
        IDENT, B_IDENT = mk(st, "ident", [128, 128], BF16)
        MASKT, B_MASKT = mk(st, "maskt", [128, 512], BF16)
        MASKL, B_MASKL = mk(st, "maskl", [128, 128], BF16)
        BONES, B_BONES = mk(st, "bones", [128, 128], BF16)
        E2, B_E2 = mk(st, "e2", [128, 2], BF16)
        ONESF, B_ONESF = mk(st, "onesf", [128, 128], F32)
        CORR, B_CORR = mk(st, "corr", [128, 4, 16], F32)
        EPSC, B_EPSC = mk(st, "epsc", [128, 2], F32)
        for b_ in (B_IDENT, B_MASKT, B_MASKL, B_BONES, B_E2, B_ONESF, B_CORR, B_EPSC):
            b_.const = True
        G_BC, B_G = mk(st, "g_bc", [128, D], F32)
        LNW, B_LNW = mk(st, "lnw_bc", [128, D], F32)
        LNB, B_LNB = mk(st, "lnb_bc", [128, D], F32)
        FG, B_FG = mk(st, "fg_bc", [128, D], F32)
        PP, B_PP = mk(st, "pp_sb", [128, NPAR, 8], F32)
        PL, B_PL = mk(st, "pl", [128, 4, 2, 256], BF16)
        DAZ, B_DA = mk(st, "daz", [128, 2, D], BF16)
        V1W, B_V1W = mk(st, "v1w", [128, 8, 128], BF16)
        V2W, B_V2W = mk(st, "v2w", [128, D], BF16)
        HT, B_HT = mk(st, "ht", [128, 8, T + 1], BF16)
        MP, B_MP = mk(st, "mp", [128, 8, T], BF16)
        YGT, B_YGT = mk(st, "ygt", [128, 8, T], BF16)
        YPI, B_YPI = YGT, B_YGT
        XIN = [mk(st, "xin%d" % i, [128, D], F32) for i in range(2)]
        HN, B_HN = mk(st, "hn", [128, D], BF16)
        JUNK, B_JUNK = HN, B_HN
        STAT, B_STAT = mk(st, "stat", [128, 8], F32)
        NRL = 6
        WLB = [mk(st, "wlb%d" % i, [128, 1024], BF16) for i in range(NRL)]
        WRB = [mk(st, "wrb%d" % i, [128, 4096], BF16) for i in range(2)]
        NTMP = 10
        TMPALL, B_TMP = mk(st, "tmpall", [128, NTMP, T + 16], F32, nb=NTMP)
        TMP = [(TMPALL[:, i, 0:T], B_TMP[i]) for i in range(NTMP)]
        UG, B_UG = TMPALL[:, 0:2, :], [B_TMP[0], B_TMP[1]]
        SA, B_SA = TMPALL[:, 2:4, :], [B_TMP[2], B_TMP[3]]
        SBb, B_SBb = TMPALL[:, 4:6, :], [B_TMP[4], B_TMP[5]]
        SG, B_SG = TMPALL[:, 6:8, 0:T], [B_TMP[6], B_TMP[7]]
        SGM, B_SGM = TMP[8]
        CTMP, B_CTMP = TMP[9]
        DG, B_DG = mk(st, "dg", [128, 2, T], BF16)
        HALO_P, B_HALOP = mk(st, "halop", [128, 8, 16], F32)
        VF, B_VF = mk(st, "vf", [128, 8, T], F32, nb=8)
        STGS, B_STGS = VF[:].rearrange("p c t -> p (c t)")[:, 0:2048], B_VF[0:4]
        VB, B_VB = mk(st, "vb", [128, 8, T], BF16, nb=8)
        VT, B_VT = mk(st, "vt", [128, NTC, D], BF16, nb=NTC)
        TWA, B_TWA = mk(st, "twa", [128, T], BF16)
        P1, B_P1 = mk(st, "p1", [128, T], BF16)
        BTZ, B_BT = mk(st, "btz", [128, 2, 4, T], BF16, nb=4)
        KTZ, B_KT = mk(st, "ktz", [128, 2, 4, T], BF16, nb=4)
        AR, B_AR = mk(st, "ar", [128, 4, NTC, 256], BF16, nb=4)
        BTT, B_BTT = mk(st, "btt", [128, NTC, 512], BF16, nb=NTC)
        KTT, B_KTT = mk(st, "ktt", [128, NTC, 512], BF16, nb=NTC)
        RKB, B_RKB = mk(st, "rkb", [128, T], BF16)
        EWC, B_EWC = mk(st, "ewc", [128, 4, NTC], F32)
        BDOT, B_BDOT = mk(st, "bdot", [128, NTC, 16], F32)
        SC, B_SC = mk(st, "sc", [128, 8, 512], BF16, nb=8)
        PK = [mk(st, "pk%d" % i, [128, 2, 512], BF16, nb=2) for i in range(2)]
        QK = [mk(st, "qk%d" % i, [128, 2, 512], BF16, nb=2) for i in range(2)]
        SK, B_SK = mk(st, "sk", [128, 2, 512], BF16, nb=2)
        SCXv = TMPALL[:, 3:7, :].rearrange("p a b -> p (a b)").bitcast(BF16)[:, 0:4096].rearrange("p (h f) -> p h f", f=512)
        B_SCX = [Buf("scx%d" % i) for i in range(8)]
        SKXv = TMPALL[:, 7, :].bitcast(BF16)[:, 0:1024].rearrange("p (q f) -> p q f", f=512)
        B_SKX = [Buf("skx%d" % i) for i in range(2)]
        ALIAS_TMP = [B_TMP[i] for i in range(3, 8)]
        ALIAS_SCAN = B_SCX + B_SKX

        def handoff(src, dst):
            toks = set()
            for b_ in src:
                if b_.lw is not None:
                    toks.add(b_.lw)
                toks.update(b_.rd)
            for d_ in dst:
                d_.rd = list(set(d_.rd) | toks)

        def sc_slot(slot):
            if slot < 2:
                return SC[:, slot * 4:(slot + 1) * 4, :], B_SC[slot * 4:(slot + 1) * 4]
            return SCXv[:, (slot - 2) * 4:(slot - 1) * 4, :], B_SCX[(slot - 2) * 4:(slot - 1) * 4]

        def sk_slot(slot):
            if slot < 2:
                return SK[:, slot, :], B_SK[slot]
            return SKXv[:, slot - 2, :], B_SKX[slot - 2]

        def pq_slot(slot):
            i_, j_ = slot // 2, slot % 2
            return (PK[i_][0][:, j_, :], PK[i_][1][j_]), (QK[i_][0][:, j_, :], QK[i_][1][j_])

        XB, B_XB = mk(st, "xbv", [128, 512], BF16, nb=2)
        UB, B_UB = mk(st, "ubv", [128, 512], BF16, nb=2)
        HS, B_HS = mk(st, "hs", [128, 8, 64], F32, nb=4)
        HBZ, B_HB = mk(st, "hbz", [128, 8, 128], BF16, nb=4)
        YA, B_YA = TMP[0][0], [TMP[0][1], Buf("ya1")]
        YB, B_YB = TMP[1][0], [TMP[1][1], Buf("yb1")]
        SGT, B_SGT = TMP[2]
        YG, B_YG = mk(st, "yg", [128, 512], BF16, nb=2)
        ST8, B_ST8 = mk(st, "st8", [128, 6, 8], F32, nb=2)
        MG, B_MG = VB, B_VB
        PSB = []
        for i in range(8):
            t_ = st.enter_context(nc.psum_tensor("ps%d" % i, [128, 512], F32))
            PSB.append((t_, Buf("ps%d" % i)))
        PTB = [(PSB[6][0][:].bitcast(BF16), PSB[6][1]), (PSB[7][0][:].bitcast(BF16), PSB[7][1])]
        ring = [0, 0, 0, 0, 0, 0]

        def ps():
            r = PSB[ring[0] % 5]
            ring[0] += 1
            return r

        PBD, B_PBD = PSB[5]

        def pt():
            r = PTB[ring[1] % 2]
            ring[1] += 1
            return r

        def psL():
            r = PSB[ring[4] % 4]
            ring[4] += 1
            return r

        def psS():
            r = PSB[(4, 6, 7)[ring[5] % 3]]
            ring[5] += 1
            return r

        def tmp():
            r = TMP[ring[3] % NTMP]
            ring[3] += 1
            return r

        def tri(dst_ap, cmp_op, pattern_step, cm):
            S.memset("pool", CTMP[:, 0:128], 1.0, [B_CTMP])
            S.op("pool", lambda e: e.affine_select(out=CTMP[:, 0:128], in_=CTMP[:, 0:128], pattern=[[pattern_step, 128]],
                                                   compare_op=cmp_op, fill=0.0, base=0, channel_multiplier=cm),
                 [B_CTMP], [B_CTMP])
            S.copy("pool", dst_ap, CTMP[:, 0:128], [B_CTMP], [B_IDENT])

        tri(IDENT[:], ALU.is_equal, -1, 1)
        tri(MASKL[:], ALU.is_gt, -1, 1)
        tri(MASKT[:, 0:128], ALU.is_gt, 1, -1)
        tri(MASKT[:, 128:256], ALU.is_ge, 1, -1)
        tri(MASKT[:, 256:384], ALU.is_gt, 1, -1)
        tri(MASKT[:, 384:512], ALU.is_ge, 1, -1)
        S.memset("pool", BONES[:], 0.0, [B_IDENT])
        S.memset("pool", BONES[0:64, 0:64], 1.0, [B_IDENT])
        S.memset("pool", BONES[64:128, 64:128], 1.0, [B_IDENT])
        S.memset("pool", E2[:], 0.0, [B_IDENT])
        S.memset("pool", E2[0:64, 0:1], 1.0, [B_IDENT])
        S.memset("pool", E2[64:128, 1:2], 1.0, [B_IDENT])
        S.memset("pool", ONESF[:], 1.0, [B_IDENT])
        S.memset("pool", CORR[:], 1.0, [B_IDENT])
        for g, w in enumerate(POOL_WINDOWS):
            for t_ in range(w - 1):
                S.memset("pool", CORR[:, g, t_:t_ + 1], float(w) / float(t_ + 1), [B_IDENT])
        S.memset("pool", BTZ[:], 0.0, B_BT)
        S.memset("pool", KTZ[:], 0.0, B_KT)
        S.memset("pool", HBZ[:], 0.0, B_HB)
        S.memset("pool", DAZ[:], 0.0, [B_DA])
        S.memset("pool", V1W[:], 0.0, [B_V1W])
        S.memset("pool", V2W[:], 0.0, [B_V2W])
        S.memset("pool", EPSC[:, 0:1], NORM_EPS, [B_IDENT])
        S.memset("pool", EPSC[:, 1:2], LNX_EPS, [B_IDENT])
        S.dma("sp", FG[:], final_g.to_broadcast([128, D]), (), [B_FG])
        _chk("consts")

        def wl(li, idx):
            t_, b_ = WLB[ring[2] % NRL]
            ring[2] += 1
            S.dma("sp", t_[:], WLd[li, idx], [B_WLd[li][idx]], [b_])
            return t_, b_

        wr_i = [0]

        def wr(li, idx):
            t_, b_ = WRB[wr_i[0] % 2]
            wr_i[0] += 1
            S.dma("sp", t_[:], WRd[li, idx], [B_WRd[li][idx]], [b_])
            return t_, b_

        def inproj(li, m, shift, alloc=None):
            pt_, pb_ = (alloc or ps)()
            w_, wb_ = wl(li, m)
            for kc in range(8):
                S.mm(pt_[:], w_[:, kc * 128:(kc + 1) * 128], HT[:, kc, 1:T + 1], kc == 0, (kc == 7 and not shift),
                     [wb_, B_HT], [pb_])
            if shift:
                w2, wb2 = wl(li, 65 + m - 16)
                for kc in range(8):
                    S.mm(pt_[:], w2[:, kc * 128:(kc + 1) * 128], HT[:, kc, 0:T], False, kc == 7,
                         [wb2, B_HT], [pb_])
            return pt_, pb_

        for li in range(NL):
            L = L0 + li
            last = (L == DEPTH - 1) and final_norm
            x_src, B_xs = (x_in, None) if li == 0 else (xbufs[(li - 1) % 2], B_xb[(li - 1) % 2])
            x_dst, B_xd = (out_d, None) if li == NL - 1 else (xbufs[li % 2], B_xb[li % 2])
            Rxs = [B_xs] if B_xs is not None else []
            Wxd = [B_xd] if B_xd is not None else []
            S.dma("sp", G_BC[:], norm_g[li:li + 1, :].to_broadcast([128, D]), (), [B_G])
            S.dma("sp", LNW[:], lnx_w[li:li + 1, :].to_broadcast([128, D]), (), [B_LNW])
            S.dma("sp", LNB[:], lnx_b[li:li + 1, :].to_broadcast([128, D]), (), [B_LNB])
            S.dma("sp", PP[:].rearrange("p k c -> p (k c)"), pp_in[li], (), [B_PP])
            S.ts("pool", PP[:, 7, :], PP[:, 5, :], -1.0, ALU.mult, [B_PP], [B_PP], s2=1.0, op1=ALU.add)
            S.dma("sp", STGS[:].rearrange("p (g kc d) -> p g kc d", g=4, kc=2),
                  pool_lin[li].rearrange("g (kc kp) d -> kp g kc d", kp=128), (), [B_STGS])
            S.copy("pool", PL[:].rearrange("p g kc d -> p (g kc d)"), STGS[:], [B_STGS], [B_PL])
            S.dma("sp", STGS[0:64, 0:D], decay_w2[li], [], [B_STGS])
            S.dma("sp", STGS[64:128, 0:D], a2[li], [], [B_STGS])
            S.copy("pool", DAZ[0:64, 0, :], STGS[0:64, 0:D], [B_STGS], [B_DA])
            S.copy("pool", DAZ[64:128, 1, :], STGS[64:128, 0:D], [B_STGS], [B_DA])
            if L > 0:
                S.dma("sp", STGS[:, 0:256].rearrange("p (kc j) -> p kc j", j=32),
                      vres_v1[li].rearrange("(kc kp) j -> kp kc j", kp=128), [], [B_STGS])
                S.copy("pool", V1W[:, :, 0:32], STGS[:, 0:256].rearrange("p (kc j) -> p kc j", j=32), [B_STGS], [B_V1W])
                S.dma("sp", STGS[0:32, 0:D], vres_v2[li], [], [B_STGS])
                S.copy("pool", V2W[0:32, :], STGS[0:32, 0:D], [B_STGS], [B_V2W])
            ppc = lambda k, c: PP[:, k, c:c + 1]
            _chk("params")

            for s in range(NSEQ):
                for ti in range(NT):
                    row0 = s * SEQ + ti * T
                    vt_idx = s * NT + ti
                    first = (ti == 0)
                    if first:
                        S.memset("pool", HT[:, :, 0:1], 0.0, [B_HT])
                    else:
                        S.copy("pool", HT[:, :, 0:1], HT[:, :, T:T + 1], [B_HT], [B_HT])
                    for tg in range(4):
                        xi, B_xi = XIN[tg % 2]
                        S.dma("sp", xi[:], x_src[row0 + tg * 128: row0 + (tg + 1) * 128, :], Rxs, [B_xi])
                        S.act(JUNK[:], xi[:], AF.Square, [B_xi], [B_JUNK, B_STAT], accum_out=STAT[:, 0:1])
                        S.act(STAT[:, 1:2], STAT[:, 0:1], AF.Sqrt, [B_STAT, B_EPSC], [B_STAT], bias=EPSC[:, 0:1], scale=1.0 / D)
                        S.op("dve", lambda e: e.reciprocal(out=STAT[:, 2:3], in_=STAT[:, 1:2]), [B_STAT], [B_STAT])
                        S.stt(HN[:], xi[:], STAT[:, 2:3], G_BC[:], ALU.mult, ALU.mult, [B_xi, B_STAT, B_G], [B_HN])
                        ptt, ptb = pt()
                        for kc in range(8):
                            S.tr(ptt[:, kc * 128:(kc + 1) * 128], HN[:, kc * 128:(kc + 1) * 128], IDENT[:],
                                 [B_HN, B_IDENT], [ptb])
                        S.copy("act", HT[:, :, 1 + tg * 128: 1 + (tg + 1) * 128],
                               ptt[:].rearrange("p (kc j) -> p kc j", j=128), [ptb], [B_HT])

                    _chk("N")
                    if first:
                        S.memset("pool", HALO_P[:], 0.0, [B_HALOP])
                    for g, w in enumerate(POOL_WINDOWS):
                        for j in range(2):
                            p_, pb_ = inproj(li, 2 * g + j, False)
                            S.copy("act", UG[:, j, 16:16 + T], p_[:], [pb_], [B_UG])
                        for j in range(2):
                            p_, pb_ = inproj(li, 8 + 2 * g + j, False)
                            S.act(SG[:, j, :], p_[:], AF.Silu, [pb_], [B_SG])
                        S.copy("pool", UG[:, :, 0:16], HALO_P[:, 2 * g:2 * g + 2, :], [B_HALOP], [B_UG])
                        S.copy("pool", HALO_P[:, 2 * g:2 * g + 2, :], UG[:, :, T:T + 16], [B_UG], [B_HALOP])
                        NJ = T + 16
                        S.tt("pool", SA[:, :, 1:NJ], UG[:, :, 1:NJ], UG[:, :, 0:NJ - 1], ALU.add, [B_UG], [B_SA])
                        cur, B_cur = SA, B_SA
                        if w >= 4:
                            S.tt("pool", SBb[:, :, 3:NJ], SA[:, :, 3:NJ], SA[:, :, 1:NJ - 2], ALU.add, [B_SA], [B_SBb])
                            cur, B_cur = SBb, B_SBb
                        if w >= 8:
                            S.tt("pool", SA[:, :, 7:NJ], SBb[:, :, 7:NJ], SBb[:, :, 3:NJ - 4], ALU.add, [B_SBb], [B_SA])
                            cur, B_cur = SA, B_SA
                        if w >= 16:
                            S.tt("pool", SBb[:, :, 15:NJ], SA[:, :, 15:NJ], SA[:, :, 7:NJ - 8], ALU.add, [B_SA], [B_SBb])
                            cur, B_cur = SBb, B_SBb
                        if first:
                            S.tt("pool", cur[:, :, 16:32], cur[:, :, 16:32],
                                 CORR[:, g, :].unsqueeze(1).to_broadcast([128, 2, 16]), ALU.mult,
                                 [B_cur, B_CORR], [B_cur])
                        S.stt(DG[:], cur[:, :, 16:16 + T], 1.0 / w, UG[:, :, 16:16 + T], ALU.mult, ALU.subtract,
                              [B_cur, B_UG], [B_DG])
                        for mo in range(2):
                            p_, pb_ = ps()
                            for kc in range(2):
                                S.mm(p_[:], PL[:, g, kc, mo * 128:(mo + 1) * 128], DG[:, kc, :], kc == 0, kc == 1,
                                     [B_PL, B_DG], [pb_])
                            S.stt(YPI[:, 2 * g + mo, :], p_[:], ppc(0, 2 * g + mo), SG[:, mo, :], ALU.mult, ALU.mult,
                                  [pb_, B_PP, B_SG], [B_YPI])
                    for m in range(8):
                        py_, pyb_ = ps()
                        w_, wb_ = wl(li, 90 + m)
                        for kc in range(8):
                            S.mm(py_[:], w_[:, kc * 128:(kc + 1) * 128], YPI[:, kc, :], kc == 0, kc == 7,
                                 [wb_, B_YPI], [pyb_])
                        pg_, pgb_ = inproj(li, 49 + m, False)
                        S.act(SGM[:], pg_[:], AF.Sigmoid, [pgb_], [B_SGM])
                        S.tt("dve", MP[:, m, :], py_[:], SGM[:], ALU.mult, [pyb_, B_SGM], [B_MP])

                    _chk("P")
                    p_, pb_ = inproj(li, 40, True)
                    S.act(TWA[0:64, :], p_[0:64, :], AF.Tanh, [pb_], [B_TWA])
                    S.copy("act", TWA[64:128, :], p_[64:128, :], [pb_], [B_TWA])
                    for c in range(8):
                        p_, pb_ = inproj(li, 32 + c, True)
                        S.copy("act", VF[:, c, :], p_[:], [pb_], [B_VF[c]])
                    if L == 0:
                        S.dma("act", vfd[vt_idx], VF[:].rearrange("p c t -> p (c t)"), B_VF, [B_vfd])
                    else:
                        for c in range(8):
                            S.copy("dve" if c % 2 == 0 else "act", VB[:, c, :], VF[:, c, :], [B_VF[c]], [B_VB[c]])
                        p1_, p1b_ = ps()
                        for kc in range(8):
                            S.mm(p1_[:], V1W[:, kc, :], VB[:, kc, :], kc == 0, kc == 7, [B_V1W, B_VB[kc]], [p1b_])
                        S.copy("act", P1[:], p1_[:], [p1b_], [B_P1])
                        for c in range(8):
                            p_, pb_ = ps()
                            S.mm(p_[:], V2W[:, c * 128:(c + 1) * 128], P1[:], True, True, [B_V2W, B_P1], [pb_])
                            mx, B_mx = tmp()
                            S.act(mx[:], p_[:], AF.Sigmoid, [pb_, B_PP], [B_mx], bias=ppc(3, c))
                            vf1, B_vf1 = tmp()
                            S.dma("sp", vf1[:], vfd[vt_idx, :, c * T:(c + 1) * T], [B_vfd], [B_vf1])
                            S.tt("pool", vf1[:], vf1[:], VF[:, c, :], ALU.subtract, [B_vf1, B_VF[c]], [B_vf1])
                            S.tt("pool", vf1[:], vf1[:], mx[:], ALU.mult, [B_vf1, B_mx], [B_vf1])
                            S.tt("dve", VF[:, c, :], VF[:, c, :], vf1[:], ALU.add, [B_VF[c], B_vf1], [B_VF[c]])
                    for c in range(8):
                        S.copy("dve" if c % 2 == 0 else "act", VB[:, c, :], VF[:, c, :], [B_VF[c]], [B_VB[c]])
                    for tc in range(NTC):
                        ptt, ptb = pt()
                        for c in range(8):
                            S.tr(ptt[:, c * 128:(c + 1) * 128], VB[:, c, tc * 128:(tc + 1) * 128], IDENT[:],
                                 [B_VB[c], B_IDENT], [ptb])
                        S.copy("act", VT[:, tc, :], ptt[:], [ptb], [B_VT[tc]])

                    _chk("R0")
                    for half in range(2):
                        for cl in range(4):
                            c = half * 4 + cl
                            pr_, prb_ = inproj(li, 16 + c, True, psL)
                            pk_, pkb_ = inproj(li, 24 + c, True, psL)
                            pd_, pdb_ = psS()
                            S.mm(pd_[:], DAZ[:, 0, c * 128:(c + 1) * 128], TWA[:], True, True, [B_DA, B_TWA], [pdb_])
                            sgd, B_sgd = tmp()
                            S.act(sgd[:], pd_[:], AF.Sigmoid, [pdb_, B_PP], [B_sgd], bias=ppc(1, c))
                            pa_, pab_ = psS()
                            S.mm(pa_[:], DAZ[:, 1, c * 128:(c + 1) * 128], TWA[:], True, True, [B_DA, B_TWA], [pab_])
                            ag, B_ag = tmp()
                            S.act(ag[:], pa_[:], AF.Sigmoid, [pab_, B_PP], [B_ag], bias=ppc(2, c))
                            cs, B_cs = tmp()
                            for tc in range(NTC):
                                S.op("dve", lambda e, cs=cs, sgd=sgd, tc=tc: e.tensor_tensor_scan(
                                    out=cs[:, tc * 128:(tc + 1) * 128], data0=ONESF[:], data1=sgd[:, tc * 128:(tc + 1) * 128],
                                    initial=0.0, op0=ALU.mult, op1=ALU.add), [B_ONESF, B_sgd], [B_cs])
                            csp, B_csp = tmp()
                            S.tt("pool", csp[:], cs[:], sgd[:], ALU.subtract, [B_cs, B_sgd], [B_csp])
                            ew, B_ew = tmp()
                            S.act(ew[:], cs[:], AF.Exp, [B_cs], [B_ew], scale=-DECAY_C)
                            ewi, B_ewi = tmp()
                            S.act(ewi[:], cs[:], AF.Exp, [B_cs], [B_ewi], scale=DECAY_C)
                            ewp, B_ewp = tmp()
                            S.act(ewp[:], csp[:], AF.Exp, [B_csp], [B_ewp], scale=-DECAY_C)
                            S.copy("pool", EWC[:, cl, :], ew[:].rearrange("p (tc j) -> p tc j", j=128)[:, :, 127],
                                   [B_ew], [B_EWC])
                            kkr, B_kkr = tmp()
                            S.ts("dve", kkr[:], pk_[:], ppc(4, c), ALU.mult, [pkb_, B_PP], [B_kkr])
                            S.tt("dve", RKB[:], kkr[:], kkr[:], ALU.mult, [B_kkr], [B_RKB])
                            pn_, pnb_ = psS()
                            S.mm(pn_[:], BONES[:], RKB[:], True, True, [B_BONES, B_RKB], [pnb_])
                            nrm, B_nrm = tmp()
                            S.act(nrm[:], pn_[:], AF.Sqrt, [pnb_], [B_nrm])
                            S.ts("dve", nrm[:], nrm[:], 1e-12, ALU.max, [B_nrm], [B_nrm])
                            S.op("dve", lambda e, nrm=nrm: e.reciprocal(out=nrm[:], in_=nrm[:]), [B_nrm], [B_nrm])
                            S.tt("pool", kkr[:], kkr[:], nrm[:], ALU.mult, [B_kkr, B_nrm], [B_kkr])
                            S.ts("pool", nrm[:], ag[:], ppc(5, c), ALU.mult, [B_ag, B_PP], [B_nrm], s2=ppc(7, c), op1=ALU.add)
                            kp, B_kp = csp, B_csp
                            S.tt("dve", kp[:], pk_[:], nrm[:], ALU.mult, [pkb_, B_nrm], [B_kp])
                            S.stt(RKB[:], pr_[:], ppc(6, c), kp[:], ALU.mult, ALU.mult, [prb_, B_PP, B_kp], [B_RKB])
                            for tc in range(NTC):
                                S.mm(PBD[:, tc * 16 + 2 * c: tc * 16 + 2 * c + 2], RKB[:, tc * 128:(tc + 1) * 128], E2[:],
                                     True, True, [B_RKB, B_E2], [B_PBD])
                            arv = AR[:, cl].rearrange("p tc (two j) -> p tc two j", two=2)
                            S.tt("dve", arv[:, :, 1, :], pr_[:].rearrange("p (tc j) -> p tc j", j=128),
                                 ew[:].rearrange("p (tc j) -> p tc j", j=128), ALU.mult, [prb_, B_ew], [B_AR[cl]])
                            S.stt(arv[:, :, 0, :], kkr[:].rearrange("p (tc j) -> p tc j", j=128), -1.0,
                                  ewp[:].rearrange("p (tc j) -> p tc j", j=128), ALU.mult, ALU.mult,
                                  [B_kkr, B_ewp], [B_AR[cl]])
                            S.tt("pool", ag[:], ag[:], kkr[:], ALU.mult, [B_ag, B_kkr], [B_ag])
                            for hh_ in range(2):
                                Pq = slice(hh_ * 64, (hh_ + 1) * 64)
                                S.tt("pool", BTZ[Pq, hh_, cl, :], ag[Pq, :], ewi[Pq, :], ALU.mult, [B_ag, B_ewi], [B_BT[cl]])
                                S.tt("pool", KTZ[Pq, hh_, cl, :], kp[Pq, :], ewi[Pq, :], ALU.mult, [B_kp, B_ewi], [B_KT[cl]])
                        for tc in range(NTC):
                            for (srcz, Bsrc, dst, Bdst) in ((BTZ, B_BT, BTT, B_BTT), (KTZ, B_KT, KTT, B_KTT)):
                                pz_, pzb_ = ps()
                                for cl in range(4):
                                    for hh_ in range(2):
                                        S.mm(pz_[:, cl * 128:(cl + 1) * 128], srcz[:, hh_, cl, tc * 128:(tc + 1) * 128], IDENT[:],
                                             hh_ == 0, hh_ == 1, [Bsrc[cl], B_IDENT], [pzb_])
                                S.copy("act", dst[:, tc, :], pz_[:], [pzb_], [Bdst[tc]])
                        S.copy("act", BDOT[:, :, half * 8:(half + 1) * 8],
                               PBD[:, 0:NTC * 16].rearrange("p (tc h) -> p tc h", h=16)[:, :, half * 8:(half + 1) * 8],
                               [B_PBD], [B_BDOT])
                        _chk("R1")
                        if first and True:
                            S.memset("pool", HS[:, half * 4:(half + 1) * 4, :], 0.0, B_HS[half * 2:half * 2 + 2])
                            S.memset("pool", HBZ[:, half * 4:(half + 1) * 4, :], 0.0, B_HB[half * 2:half * 2 + 2])
                        _chk("S00")
                        wg_, wgb_ = wr(li, half)

                        handoff(ALIAS_TMP, ALIAS_SCAN)

                        def build_unit(tc, q, half=half):
                            tcs = slice(tc * 128, (tc + 1) * 128)
                            slot = (tc % 2) * 2 + q
                            SCs, B_SCs = sc_slot(slot)
                            SKs, B_SKs = sk_slot(slot)
                            (Pk, BPk1), (Qk, BQk) = pq_slot(slot)
                            banks = (0, 1, 2) if q == 0 else (3, 4, 5)
                            bank = [0]

                            def pb():
                                r = PSB[banks[bank[0] % 3]]
                                bank[0] += 1
                                return r
                            pq_, pqb_ = pb()
                            for hq in range(4):
                                h = q * 4 + hq
                                cl, hh = h // 2, h % 2
                                S.mm(pq_[:, hq * 128:(hq + 1) * 128], AR[:, cl, tc, 0:128], BTZ[:, hh, cl, tcs], True, True,
                                     [B_AR[cl], B_BT[cl]], [pqb_])
                            S.tt("dve", Qk.rearrange("p (h j) -> p h j", j=128),
                                 pq_[:].rearrange("p (h j) -> p h j", j=128),
                                 MASKL[:].unsqueeze(1).to_broadcast([128, 4, 128]), ALU.mult,
                                 [pqb_, B_MASKL], [BQk])
                            for hq in range(4):
                                h = q * 4 + hq
                                cl, hh = h // 2, h % 2
                                psc_, pscb_ = pb()
                                S.mm(psc_[:, 0:256], BTZ[:, hh, cl, tcs], AR[:, cl, tc, :], True, True,
                                     [B_BT[cl], B_AR[cl]], [pscb_])
                                S.mm(psc_[:, 256:512], KTZ[:, hh, cl, tcs], AR[:, cl, tc, :], True, True,
                                     [B_KT[cl], B_AR[cl]], [pscb_])
                                S.tt("dve", SCs[:, hq, :], psc_[:], MASKT[:], ALU.mult, [pscb_, B_MASKT], [B_SCs[hq]])
                                if hq % 2 == 1:
                                    yield
                            S.tt("dve", SKs.rearrange("p (h j) -> p h j", j=128), SCs[:, :, 0:128],
                                 IDENT[:].unsqueeze(1).to_broadcast([128, 4, 128]), ALU.add, B_SCs + [B_IDENT], [B_SKs])
                            yield
                            for k in range(7):
                                def Pk_ap(hq, k=k):
                                    if k == 0:
                                        return SCs[:, hq, 0:128]
                                    return Pk[:, hq * 128:(hq + 1) * 128]
                                BPk = B_SCs if k == 0 else [BPk1]
                                if k <= 5:
                                    pQ_, pQb_ = pb()
                                    for hq in range(4):
                                        S.mm(pQ_[:, hq * 128:(hq + 1) * 128], Pk_ap(hq), Qk[:, hq * 128:(hq + 1) * 128],
                                             True, True, BPk + [BQk], [pQb_])
                                if k >= 1:
                                    pS_, pSb_ = pb()
                                    for hq in range(4):
                                        S.mm(pS_[:, hq * 128:(hq + 1) * 128], Qk[:, hq * 128:(hq + 1) * 128],
                                             SKs[:, hq * 128:(hq + 1) * 128], True, True, [BQk, B_SKs], [pSb_])
                                if k <= 4:
                                    pP_, pPb_ = pb()
                                    for hq in range(4):
                                        S.mm(pP_[:, hq * 128:(hq + 1) * 128], Qk[:, hq * 128:(hq + 1) * 128], Pk_ap(hq),
                                             True, True, BPk + [BQk], [pPb_])
                                if k >= 1:
                                    S.tt("dve", SKs, pS_[:], SKs, ALU.add, [pSb_, B_SKs], [B_SKs])
                                if k <= 5:
                                    S.copy("act", Qk, pQ_[:], [pQb_], [BQk])
                                if k <= 4:
                                    S.copy("act", Pk, pP_[:], [pPb_], [BPk1])
                                yield

                        def seq_unit(tc, q, half=half, wg_=wg_, wgb_=wgb_):
                            tcs = slice(tc * 128, (tc + 1) * 128)
                            slot = (tc % 2) * 2 + q
                            SCs, B_SCs = sc_slot(slot)
                            SKs, B_SKs = sk_slot(slot)
                            qc = slice(q * 256, (q + 1) * 256)
                            B_hs = B_HS[half * 2 + q]
                            B_hb = B_HB[half * 2 + q]
                            if q == 0:
                                pgt_, pgtb_ = PSB[6]
                                for kc in range(8):
                                    S.mm(pgt_[:], HT[:, kc, 1 + tc * 128: 1 + (tc + 1) * 128], wg_[:, kc * 512:(kc + 1) * 512],
                                         kc == 0, kc == 7, [B_HT, wgb_], [pgtb_])
                                S.act(SGT[:], pgt_[:], AF.Silu, [pgtb_], [B_SGT])
                                yield
                            px_, pxb_ = PSB[7]
                            for pl in range(2):
                                cl = q * 2 + pl
                                c = half * 4 + cl
                                S.mm(px_[:, pl * 128:(pl + 1) * 128], AR[:, cl, tc, 0:128], HBZ[:, c, :], True, False,
                                     [B_AR[cl], B_hb], [pxb_])
                                for hh in range(2):
                                    hl = pl * 2 + hh
                                    S.mm(px_[:, hl * 64:(hl + 1) * 64], SCs[:, hl, 256:384], VT[:, tc, (c * 2 + hh) * 64:(c * 2 + hh + 1) * 64],
                                         False, hh == 1, [B_SCs[hl], B_VT[tc]], [pxb_])
                            S.copy("act", XB[:, qc], px_[:, 0:256], [pxb_], [B_XB[q]])
                            yield
                            pu_, pub_ = PSB[6]
                            for hl in range(4):
                                S.mm(pu_[:, hl * 64:(hl + 1) * 64], SKs[:, hl * 128:(hl + 1) * 128],
                                     XB[:, q * 256 + hl * 64: q * 256 + (hl + 1) * 64], True, True, [B_SKs, B_XB[q]], [pub_])
                            S.copy("act", UB[:, qc], pu_[:, 0:256], [pub_], [B_UB[q]])
                            yield
                            py_, pyb_ = PSB[7]
                            for pl in range(2):
                                cl = q * 2 + pl
                                c = half * 4 + cl
                                S.mm(py_[:, pl * 128:(pl + 1) * 128], AR[:, cl, tc, 128:256], HBZ[:, c, :], True, False,
                                     [B_AR[cl], B_hb], [pyb_])
                                for hh in range(2):
                                    h = cl * 2 + hh
                                    hl = pl * 2 + hh
                                    vcol = slice((c * 2 + hh) * 64, (c * 2 + hh + 1) * 64)
                                    S.mm(py_[:, hl * 64:(hl + 1) * 64], SCs[:, hl, 128:256], UB[:, h * 64:(h + 1) * 64], False, False,
                                         [B_SCs[hl], B_UB[q]], [pyb_])
                                    S.mm(py_[:, hl * 64:(hl + 1) * 64], SCs[:, hl, 384:512], VT[:, tc, vcol], False, hh == 1,
                                         [B_SCs[hl], B_VT[tc]], [pyb_])
                            ph_, phb_ = PSB[6]
                            for pl in range(2):
                                cl = q * 2 + pl
                                c = half * 4 + cl
                                pc = slice(cl * 128, (cl + 1) * 128)
                                S.mm(ph_[:, pl * 128:(pl + 1) * 128], BTT[:, tc, pc], UB[:, pc], True, False, [B_BTT[tc], B_UB[q]], [phb_])
                                S.mm(ph_[:, pl * 128:(pl + 1) * 128], KTT[:, tc, pc], VT[:, tc, c * 128:(c + 1) * 128], False, True,
                                     [B_KTT[tc], B_VT[tc]], [phb_])
                            for hh in range(2):
                                Ph = slice(hh * 64, (hh + 1) * 64)
                                c0 = half * 4 + q * 2
                                hsv = HS[Ph, c0:c0 + 2, :]
                                phv = ph_[Ph, 0:256].rearrange("p (c x) -> p c x", x=128)[:, :, hh * 64:(hh + 1) * 64]
                                S.tt("dve", hsv, phv, hsv, ALU.add, [phb_, B_hs], [B_hs])
                                S.tt("dve", hsv, hsv, EWC[Ph, q * 2:q * 2 + 2, tc].unsqueeze(2).to_broadcast([64, 2, 64]), ALU.mult,
                                     [B_hs, B_EWC], [B_hs])
                                S.copy("pool", HBZ[Ph, c0:c0 + 2, hh * 64:(hh + 1) * 64], hsv, [B_hs], [B_hb])
                            yield
                            B_st = B_ST8[q]
                            hq4 = slice(q * 4, (q + 1) * 4)
                            y3 = py_[:, 0:256].rearrange("p (h v) -> p h v", v=64)
                            ya3 = YA[:, qc].rearrange("p (h v) -> p h v", v=64)
                            yb3 = YB[:, qc].rearrange("p (h v) -> p h v", v=64)
                            S.op("dve", lambda e, y3=y3: e.tensor_reduce(out=ST8[:, 0, hq4], in_=y3, axis=AX.X, op=ALU.add),
                                 [pyb_], [B_st])
                            S.ts("dve", ST8[:, 2, hq4], ST8[:, 0, hq4], 1.0 / 64, ALU.mult, [B_st], [B_st])
                            S.tt("dve", ya3, y3, ST8[:, 2, hq4].unsqueeze(2).to_broadcast([128, 4, 64]), ALU.subtract,
                                 [pyb_, B_st], [B_YA[q]])
                            yield
                            S.act(YB[:, qc], YA[:, qc], AF.Square, [B_YA[q]], [B_YB[q]])
                            S.op("dve", lambda e, yb3=yb3: e.tensor_reduce(out=ST8[:, 1, hq4], in_=yb3, axis=AX.X, op=ALU.add),
                                 [B_YB[q]], [B_st])
                            S.act(ST8[:, 5, hq4], ST8[:, 1, hq4], AF.Sqrt, [B_st, B_EPSC], [B_st], bias=EPSC[:, 1:2], scale=1.0 / 64)
                            S.op("dve", lambda e: e.reciprocal(out=ST8[:, 5, hq4], in_=ST8[:, 5, hq4]), [B_st], [B_st])
                            yield
                            S.tt("dve", ya3, ya3, ST8[:, 5, hq4].unsqueeze(2).to_broadcast([128, 4, 64]), ALU.mult,
                                 [B_YA[q], B_st], [B_YA[q]])
                            hc = slice(half * 512 + q * 256, half * 512 + (q + 1) * 256)
                            S.tt("pool", YA[:, qc], YA[:, qc], LNW[:, hc], ALU.mult, [B_YA[q], B_LNW], [B_YA[q]])
                            S.tt("pool", YA[:, qc], YA[:, qc], LNB[:, hc], ALU.add, [B_YA[q], B_LNB], [B_YA[q]])
                            S.tt("dve", yb3, VT[:, tc, hc].rearrange("p (h v) -> p h v", v=64),
                                 BDOT[:, tc, half * 8 + q * 4: half * 8 + (q + 1) * 4].unsqueeze(2).to_broadcast([128, 4, 64]), ALU.mult,
                                 [B_VT[tc], B_BDOT], [B_YB[q]])
                            yield
                            S.tt("pool", YA[:, qc], YA[:, qc], YB[:, qc], ALU.add, [B_YA[q], B_YB[q]], [B_YA[q]])
                            S.tt("pool", YG[:, qc], YA[:, qc], SGT[:, qc], ALU.mult, [B_YA[q], B_SGT], [B_YG[q]])
                            yield
                            ptt, ptb = PTB[0]
                            for pl in range(2):
                                S.tr(ptt[:, pl * 128:(pl + 1) * 128], YG[:, q * 256 + pl * 128: q * 256 + (pl + 1) * 128], IDENT[:],
                                     [B_YG[q], B_IDENT], [ptb])
                            c0 = half * 4 + q * 2
                            S.copy("act", YGT[:, c0:c0 + 2, tcs],
                                   ptt[:, 0:256].rearrange("p (c j) -> p c j", j=128), [ptb], [B_YGT])
                            yield

                        def chain(*gs):
                            for g_ in gs:
                                yield from g_

                        def interleave(gens):
                            gens = list(gens)
                            while gens:
                                for g_ in list(gens):
                                    try:
                                        next(g_)
                                    except StopIteration:
                                        gens.remove(g_)

                        interleave([build_unit(0, 0), build_unit(0, 1)])
                        for tc in range(NTC):
                            gens = [chain(seq_unit(tc, 0), seq_unit(tc, 1))]
                            if tc + 1 < NTC:
                                gens += [build_unit(tc + 1, 0), build_unit(tc + 1, 1)]
                            interleave(gens)
                        handoff(ALIAS_SCAN, ALIAS_TMP)

                    _chk("S")
                    for m in range(8):
                        py_, pyb_ = ps()
                        w_, wb_ = wl(li, 98 + m)
                        for kc in range(8):
                            S.mm(py_[:], w_[:, kc * 128:(kc + 1) * 128], YGT[:, kc, :], kc == 0, kc == 7,
                                 [wb_, B_YGT], [pyb_])
                        pg_, pgb_ = inproj(li, 57 + m, False)
                        S.act(SGM[:], pg_[:], AF.Sigmoid, [pgb_], [B_SGM])
                        t1, B_t1 = TMP[7]
                        S.tt("dve", t1[:], py_[:], SGM[:], ALU.mult, [pyb_, B_SGM], [B_t1])
                        S.tt("pool", MG[:, m, :], t1[:], MP[:, m, :], ALU.add, [B_t1, B_MP], [B_MG[m]])
                    _chk("O1")
                    wo = [wr(li, 2), wr(li, 3)]
                    for tg in range(4):
                        xi, B_xi = XIN[tg % 2]
                        rows = slice(row0 + tg * 128, row0 + (tg + 1) * 128)
                        S.dma("sp", xi[:], x_src[rows, :], Rxs, [B_xi])
                        for hf in range(2):
                            po_, pob_ = ps()
                            for kc in range(8):
                                S.mm(po_[:], MG[:, kc, tg * 128:(tg + 1) * 128], wo[hf][0][:, kc * 512:(kc + 1) * 512],
                                     kc == 0, kc == 7, [B_MG, wo[hf][1]], [pob_])
                            S.tt("dve", xi[:, hf * 512:(hf + 1) * 512], po_[:], xi[:, hf * 512:(hf + 1) * 512], ALU.add,
                                 [pob_, B_xi], [B_xi])
                        if last:
                            S.act(JUNK[:], xi[:], AF.Square, [B_xi], [B_JUNK, B_STAT], accum_out=STAT[:, 4:5])
                            S.act(STAT[:, 5:6], STAT[:, 4:5], AF.Sqrt, [B_STAT, B_EPSC], [B_STAT], bias=EPSC[:, 0:1], scale=1.0 / D)
                            S.op("dve", lambda e: e.reciprocal(out=STAT[:, 6:7], in_=STAT[:, 5:6]), [B_STAT], [B_STAT])
                            S.stt(xi[:], xi[:], STAT[:, 6:7], FG[:], ALU.mult, ALU.mult, [B_xi, B_STAT, B_FG], [B_xi])
                        _chk("O2")
                        S.dma("act", x_dst[rows, :], xi[:], [B_xi], Wxd)
                        _chk("O3")
                    _chk("T")


def _pack_pp(inp, L0, L1):
    nl = L1 - L0
    pp = np.zeros((nl, 128, NPAR, 8), np.float32)
    for li in range(nl):
        L = L0 + li
        vecs = [inp["pool_scale"][L], inp["decay_w0"][L], inp["a0"][L],
                inp["vres_v0"][L - 1] if L > 0 else None, inp["k_k"][L], inp["k_a"][L],
                np.asarray(inp["r_k"][L]).reshape(-1)]
        for k, v in enumerate(vecs):
            if v is None:
                continue
            pp[li, :, k, :] = np.asarray(v, np.float32).reshape(8, 128).T
    return pp.reshape(nl, 128, NPAR * 8)


def _layer_inputs(inp, L0, L1):
    f = lambda a: np.ascontiguousarray(np.asarray(a, np.float32))
    nl = L1 - L0
    v1 = np.zeros((nl, D, 32), np.float32)
    v2 = np.zeros((nl, 32, D), np.float32)
    for li in range(nl):
        L = L0 + li
        if L > 0:
            v1[li] = inp["vres_v1"][L - 1]
            v2[li] = inp["vres_v2"][L - 1]
    return {
        "w_in": f(inp["w_in"][L0:L1]), "pool_lin": f(inp["pool_lin"][L0:L1]),
        "w_pool_proj": f(inp["w_pool_proj"][L0:L1]), "mu_shift": f(inp["mu_shift"][L0:L1]),
        "decay_w2": f(inp["decay_w2"][L0:L1]), "a2": f(inp["a2"][L0:L1]),
        "vres_v1": v1, "vres_v2": v2,
        "w_rwkv_proj": f(inp["w_rwkv_proj"][L0:L1]), "w_out": f(inp["w_out"][L0:L1]),
        "norm_g": f(inp["norm_g"][L0:L1]), "lnx_w": f(inp["lnx_w"][L0:L1]), "lnx_b": f(inp["lnx_b"][L0:L1]),
        "final_g": f(inp["final_g"]).reshape(1, D), "pp": _pack_pp(inp, L0, L1),
    }


_PROG_CACHE = {}
STOP = None


class _Stop(Exception):
    pass


_CNT = [0]


_TILE = [0]


def _chk(name):
    if name == "T":
        _TILE[0] += 1
    if STOP == name or STOP == "%s@%d" % (name, _TILE[0]):
        raise _Stop()
    if name == "cnt" and STOP is not None and STOP.startswith("cnt"):
        _CNT[0] += 1
        if _CNT[0] >= int(STOP[3:]):
            raise _Stop()


def run_layers(inp, xs, L0, L1, NT, vfirst=None, cores=None):
    key = (L0, L1, NT)
    if key not in _PROG_CACHE:
        _PROG_CACHE[key] = build_program(L0, L1, NT, True)
    nc = _PROG_CACHE[key]
    shared = _layer_inputs(inp, L0, L1)
    in_maps = []
    for i, xc in enumerate(xs):
        m = dict(shared)
        m["x"] = np.ascontiguousarray(xc)
        if L0 > 0:
            m["vfirst_in"] = vfirst[i]
        in_maps.append(m)
    cores = list(range(len(xs))) if cores is None else cores
    import os
    if os.environ.get("KTRACE"):
        res = run_bass_kernel_spmd(nc, in_maps, core_ids=cores, trace=True)
        print("EXEC_TIME_NS", res.exec_time_ns, flush=True)
        try:
            print("TRACE", res.instructions_and_trace[1] if res.instructions_and_trace else None, flush=True)
        except Exception as ex:
            print("TRACE?", ex)
    else:
        res = run_bass_kernel_spmd(nc, in_maps, core_ids=cores)
    outs = [r["out"] for r in res.results]
    vf = [r["vfirst_out"] for r in res.results] if (L0 == 0 and L1 < DEPTH) else None
    return outs, vf


FUSED = True


def kernel(**inputs):
    x = np.asarray(inputs["x"], np.float32)
    xs = [np.ascontiguousarray(x[2 * i:2 * i + 2].reshape(NSEQ * SEQ, D)) for i in range(8)]
    if FUSED:
        outs, _ = run_layers(inputs, xs, 0, DEPTH, SEQ // T)
    else:
        outs, vf = run_layers(inputs, xs, 0, 1, SEQ // T)
        for L in range(1, DEPTH):
            outs, _ = run_layers(inputs, outs, L, L + 1, SEQ // T, vfirst=vf)
    return np.stack([o.reshape(NSEQ, SEQ, D) for o in outs], 0).reshape(16, SEQ, D).astype(np.float32)
```

```python
import numpy as np
from contextlib import ExitStack
import concourse.bass as bass
import concourse.mybir as mybir
from concourse.bass_utils import run_bass_kernel_spmd

F32 = mybir.dt.float32
BF16 = mybir.dt.bfloat16
AF = mybir.ActivationFunctionType
ALU = mybir.AluOpType
AX = mybir.AxisListType

D = 1024
SEQ = 2048
NSEQ = 2
DEPTH = 4
T = 512
NTC = 4
IN_W = 8320
NWL = 106
NORM_EPS = 1e-6
LNX_EPS = 1e-5 * 64
DECAY_C = 0.6065306597126334
POOL_WINDOWS = (2, 4, 8, 16)
NPAR = 8


class Buf:
    __slots__ = ("name", "lw", "rd", "const")

    def __init__(self, name, const=False):
        self.name = name
        self.lw = None
        self.rd = []
        self.const = const


class Sched:
    ENGS = ("pe", "act", "dve", "pool", "sp")

    def __init__(self, nc, stack, n_dsem=32, strict=("act", "dve", "pool")):
        self.nc = nc
        self.sem = {e: stack.enter_context(nc.semaphore("s_" + e)) for e in self.ENGS}
        self.dsem = [stack.enter_context(nc.semaphore("d%d" % i)) for i in range(n_dsem)]
        self.dval = [0] * n_dsem
        self.dnext = 0
        self.cnt = {e: 0 for e in self.ENGS}
        self.prog = {e: [] for e in self.ENGS}
        self.seen = {e: {} for e in self.ENGS}
        self.strict = set(strict)
        self.nwait = 0
        self.ninst = 0

    @staticmethod
    def _flat(x):
        out = []
        for b in x:
            if isinstance(b, (list, tuple)):
                out.extend(Sched._flat(b))
            elif b is not None:
                out.append(b)
        return out

    def _deps(self, eng, reads, writes):
        deps = set()
        for b in reads:
            if b.lw is not None:
                deps.add(b.lw)
        for b in writes:
            if b.lw is not None:
                deps.add(b.lw)
            for t in b.rd:
                deps.add(t)
        need = {}
        for (kind, ident, val) in deps:
            if kind == "e" and ident == eng and eng not in self.strict:
                continue
            key = (kind, ident)
            if self.seen[eng].get(key, 0) >= val:
                continue
            if need.get(key, 0) < val:
                need[key] = val
        for key, val in need.items():
            self.seen[eng][key] = val
            sem = self.sem[key[1]] if key[0] == "e" else self.dsem[key[1]]
            self.prog[eng].append(("w", sem, val))
            self.nwait += 1

    def _commit(self, tok, reads, writes):
        for b in writes:
            b.lw = tok
            b.rd = []
        for b in reads:
            if b.const or b in writes:
                continue
            b.rd.append(tok)
            if len(b.rd) > 400:
                b.rd = b.rd[-400:]

    def op(self, eng, fn, reads=(), writes=()):
        reads, writes = self._flat(reads), self._flat(writes)
        self._deps(eng, reads, writes)
        self.cnt[eng] += 1
        n = self.cnt[eng]
        self.prog[eng].append(("i", fn, self.sem[eng], 1))
        self._commit(("e", eng, n), reads, writes)
        self.ninst += 1

    def dma(self, q, out, in_, reads=(), writes=(), **kw):
        reads, writes = self._flat(reads), self._flat(writes)
        k = self.dnext
        self.dnext = (self.dnext + 1) % len(self.dsem)
        if self.dval[k] > 0 and self.seen[q].get(("d", k), 0) < self.dval[k]:
            self.seen[q][("d", k)] = self.dval[k]
            self.prog[q].append(("w", self.dsem[k], self.dval[k]))
        self._deps(q, reads, writes)
        self.dval[k] += 16
        v = self.dval[k]
        self.prog[q].append(("i", lambda e: e.dma_start(out=out, in_=in_, **kw), self.dsem[k], 16))
        self._commit(("d", k, v), reads, writes)
        self.ninst += 1

    def barrier(self):
        for e in self.ENGS:
            for p in self.ENGS:
                if p == e or self.cnt[p] == 0:
                    continue
                if self.seen[e].get(("e", p), 0) < self.cnt[p]:
                    self.seen[e][("e", p)] = self.cnt[p]
                    self.prog[e].append(("w", self.sem[p], self.cnt[p]))
            for k, v in enumerate(self.dval):
                if v > 0 and self.seen[e].get(("d", k), 0) < v:
                    self.seen[e][("d", k)] = v
                    self.prog[e].append(("w", self.dsem[k], v))

    def finish(self, q="sp"):
        for k, v in enumerate(self.dval):
            if v > 0:
                self.prog[q].append(("w", self.dsem[k], v))

    def run(self, block):
        def body(prog):
            def f(e):
                for it in prog:
                    if it[0] == "w":
                        e.wait_ge(it[1], it[2])
                    else:
                        it[1](e).then_inc(it[2], it[3])
            return f
        block.tensor(body(self.prog["pe"]))
        block.scalar(body(self.prog["act"]))
        block.vector(body(self.prog["dve"]))
        block.gpsimd(body(self.prog["pool"]))
        block.sync(body(self.prog["sp"]))

    def mm(self, out, lhsT, rhs, start, stop, R, W):
        self.op("pe", lambda e: e.matmul(out, lhsT, rhs, start=start, stop=stop), R, W)

    def tr(self, out, in_, ident, R, W):
        self.op("pe", lambda e: e.transpose(out, in_, ident), R, W)

    def act(self, out, in_, func, R, W, bias=None, scale=None, accum_out=None):
        kw = {}
        if bias is not None:
            kw["bias"] = bias
        if scale is not None:
            kw["scale"] = scale
        if accum_out is not None:
            kw["accum_out"] = accum_out
        self.op("act", lambda e: e.activation(out=out, in_=in_, func=func, **kw), R, W)

    def tt(self, eng, out, in0, in1, op, R, W):
        self.op(eng, lambda e: e.tensor_tensor(out=out, in0=in0, in1=in1, op=op), R, W)

    def ts(self, eng, out, in0, s1, op0, R, W, s2=None, op1=None):
        if op1 is None:
            self.op(eng, lambda e: e.tensor_scalar(out=out, in0=in0, scalar1=s1, scalar2=None, op0=op0), R, W)
        else:
            self.op(eng, lambda e: e.tensor_scalar(out=out, in0=in0, scalar1=s1, scalar2=s2, op0=op0, op1=op1), R, W)

    def stt(self, out, in0, scalar, in1, op0, op1, R, W):
        self.op("dve", lambda e: e.scalar_tensor_tensor(out=out, in0=in0, scalar=scalar, in1=in1, op0=op0, op1=op1), R, W)

    def copy(self, eng, out, in_, R, W):
        if eng == "act":
            self.op("act", lambda e: e.activation(out=out, in_=in_, func=AF.Copy), R, W)
        else:
            self.op(eng, lambda e: e.tensor_copy(out=out, in_=in_), R, W)

    def memset(self, eng, ap, val, W):
        self.op(eng, lambda e: e.memset(ap, val), (), W)


def build_program(L0, L1, NT, final_norm):
    NL = L1 - L0
    NTOK = NSEQ * SEQ
    nc = bass.Bass("TRN2", target_bir_lowering=False, dynamic_dma_scratch_size=2048)
    dt_in = lambda name, shape: nc.dram_tensor(name, shape, F32, kind="ExternalInput").ap()
    x_in = dt_in("x", [NTOK, D])
    w_in = dt_in("w_in", [NL, D, IN_W])
    pool_lin = dt_in("pool_lin", [NL, 4, 256, 256])
    w_pool_proj = dt_in("w_pool_proj", [NL, D, D])
    mu_shift = dt_in("mu_shift", [NL, 3200])
    decay_w2 = dt_in("decay_w2", [NL, 64, D])
    a2 = dt_in("a2", [NL, 64, D])
    vres_v1 = dt_in("vres_v1", [NL, D, 32])
    vres_v2 = dt_in("vres_v2", [NL, 32, D])
    w_rwkv_proj = dt_in("w_rwkv_proj", [NL, D, D])
    w_out = dt_in("w_out", [NL, D, D])
    norm_g = dt_in("norm_g", [NL, D])
    lnx_w = dt_in("lnx_w", [NL, D])
    lnx_b = dt_in("lnx_b", [NL, D])
    final_g = dt_in("final_g", [1, D])
    pp_in = dt_in("pp", [NL, 128, NPAR * 8])
    out_d = nc.dram_tensor("out", [NTOK, D], F32, kind="ExternalOutput").ap()
    NVT = NSEQ * NT
    if L0 == 0 and L1 < DEPTH:
        vfd = nc.dram_tensor("vfirst_out", [NVT, 128, 8 * T], F32, kind="ExternalOutput").ap()
    elif L0 == 0:
        vfd = nc.dram_tensor("vfirst_scr", [NVT, 128, 8 * T], F32, kind="Internal").ap()
    else:
        vfd = dt_in("vfirst_in", [NVT, 128, 8 * T])
    xbufs = [nc.dram_tensor("xbuf%d" % i, [NTOK, D], F32, kind="Internal").ap() for i in range(2)] if NL > 1 else []
    WLd = nc.dram_tensor("wl_scr", [NL, NWL, 128, 1024], BF16, kind="Internal").ap()
    WRd = nc.dram_tensor("wr_scr", [NL, 4, 128, 4096], BF16, kind="Internal").ap()
    B_vfd = Buf("vfd")
    B_xb = [Buf("xb0"), Buf("xb1")]
    B_WLd = [[Buf("wld") for _ in range(NWL)] for _ in range(NL)]
    B_WRd = [[Buf("wrd") for _ in range(4)] for _ in range(NL)]

    with ExitStack() as st:
        S = Sched(nc, st)

        def mk(stack, name, shape, dt, nb=0):
            t = stack.enter_context(nc.sbuf_tensor(name, shape, dt))
            if nb:
                return t, [Buf("%s%d" % (name, i)) for i in range(nb)]
            return t, Buf(name)

        with ExitStack() as pst:
            NPB = 4
            STG = [mk(pst, "stg%d" % i, [128, 8, 512], F32) for i in range(NPB)]
            CB = [mk(pst, "cb%d" % i, [128, 4, 1024], BF16) for i in range(NPB)]
            CB2 = [mk(pst, "cbp%d" % i, [128, 4, 1024], BF16) for i in range(NPB)]
            MU, B_MU = mk(pst, "mu_bc", [128, 3200], F32)
            MU1, B_MU1 = mk(pst, "mu1_bc", [128, 3200], F32)
            cast_engs = ["pool", "dve", "act"]
            ce = [0]

            def cast(out, in_, R, W):
                e = cast_engs[ce[0] % 3]
                ce[0] += 1
                S.copy(e, out, in_, R, W)

            blk = [0]
            for li in range(NL):
                S.dma("sp", MU[:], mu_shift[li:li + 1, :].to_broadcast([128, 3200]), (), [B_MU])
                S.ts("pool", MU1[:], MU[:], -1.0, ALU.mult, [B_MU], [B_MU1], s2=1.0, op1=ALU.add)
                segs = []
                for c0 in list(range(0, 5248, 512)):
                    segs.append((w_in[li], c0, min(512, 5248 - c0), "L", c0 // 128))
                for c0 in range(6272, 8320, 512):
                    segs.append((w_in[li], c0, 512, "L", c0 // 128))
                segs.append((w_in[li], 5248, 512, "R", 0))
                segs.append((w_in[li], 5760, 512, "R", 1))
                for c0 in (0, 512):
                    segs.append((w_pool_proj[li], c0, 512, "L", 90 + c0 // 128))
                for c0 in (0, 512):
                    segs.append((w_rwkv_proj[li], c0, 512, "L", 98 + c0 // 128))
                segs.append((w_out[li], 0, 512, "R", 2))
                segs.append((w_out[li], 512, 512, "R", 3))
                for (src, c0, ncol, kind, base) in segs:
                    b = blk[0] % NPB
                    blk[0] += 1
                    stg, B_stg = STG[b]
                    cb, B_cb = CB[b]
                    cb2, B_cb2 = CB2[b]
                    S.dma("sp", stg[:, :, 0:ncol], src[:, c0:c0 + ncol].rearrange("(kc kp) c -> kp kc c", kp=128),
                          (), [B_stg])
                    if kind == "R":
                        cbr = cb[:].rearrange("p a f -> p (a f)").rearrange("p (kc j) -> p kc j", j=512)
                        for kc in range(8):
                            cast(cbr[:, kc, :], stg[:, kc, :], [B_stg], [B_cb])
                        S.dma("act", WRd[li, base], cb[:].rearrange("p a f -> p (a f)"), [B_cb], [B_WRd[li][base]])
                        continue
                    nm = ncol // 128
                    for mi in range(nm):
                        m = base + mi
                        o = cb[:, mi, :].rearrange("p (kc j) -> p kc j", j=128)
                        i_ = stg[:, :, mi * 128:(mi + 1) * 128]
                        is_shift = (base < 90) and (16 <= m <= 40)
                        if is_shift:
                            mc = (m - 16) * 128
                            S.tt("pool" if mi % 2 == 0 else "dve", o, i_,
                                 MU1[:, mc:mc + 128].unsqueeze(1).to_broadcast([128, 8, 128]), ALU.mult,
                                 [B_stg, B_MU1], [B_cb])
                            o2 = cb2[:, mi, :].rearrange("p (kc j) -> p kc j", j=128)
                            S.tt("dve" if mi % 2 == 0 else "pool", o2, i_,
                                 MU[:, mc:mc + 128].unsqueeze(1).to_broadcast([128, 8, 128]), ALU.mult,
                                 [B_stg, B_MU], [B_cb2])
                            S.dma("act", WLd[li, 65 + m - 16], cb2[:, mi, :], [B_cb2], [B_WLd[li][65 + m - 16]])
                        else:
                            cast(o, i_, [B_stg], [B_cb])
                        S.dma("act", WLd[li, m], cb[:, mi, :], [B_cb], [B_WLd[li][m]])
        S.barrier()
        try:
            _chk("prepass")
            _main(locals())
        except _Stop:
            pass
        S.finish("sp")
        print("program: %d instructions, %d waits" % (S.ninst, S.nwait), {e: S.cnt[e] for e in S.ENGS}, flush=True)
        with nc.Block() as block:
            S.run(block)
    return nc


def _main(env):
    globals_ = env
    nc = env["nc"]; st = env["st"]; S = env["S"]; mk = env["mk"]
    NL = env["NL"]; L0 = env["L0"]; L1 = env["L1"]; NT = env["NT"]; final_norm = env["final_norm"]
    x_in = env["x_in"]; out_d = env["out_d"]; vfd = env["vfd"]; xbufs = env["xbufs"]
    WLd = env["WLd"]; WRd = env["WRd"]; B_vfd = env["B_vfd"]; B_xb = env["B_xb"]; B_WLd = env["B_WLd"]; B_WRd = env["B_WRd"]
    pool_lin = env["pool_lin"]; decay_w2 = env["decay_w2"]; a2 = env["a2"]; vres_v1 = env["vres_v1"]; vres_v2 = env["vres_v2"]
    norm_g = env["norm_g"]; lnx_w = env["lnx_w"]; lnx_b = env["lnx_b"]; final_g = env["final_g"]; pp_in = env["pp_in"]
    if True:
        IDENT, B_IDENT = mk(st, "ident", [128, 128], BF16)
        MASKT, B_MASKT = mk(st, "maskt", [128, 512], BF16)
        MASKL, B_MASKL = mk(st, "maskl", [128, 128], BF16)
        BONES, B_BONES = mk(st, "bones", [128, 128], BF16)
        E2, B_E2 = mk(st, "e2", [128, 2], BF16)
        ONESF, B_ONESF = mk(st, "onesf", [128, 128], F32)
        CORR, B_CORR = mk(st, "corr", [128, 4, 16], F32)
        EPSC, B_EPSC = mk(st, "epsc", [128, 2], F32)
        for b_ in (B_IDENT, B_MASKT, B_MASKL, B_BONES, B_E2, B_ONESF, B_CORR, B_EPSC):
            b_.const = True
        G_BC, B_G = mk(st, "g_bc", [128, D], F32)
        LNW, B_LNW = mk(st, "lnw_bc", [128, D], F32)
        LNB, B_LNB = mk(st, "lnb_bc", [128, D], F32)
        FG, B_FG = mk(st, "fg_bc", [128, D], F32)
        PP, B_PP = mk(st, "pp_sb", [128, NPAR, 8], F32)
        PL, B_PL = mk(st, "pl", [128, 4, 2, 256], BF16)
        DAZ, B_DA = mk(st, "daz", [128, 2, D], BF16)
        V1W, B_V1W = mk(st, "v1w", [128, 8, 128], BF16)
        V2W, B_V2W = mk(st, "v2w", [128, D], BF16)
        HT, B_HT = mk(st, "ht", [128, 8, T + 1], BF16)
        MP, B_MP = mk(st, "mp", [128, 8, T], BF16)
        YGT, B_YGT = mk(st, "ygt", [128, 8, T], BF16)
        YPI, B_YPI = YGT, B_YGT
        XIN = [mk(st, "xin%d" % i, [128, D], F32) for i in range(2)]
        HN, B_HN = mk(st, "hn", [128, D], BF16)
        JUNK, B_JUNK = HN, B_HN
        STAT, B_STAT = mk(st, "stat", [128, 8], F32)
        NRL = 6
        WLB = [mk(st, "wlb%d" % i, [128, 1024], BF16) for i in range(NRL)]
        WRB = [mk(st, "wrb%d" % i, [128, 4096], BF16) for i in range(2)]
        NTMP = 14
        TMPALL, B_TMP = mk(st, "tmpall", [128, NTMP, T + 16], F32, nb=NTMP)
        TMP = [(TMPALL[:, i, 0:T], B_TMP[i]) for i in range(NTMP)]
        UG, B_UG = TMPALL[:, 0:2, :], [B_TMP[0], B_TMP[1]]
        SA, B_SA = TMPALL[:, 2:4, :], [B_TMP[2], B_TMP[3]]
        SBb, B_SBb = TMPALL[:, 4:6, :], [B_TMP[4], B_TMP[5]]
        SG, B_SG = TMPALL[:, 6:8, 0:T], [B_TMP[6], B_TMP[7]]
        SGM, B_SGM = TMP[8]
        CTMP, B_CTMP = TMP[9]
        DG, B_DG = mk(st, "dg", [128, 2, T], BF16)
        HALO_P, B_HALOP = mk(st, "halop", [128, 8, 16], F32)
        VF, B_VF = mk(st, "vf", [128, 8, T], F32, nb=8)
        STGS, B_STGS = VF[:].rearrange("p c t -> p (c t)")[:, 0:2048], B_VF[0:4]
        VB, B_VB = mk(st, "vb", [128, 8, T], BF16, nb=8)
        VT, B_VT = mk(st, "vt", [128, NTC, D], BF16, nb=NTC)
        TWA, B_TWA = mk(st, "twa", [128, T], BF16)
        P1, B_P1 = mk(st, "p1", [128, T], BF16)
        BTZ, B_BT = mk(st, "btz", [128, 2, 4, T], BF16, nb=4)
        KTZ, B_KT = mk(st, "ktz", [128, 2, 4, T], BF16, nb=4)
        AR, B_AR = mk(st, "ar", [128, 4, NTC, 256], BF16, nb=4)
        BTT, B_BTT = mk(st, "btt", [128, NTC, 512], BF16, nb=NTC)
        KTT, B_KTT = mk(st, "ktt", [128, NTC, 512], BF16, nb=NTC)
        RKB2, B_RKB2 = mk(st, "rkb2", [128, 2, T], BF16, nb=2)
        EWC, B_EWC = mk(st, "ewc", [128, 4, NTC], F32)
        BDOT, B_BDOT = mk(st, "bdot", [128, NTC, 16], F32)
        SC, B_SC = mk(st, "sc", [128, 8, 512], BF16, nb=8)
        PK = [mk(st, "pk%d" % i, [128, 2, 512], BF16, nb=2) for i in range(2)]
        QK = [mk(st, "qk%d" % i, [128, 2, 512], BF16, nb=2) for i in range(2)]
        SK, B_SK = mk(st, "sk", [128, 2, 512], BF16, nb=2)
        SCXv = TMPALL[:, 3:7, :].rearrange("p a b -> p (a b)").bitcast(BF16)[:, 0:4096].rearrange("p (h f) -> p h f", f=512)
        B_SCX = [Buf("scx%d" % i) for i in range(8)]
        SKXv = TMPALL[:, 7, :].bitcast(BF16)[:, 0:1024].rearrange("p (q f) -> p q f", f=512)
        B_SKX = [Buf("skx%d" % i) for i in range(2)]
        ALIAS_TMP = [B_TMP[i] for i in range(3, 8)]
        ALIAS_SCAN = B_SCX + B_SKX

        def handoff(src, dst):
            toks = set()
            for b_ in src:
                if b_.lw is not None:
                    toks.add(b_.lw)
                toks.update(b_.rd)
            for d_ in dst:
                d_.rd = list(set(d_.rd) | toks)

        def sc_slot(slot):
            if slot < 2:
                return SC[:, slot * 4:(slot + 1) * 4, :], B_SC[slot * 4:(slot + 1) * 4]
            return SCXv[:, (slot - 2) * 4:(slot - 1) * 4, :], B_SCX[(slot - 2) * 4:(slot - 1) * 4]

        def sk_slot(slot):
            if slot < 2:
                return SK[:, slot, :], B_SK[slot]
            return SKXv[:, slot - 2, :], B_SKX[slot - 2]

        def pq_slot(slot):
            i_, j_ = slot // 2, slot % 2
            return (PK[i_][0][:, j_, :], PK[i_][1][j_]), (QK[i_][0][:, j_, :], QK[i_][1][j_])

        XB, B_XB = mk(st, "xbv", [128, 512], BF16, nb=2)
        UB, B_UB = mk(st, "ubv", [128, 512], BF16, nb=2)
        HS, B_HS = mk(st, "hs", [128, 8, 64], F32, nb=4)
        HBZ, B_HB = mk(st, "hbz", [128, 8, 128], BF16, nb=4)
        YA, B_YA = TMP[0][0], [TMP[0][1], Buf("ya1")]
        YB, B_YB = TMP[1][0], [TMP[1][1], Buf("yb1")]
        SGT, B_SGT = TMP[2]
        YG, B_YG = mk(st, "yg", [128, 512], BF16, nb=2)
        ST8, B_ST8 = mk(st, "st8", [128, 6, 8], F32, nb=2)
        MG, B_MG = VB, B_VB
        PSB = []
        for i in range(8):
            t_ = st.enter_context(nc.psum_tensor("ps%d" % i, [128, 512], F32))
            PSB.append((t_, Buf("ps%d" % i)))
        PTB = [(PSB[6][0][:].bitcast(BF16), PSB[6][1]), (PSB[7][0][:].bitcast(BF16), PSB[7][1])]
        ring = [0, 0, 0, 0, 0, 0]

        def ps():
            r = PSB[ring[0] % 5]
            ring[0] += 1
            return r

        PBD, B_PBD = PSB[5]

        def pt():
            r = PTB[ring[1] % 2]
            ring[1] += 1
            return r

        def psL():
            r = PSB[ring[4] % 4]
            ring[4] += 1
            return r

        def psS():
            r = PSB[(4, 6, 7)[ring[5] % 3]]
            ring[5] += 1
            return r

        def tmp():
            r = TMP[ring[3] % NTMP]
            ring[3] += 1
            return r

        def tri(dst_ap, cmp_op, pattern_step, cm):
            S.memset("pool", CTMP[:, 0:128], 1.0, [B_CTMP])
            S.op("pool", lambda e: e.affine_select(out=CTMP[:, 0:128], in_=CTMP[:, 0:128], pattern=[[pattern_step, 128]],
                                                   compare_op=cmp_op, fill=0.0, base=0, channel_multiplier=cm),
                 [B_CTMP], [B_CTMP])
            S.copy("pool", dst_ap, CTMP[:, 0:128], [B_CTMP], [B_IDENT])

        tri(IDENT[:], ALU.is_equal, -1, 1)
        tri(MASKL[:], ALU.is_gt, -1, 1)
        tri(MASKT[:, 0:128], ALU.is_gt, 1, -1)
        tri(MASKT[:, 128:256], ALU.is_ge, 1, -1)
        tri(MASKT[:, 256:384], ALU.is_gt, 1, -1)
        tri(MASKT[:, 384:512], ALU.is_ge, 1, -1)
        S.memset("pool", BONES[:], 0.0, [B_IDENT])
        S.memset("pool", BONES[0:64, 0:64], 1.0, [B_IDENT])
        S.memset("pool", BONES[64:128, 64:128], 1.0, [B_IDENT])
        S.memset("pool", E2[:], 0.0, [B_IDENT])
        S.memset("pool", E2[0:64, 0:1], 1.0, [B_IDENT])
        S.memset("pool", E2[64:128, 1:2], 1.0, [B_IDENT])
        S.memset("pool", ONESF[:], 1.0, [B_IDENT])
        S.memset("pool", CORR[:], 1.0, [B_IDENT])
        for g, w in enumerate(POOL_WINDOWS):
            for t_ in range(w - 1):
                S.memset("pool", CORR[:, g, t_:t_ + 1], float(w) / float(t_ + 1), [B_IDENT])
        S.memset("pool", BTZ[:], 0.0, B_BT)
        S.memset("pool", KTZ[:], 0.0, B_KT)
        S.memset("pool", HBZ[:], 0.0, B_HB)
        S.memset("pool", DAZ[:], 0.0, [B_DA])
        S.memset("pool", V1W[:], 0.0, [B_V1W])
        S.memset("pool", V2W[:], 0.0, [B_V2W])
        S.memset("pool", EPSC[:, 0:1], NORM_EPS, [B_IDENT])
        S.memset("pool", EPSC[:, 1:2], LNX_EPS, [B_IDENT])
        S.dma("sp", FG[:], final_g.to_broadcast([128, D]), (), [B_FG])
        _chk("consts")

        def wl(li, idx):
            t_, b_ = WLB[ring[2] % NRL]
            ring[2] += 1
            S.dma("sp", t_[:], WLd[li, idx], [B_WLd[li][idx]], [b_])
            return t_, b_

        wr_i = [0]

        def wr(li, idx):
            t_, b_ = WRB[wr_i[0] % 2]
            wr_i[0] += 1
            S.dma("sp", t_[:], WRd[li, idx], [B_WRd[li][idx]], [b_])
            return t_, b_

        def inproj(li, m, shift, alloc=None):
            pt_, pb_ = (alloc or ps)()
            w_, wb_ = wl(li, m)
            for kc in range(8):
                S.mm(pt_[:], w_[:, kc * 128:(kc + 1) * 128], HT[:, kc, 1:T + 1], kc == 0, (kc == 7 and not shift),
                     [wb_, B_HT], [pb_])
            if shift:
                w2, wb2 = wl(li, 65 + m - 16)
                for kc in range(8):
                    S.mm(pt_[:], w2[:, kc * 128:(kc + 1) * 128], HT[:, kc, 0:T], False, kc == 7,
                         [wb2, B_HT], [pb_])
            return pt_, pb_

        for li in range(NL):
            L = L0 + li
            last = (L == DEPTH - 1) and final_norm
            x_src, B_xs = (x_in, None) if li == 0 else (xbufs[(li - 1) % 2], B_xb[(li - 1) % 2])
            x_dst, B_xd = (out_d, None) if li == NL - 1 else (xbufs[li % 2], B_xb[li % 2])
            Rxs = [B_xs] if B_xs is not None else []
            Wxd = [B_xd] if B_xd is not None else []
            S.dma("sp", G_BC[:], norm_g[li:li + 1, :].to_broadcast([128, D]), (), [B_G])
            S.dma("sp", LNW[:], lnx_w[li:li + 1, :].to_broadcast([128, D]), (), [B_LNW])
            S.dma("sp", LNB[:], lnx_b[li:li + 1, :].to_broadcast([128, D]), (), [B_LNB])
            S.dma("sp", PP[:].rearrange("p k c -> p (k c)"), pp_in[li], (), [B_PP])
            S.ts("pool", PP[:, 7, :], PP[:, 5, :], -1.0, ALU.mult, [B_PP], [B_PP], s2=1.0, op1=ALU.add)
            S.dma("sp", STGS[:].rearrange("p (g kc d) -> p g kc d", g=4, kc=2),
                  pool_lin[li].rearrange("g (kc kp) d -> kp g kc d", kp=128), (), [B_STGS])
            S.copy("pool", PL[:].rearrange("p g kc d -> p (g kc d)"), STGS[:], [B_STGS], [B_PL])
            S.dma("sp", STGS[0:64, 0:D], decay_w2[li], [], [B_STGS])
            S.dma("sp", STGS[64:128, 0:D], a2[li], [], [B_STGS])
            S.copy("pool", DAZ[0:64, 0, :], STGS[0:64, 0:D], [B_STGS], [B_DA])
            S.copy("pool", DAZ[64:128, 1, :], STGS[64:128, 0:D], [B_STGS], [B_DA])
            if L > 0:
                S.dma("sp", STGS[:, 0:256].rearrange("p (kc j) -> p kc j", j=32),
                      vres_v1[li].rearrange("(kc kp) j -> kp kc j", kp=128), [], [B_STGS])
                S.copy("pool", V1W[:, :, 0:32], STGS[:, 0:256].rearrange("p (kc j) -> p kc j", j=32), [B_STGS], [B_V1W])
                S.dma("sp", STGS[0:32, 0:D], vres_v2[li], [], [B_STGS])
                S.copy("pool", V2W[0:32, :], STGS[0:32, 0:D], [B_STGS], [B_V2W])
            ppc = lambda k, c: PP[:, k, c:c + 1]
            _chk("params")

            for s in range(NSEQ):
                for ti in range(NT):
                    row0 = s * SEQ + ti * T
                    vt_idx = s * NT + ti
                    first = (ti == 0)
                    if first:
                        S.memset("pool", HT[:, :, 0:1], 0.0, [B_HT])
                    else:
                        S.copy("pool", HT[:, :, 0:1], HT[:, :, T:T + 1], [B_HT], [B_HT])
                    for tg in range(4):
                        xi, B_xi = XIN[tg % 2]
                        S.dma("sp", xi[:], x_src[row0 + tg * 128: row0 + (tg + 1) * 128, :], Rxs, [B_xi])
                        S.act(JUNK[:], xi[:], AF.Square, [B_xi], [B_JUNK, B_STAT], accum_out=STAT[:, 0:1])
                        S.act(STAT[:, 1:2], STAT[:, 0:1], AF.Sqrt, [B_STAT, B_EPSC], [B_STAT], bias=EPSC[:, 0:1], scale=1.0 / D)
                        S.op("dve", lambda e: e.reciprocal(out=STAT[:, 2:3], in_=STAT[:, 1:2]), [B_STAT], [B_STAT])
                        S.stt(HN[:], xi[:], STAT[:, 2:3], G_BC[:], ALU.mult, ALU.mult, [B_xi, B_STAT, B_G], [B_HN])
                        ptt, ptb = pt()
                        for kc in range(8):
                            S.tr(ptt[:, kc * 128:(kc + 1) * 128], HN[:, kc * 128:(kc + 1) * 128], IDENT[:],
                                 [B_HN, B_IDENT], [ptb])
                        S.copy("act", HT[:, :, 1 + tg * 128: 1 + (tg + 1) * 128],
                               ptt[:].rearrange("p (kc j) -> p kc j", j=128), [ptb], [B_HT])

                    _chk("N")
                    if first:
                        S.memset("pool", HALO_P[:], 0.0, [B_HALOP])
                    for g, w in enumerate(POOL_WINDOWS):
                        for j in range(2):
                            p_, pb_ = inproj(li, 2 * g + j, False)
                            S.copy("act", UG[:, j, 16:16 + T], p_[:], [pb_], [B_UG])
                        for j in range(2):
                            p_, pb_ = inproj(li, 8 + 2 * g + j, False)
                            S.act(SG[:, j, :], p_[:], AF.Silu, [pb_], [B_SG])
                        S.copy("pool", UG[:, :, 0:16], HALO_P[:, 2 * g:2 * g + 2, :], [B_HALOP], [B_UG])
                        S.copy("pool", HALO_P[:, 2 * g:2 * g + 2, :], UG[:, :, T:T + 16], [B_UG], [B_HALOP])
                        NJ = T + 16
                        S.tt("pool", SA[:, :, 1:NJ], UG[:, :, 1:NJ], UG[:, :, 0:NJ - 1], ALU.add, [B_UG], [B_SA])
                        cur, B_cur = SA, B_SA
                        if w >= 4:
                            S.tt("pool", SBb[:, :, 3:NJ], SA[:, :, 3:NJ], SA[:, :, 1:NJ - 2], ALU.add, [B_SA], [B_SBb])
                            cur, B_cur = SBb, B_SBb
                        if w >= 8:
                            S.tt("pool", SA[:, :, 7:NJ], SBb[:, :, 7:NJ], SBb[:, :, 3:NJ - 4], ALU.add, [B_SBb], [B_SA])
                            cur, B_cur = SA, B_SA
                        if w >= 16:
                            S.tt("pool", SBb[:, :, 15:NJ], SA[:, :, 15:NJ], SA[:, :, 7:NJ - 8], ALU.add, [B_SA], [B_SBb])
                            cur, B_cur = SBb, B_SBb
                        if first:
                            S.tt("pool", cur[:, :, 16:32], cur[:, :, 16:32],
                                 CORR[:, g, :].unsqueeze(1).to_broadcast([128, 2, 16]), ALU.mult,
                                 [B_cur, B_CORR], [B_cur])
                        S.stt(DG[:], cur[:, :, 16:16 + T], 1.0 / w, UG[:, :, 16:16 + T], ALU.mult, ALU.subtract,
                              [B_cur, B_UG], [B_DG])
                        for mo in range(2):
                            p_, pb_ = ps()
                            for kc in range(2):
                                S.mm(p_[:], PL[:, g, kc, mo * 128:(mo + 1) * 128], DG[:, kc, :], kc == 0, kc == 1,
                                     [B_PL, B_DG], [pb_])
                            S.stt(YPI[:, 2 * g + mo, :], p_[:], ppc(0, 2 * g + mo), SG[:, mo, :], ALU.mult, ALU.mult,
                                  [pb_, B_PP, B_SG], [B_YPI])
                    for m in range(8):
                        py_, pyb_ = ps()
                        w_, wb_ = wl(li, 90 + m)
                        for kc in range(8):
                            S.mm(py_[:], w_[:, kc * 128:(kc + 1) * 128], YPI[:, kc, :], kc == 0, kc == 7,
                                 [wb_, B_YPI], [pyb_])
                        pg_, pgb_ = inproj(li, 49 + m, False)
                        S.act(SGM[:], pg_[:], AF.Sigmoid, [pgb_], [B_SGM])
                        S.tt("dve", MP[:, m, :], py_[:], SGM[:], ALU.mult, [pyb_, B_SGM], [B_MP])

                    _chk("P")
                    p_, pb_ = inproj(li, 40, True)
                    S.act(TWA[0:64, :], p_[0:64, :], AF.Tanh, [pb_], [B_TWA])
                    S.copy("act", TWA[64:128, :], p_[64:128, :], [pb_], [B_TWA])
                    for c in range(8):
                        p_, pb_ = inproj(li, 32 + c, True)
                        S.copy("act", VF[:, c, :], p_[:], [pb_], [B_VF[c]])
                    if L == 0:
                        S.dma("act", vfd[vt_idx], VF[:].rearrange("p c t -> p (c t)"), B_VF, [B_vfd])
                    else:
                        for c in range(8):
                            S.copy("dve" if c % 2 == 0 else "act", VB[:, c, :], VF[:, c, :], [B_VF[c]], [B_VB[c]])
                        p1_, p1b_ = ps()
                        for kc in range(8):
                            S.mm(p1_[:], V1W[:, kc, :], VB[:, kc, :], kc == 0, kc == 7, [B_V1W, B_VB[kc]], [p1b_])
                        S.copy("act", P1[:], p1_[:], [p1b_], [B_P1])
                        for c in range(8):
                            p_, pb_ = ps()
                            S.mm(p_[:], V2W[:, c * 128:(c + 1) * 128], P1[:], True, True, [B_V2W, B_P1], [pb_])
                            mx, B_mx = tmp()
                            S.act(mx[:], p_[:], AF.Sigmoid, [pb_, B_PP], [B_mx], bias=ppc(3, c))
                            vf1, B_vf1 = tmp()
                            S.dma("sp", vf1[:], vfd[vt_idx, :, c * T:(c + 1) * T], [B_vfd], [B_vf1])
                            S.tt("pool", vf1[:], vf1[:], VF[:, c, :], ALU.subtract, [B_vf1, B_VF[c]], [B_vf1])
                            S.tt("pool", vf1[:], vf1[:], mx[:], ALU.mult, [B_vf1, B_mx], [B_vf1])
                            S.tt("dve", VF[:, c, :], VF[:, c, :], vf1[:], ALU.add, [B_VF[c], B_vf1], [B_VF[c]])
                    for c in range(8):
                        S.copy("dve" if c % 2 == 0 else "act", VB[:, c, :], VF[:, c, :], [B_VF[c]], [B_VB[c]])
                    for tc in range(NTC):
                        ptt, ptb = pt()
                        for c in range(8):
                            S.tr(ptt[:, c * 128:(c + 1) * 128], VB[:, c, tc * 128:(tc + 1) * 128], IDENT[:],
                                 [B_VB[c], B_IDENT], [ptb])
                        S.copy("act", VT[:, tc, :], ptt[:], [ptb], [B_VT[tc]])

                    _chk("R0")
                    for half in range(2):
                        def prep_c(cl, half=half):
                            c = half * 4 + cl
                            st_ = (cl % 2) * 7
                            sl = [TMP[st_ + i] for i in range(7)]
                            RK, B_RK = RKB2[:, cl % 2, :], B_RKB2[cl % 2]
                            pr_, prb_ = inproj(li, 16 + c, True, psL)
                            yield
                            pk_, pkb_ = inproj(li, 24 + c, True, psL)
                            yield
                            pd_, pdb_ = psS()
                            S.mm(pd_[:], DAZ[:, 0, c * 128:(c + 1) * 128], TWA[:], True, True, [B_DA, B_TWA], [pdb_])
                            sgd, B_sgd = sl[0]
                            S.act(sgd[:], pd_[:], AF.Sigmoid, [pdb_, B_PP], [B_sgd], bias=ppc(1, c))
                            pa_, pab_ = psS()
                            S.mm(pa_[:], DAZ[:, 1, c * 128:(c + 1) * 128], TWA[:], True, True, [B_DA, B_TWA], [pab_])
                            ag, B_ag = sl[1]
                            S.act(ag[:], pa_[:], AF.Sigmoid, [pab_, B_PP], [B_ag], bias=ppc(2, c))
                            kkr, B_kkr = sl[5]
                            S.ts("dve", kkr[:], pk_[:], ppc(4, c), ALU.mult, [pkb_, B_PP], [B_kkr])
                            S.tt("dve", RK, kkr[:], kkr[:], ALU.mult, [B_kkr], [B_RK])
                            pn_, pnb_ = psS()
                            S.mm(pn_[:], BONES[:], RK, True, True, [B_BONES, B_RK], [pnb_])
                            nrm, B_nrm = sl[6]
                            S.act(nrm[:], pn_[:], AF.Sqrt, [pnb_], [B_nrm])
                            yield
                            cs, B_cs = sl[2]
                            for tc in range(NTC):
                                S.op("dve", lambda e, cs=cs, sgd=sgd, tc=tc: e.tensor_tensor_scan(
                                    out=cs[:, tc * 128:(tc + 1) * 128], data0=ONESF[:], data1=sgd[:, tc * 128:(tc + 1) * 128],
                                    initial=0.0, op0=ALU.mult, op1=ALU.add), [B_ONESF, B_sgd], [B_cs])
                            csp, B_csp = sl[3]
                            S.tt("pool", csp[:], cs[:], sgd[:], ALU.subtract, [B_cs, B_sgd], [B_csp])
                            yield
                            S.ts("dve", nrm[:], nrm[:], 1e-12, ALU.max, [B_nrm], [B_nrm])
                            S.op("dve", lambda e, nrm=nrm: e.reciprocal(out=nrm[:], in_=nrm[:]), [B_nrm], [B_nrm])
                            S.tt("pool", kkr[:], kkr[:], nrm[:], ALU.mult, [B_kkr, B_nrm], [B_kkr])
                            S.ts("pool", nrm[:], ag[:], ppc(5, c), ALU.mult, [B_ag, B_PP], [B_nrm], s2=ppc(7, c), op1=ALU.add)
                            yield
                            ew, B_ew = sl[4]
                            S.act(ew[:], cs[:], AF.Exp, [B_cs], [B_ew], scale=-DECAY_C)
                            ewi, B_ewi = cs, B_cs
                            S.act(ewi[:], cs[:], AF.Exp, [B_cs], [B_ewi], scale=DECAY_C)
                            ewp, B_ewp = csp, B_csp
                            S.act(ewp[:], csp[:], AF.Exp, [B_csp], [B_ewp], scale=-DECAY_C)
                            S.copy("pool", EWC[:, cl, :], ew[:].rearrange("p (tc j) -> p tc j", j=128)[:, :, 127],
                                   [B_ew], [B_EWC])
                            yield
                            kp, B_kp = sgd, B_sgd
                            S.tt("dve", kp[:], pk_[:], nrm[:], ALU.mult, [pkb_, B_nrm], [B_kp])
                            S.stt(RK, pr_[:], ppc(6, c), kp[:], ALU.mult, ALU.mult, [prb_, B_PP, B_kp], [B_RK])
                            for tc in range(NTC):
                                S.mm(PBD[:, tc * 16 + 2 * c: tc * 16 + 2 * c + 2], RK[:, tc * 128:(tc + 1) * 128], E2[:],
                                     True, True, [B_RK, B_E2], [B_PBD])
                            yield
                            arv = AR[:, cl].rearrange("p tc (two j) -> p tc two j", two=2)
                            S.tt("dve", arv[:, :, 1, :], pr_[:].rearrange("p (tc j) -> p tc j", j=128),
                                 ew[:].rearrange("p (tc j) -> p tc j", j=128), ALU.mult, [prb_, B_ew], [B_AR[cl]])
                            S.stt(arv[:, :, 0, :], kkr[:].rearrange("p (tc j) -> p tc j", j=128), -1.0,
                                  ewp[:].rearrange("p (tc j) -> p tc j", j=128), ALU.mult, ALU.mult,
                                  [B_kkr, B_ewp], [B_AR[cl]])
                            yield
                            S.tt("pool", ag[:], ag[:], kkr[:], ALU.mult, [B_ag, B_kkr], [B_ag])
                            for hh_ in range(2):
                                Pq = slice(hh_ * 64, (hh_ + 1) * 64)
                                S.tt("pool", BTZ[Pq, hh_, cl, :], ag[Pq, :], ewi[Pq, :], ALU.mult, [B_ag, B_ewi], [B_BT[cl]])
                                S.tt("pool", KTZ[Pq, hh_, cl, :], kp[Pq, :], ewi[Pq, :], ALU.mult, [B_kp, B_ewi], [B_KT[cl]])
                            yield

                        def interleave2(gens):
                            gens = list(gens)
                            while gens:
                                for g_ in list(gens):
                                    try:
                                        next(g_)
                                    except StopIteration:
                                        gens.remove(g_)

                        interleave2([prep_c(0), prep_c(1)])
                        interleave2([prep_c(2), prep_c(3)])
                        for tc in range(NTC):
                            for (srcz, Bsrc, dst, Bdst) in ((BTZ, B_BT, BTT, B_BTT), (KTZ, B_KT, KTT, B_KTT)):
                                pz_, pzb_ = ps()
                                for cl in range(4):
                                    for hh_ in range(2):
                                        S.mm(pz_[:, cl * 128:(cl + 1) * 128], srcz[:, hh_, cl, tc * 128:(tc + 1) * 128], IDENT[:],
                                             hh_ == 0, hh_ == 1, [Bsrc[cl], B_IDENT], [pzb_])
                                S.copy("act", dst[:, tc, :], pz_[:], [pzb_], [Bdst[tc]])
                        S.copy("act", BDOT[:, :, half * 8:(half + 1) * 8],
                               PBD[:, 0:NTC * 16].rearrange("p (tc h) -> p tc h", h=16)[:, :, half * 8:(half + 1) * 8],
                               [B_PBD], [B_BDOT])
                        _chk("R1")
                        if first and True:
                            S.memset("pool", HS[:, half * 4:(half + 1) * 4, :], 0.0, B_HS[half * 2:half * 2 + 2])
                            S.memset("pool", HBZ[:, half * 4:(half + 1) * 4, :], 0.0, B_HB[half * 2:half * 2 + 2])
                        _chk("S00")
                        wg_, wgb_ = wr(li, half)

                        handoff(ALIAS_TMP, ALIAS_SCAN)

                        def build_unit(tc, q, half=half):
                            tcs = slice(tc * 128, (tc + 1) * 128)
                            slot = (tc % 2) * 2 + q
                            SCs, B_SCs = sc_slot(slot)
                            SKs, B_SKs = sk_slot(slot)
                            (Pk, BPk1), (Qk, BQk) = pq_slot(slot)
                            banks = (0, 1, 2) if q == 0 else (3, 4, 5)
                            bank = [0]

                            def pb():
                                r = PSB[banks[bank[0] % 3]]
                                bank[0] += 1
                                return r
                            pq_, pqb_ = pb()
                            for hq in range(4):
                                h = q * 4 + hq
                                cl, hh = h // 2, h % 2
                                S.mm(pq_[:, hq * 128:(hq + 1) * 128], AR[:, cl, tc, 0:128], BTZ[:, hh, cl, tcs], True, True,
                                     [B_AR[cl], B_BT[cl]], [pqb_])
                            S.tt("dve", Qk.rearrange("p (h j) -> p h j", j=128),
                                 pq_[:].rearrange("p (h j) -> p h j", j=128),
                                 MASKL[:].unsqueeze(1).to_broadcast([128, 4, 128]), ALU.mult,
                                 [pqb_, B_MASKL], [BQk])
                            for hq in range(4):
                                h = q * 4 + hq
                                cl, hh = h // 2, h % 2
                                psc_, pscb_ = pb()
                                S.mm(psc_[:, 0:256], BTZ[:, hh, cl, tcs], AR[:, cl, tc, :], True, True,
                                     [B_BT[cl], B_AR[cl]], [pscb_])
                                S.mm(psc_[:, 256:512], KTZ[:, hh, cl, tcs], AR[:, cl, tc, :], True, True,
                                     [B_KT[cl], B_AR[cl]], [pscb_])
                                S.tt("dve", SCs[:, hq, :], psc_[:], MASKT[:], ALU.mult, [pscb_, B_MASKT], [B_SCs[hq]])
                                if hq % 2 == 1:
                                    yield
                            S.tt("dve", SKs.rearrange("p (h j) -> p h j", j=128), SCs[:, :, 0:128],
                                 IDENT[:].unsqueeze(1).to_broadcast([128, 4, 128]), ALU.add, B_SCs + [B_IDENT], [B_SKs])
                            yield
                            for k in range(7):
                                def Pk_ap(hq, k=k):
                                    if k == 0:
                                        return SCs[:, hq, 0:128]
                                    return Pk[:, hq * 128:(hq + 1) * 128]
                                BPk = B_SCs if k == 0 else [BPk1]
                                if k <= 5:
                                    pQ_, pQb_ = pb()
                                    for hq in range(4):
                                        S.mm(pQ_[:, hq * 128:(hq + 1) * 128], Pk_ap(hq), Qk[:, hq * 128:(hq + 1) * 128],
                                             True, True, BPk + [BQk], [pQb_])
                                if k >= 1:
                                    pS_, pSb_ = pb()
                                    for hq in range(4):
                                        S.mm(pS_[:, hq * 128:(hq + 1) * 128], Qk[:, hq * 128:(hq + 1) * 128],
                                             SKs[:, hq * 128:(hq + 1) * 128], True, True, [BQk, B_SKs], [pSb_])
                                if k <= 4:
                                    pP_, pPb_ = pb()
                                    for hq in range(4):
                                        S.mm(pP_[:, hq * 128:(hq + 1) * 128], Qk[:, hq * 128:(hq + 1) * 128], Pk_ap(hq),
                                             True, True, BPk + [BQk], [pPb_])
                                if k >= 1:
                                    S.tt("dve", SKs, pS_[:], SKs, ALU.add, [pSb_, B_SKs], [B_SKs])
                                if k <= 5:
                                    S.copy("act", Qk, pQ_[:], [pQb_], [BQk])
                                if k <= 4:
                                    S.copy("act", Pk, pP_[:], [pPb_], [BPk1])
                                yield

                        def seq_unit(tc, q, half=half, wg_=wg_, wgb_=wgb_):
                            tcs = slice(tc * 128, (tc + 1) * 128)
                            slot = (tc % 2) * 2 + q
                            SCs, B_SCs = sc_slot(slot)
                            SKs, B_SKs = sk_slot(slot)
                            qc = slice(q * 256, (q + 1) * 256)
                            B_hs = B_HS[half * 2 + q]
                            B_hb = B_HB[half * 2 + q]
                            if q == 0:
                                pgt_, pgtb_ = PSB[6]
                                for kc in range(8):
                                    S.mm(pgt_[:], HT[:, kc, 1 + tc * 128: 1 + (tc + 1) * 128], wg_[:, kc * 512:(kc + 1) * 512],
                                         kc == 0, kc == 7, [B_HT, wgb_], [pgtb_])
                                S.act(SGT[:], pgt_[:], AF.Silu, [pgtb_], [B_SGT])
                                yield
                            px_, pxb_ = PSB[7]
                            for pl in range(2):
                                cl = q * 2 + pl
                                c = half * 4 + cl
                                S.mm(px_[:, pl * 128:(pl + 1) * 128], AR[:, cl, tc, 0:128], HBZ[:, c, :], True, False,
                                     [B_AR[cl], B_hb], [pxb_])
                                for hh in range(2):
                                    hl = pl * 2 + hh
                                    S.mm(px_[:, hl * 64:(hl + 1) * 64], SCs[:, hl, 256:384], VT[:, tc, (c * 2 + hh) * 64:(c * 2 + hh + 1) * 64],
                                         False, hh == 1, [B_SCs[hl], B_VT[tc]], [pxb_])
                            S.copy("act", XB[:, qc], px_[:, 0:256], [pxb_], [B_XB[q]])
                            yield
                            pu_, pub_ = PSB[6]
                            for hl in range(4):
                                S.mm(pu_[:, hl * 64:(hl + 1) * 64], SKs[:, hl * 128:(hl + 1) * 128],
                                     XB[:, q * 256 + hl * 64: q * 256 + (hl + 1) * 64], True, True, [B_SKs, B_XB[q]], [pub_])
                            S.copy("act", UB[:, qc], pu_[:, 0:256], [pub_], [B_UB[q]])
                            yield
                            py_, pyb_ = PSB[7]
                            for pl in range(2):
                                cl = q * 2 + pl
                                c = half * 4 + cl
                                S.mm(py_[:, pl * 128:(pl + 1) * 128], AR[:, cl, tc, 128:256], HBZ[:, c, :], True, False,
                                     [B_AR[cl], B_hb], [pyb_])
                                for hh in range(2):
                                    h = cl * 2 + hh
                                    hl = pl * 2 + hh
                                    vcol = slice((c * 2 + hh) * 64, (c * 2 + hh + 1) * 64)
                                    S.mm(py_[:, hl * 64:(hl + 1) * 64], SCs[:, hl, 128:256], UB[:, h * 64:(h + 1) * 64], False, False,
                                         [B_SCs[hl], B_UB[q]], [pyb_])
                                    S.mm(py_[:, hl * 64:(hl + 1) * 64], SCs[:, hl, 384:512], VT[:, tc, vcol], False, hh == 1,
                                         [B_SCs[hl], B_VT[tc]], [pyb_])
                            ph_, phb_ = PSB[6]
                            for pl in range(2):
                                cl = q * 2 + pl
                                c = half * 4 + cl
                                pc = slice(cl * 128, (cl + 1) * 128)
                                S.mm(ph_[:, pl * 128:(pl + 1) * 128], BTT[:, tc, pc], UB[:, pc], True, False, [B_BTT[tc], B_UB[q]], [phb_])
                                S.mm(ph_[:, pl * 128:(pl + 1) * 128], KTT[:, tc, pc], VT[:, tc, c * 128:(c + 1) * 128], False, True,
                                     [B_KTT[tc], B_VT[tc]], [phb_])
                            for hh in range(2):
                                Ph = slice(hh * 64, (hh + 1) * 64)
                                c0 = half * 4 + q * 2
                                hsv = HS[Ph, c0:c0 + 2, :]
                                phv = ph_[Ph, 0:256].rearrange("p (c x) -> p c x", x=128)[:, :, hh * 64:(hh + 1) * 64]
                                S.tt("dve", hsv, phv, hsv, ALU.add, [phb_, B_hs], [B_hs])
                                S.tt("dve", hsv, hsv, EWC[Ph, q * 2:q * 2 + 2, tc].unsqueeze(2).to_broadcast([64, 2, 64]), ALU.mult,
                                     [B_hs, B_EWC], [B_hs])
                                S.copy("pool", HBZ[Ph, c0:c0 + 2, hh * 64:(hh + 1) * 64], hsv, [B_hs], [B_hb])
                            yield
                            B_st = B_ST8[q]
                            hq4 = slice(q * 4, (q + 1) * 4)
                            y3 = py_[:, 0:256].rearrange("p (h v) -> p h v", v=64)
                            ya3 = YA[:, qc].rearrange("p (h v) -> p h v", v=64)
                            yb3 = YB[:, qc].rearrange("p (h v) -> p h v", v=64)
                            S.op("dve", lambda e, y3=y3: e.tensor_reduce(out=ST8[:, 0, hq4], in_=y3, axis=AX.X, op=ALU.add),
                                 [pyb_], [B_st])
                            S.ts("dve", ST8[:, 2, hq4], ST8[:, 0, hq4], 1.0 / 64, ALU.mult, [B_st], [B_st])
                            S.tt("dve", ya3, y3, ST8[:, 2, hq4].unsqueeze(2).to_broadcast([128, 4, 64]), ALU.subtract,
                                 [pyb_, B_st], [B_YA[q]])
                            yield
                            S.tt("pool", YB[:, qc], YA[:, qc], YA[:, qc], ALU.mult, [B_YA[q]], [B_YB[q]])
                            S.op("dve", lambda e, yb3=yb3: e.tensor_reduce(out=ST8[:, 1, hq4], in_=yb3, axis=AX.X, op=ALU.add),
                                 [B_YB[q]], [B_st])
                            S.act(ST8[:, 5, hq4], ST8[:, 1, hq4], AF.Sqrt, [B_st, B_EPSC], [B_st], bias=EPSC[:, 1:2], scale=1.0 / 64)
                            S.op("dve", lambda e: e.reciprocal(out=ST8[:, 5, hq4], in_=ST8[:, 5, hq4]), [B_st], [B_st])
                            yield
                            S.tt("dve", ya3, ya3, ST8[:, 5, hq4].unsqueeze(2).to_broadcast([128, 4, 64]), ALU.mult,
                                 [B_YA[q], B_st], [B_YA[q]])
                            hc = slice(half * 512 + q * 256, half * 512 + (q + 1) * 256)
                            S.tt("pool", YA[:, qc], YA[:, qc], LNW[:, hc], ALU.mult, [B_YA[q], B_LNW], [B_YA[q]])
                            S.tt("pool", YA[:, qc], YA[:, qc], LNB[:, hc], ALU.add, [B_YA[q], B_LNB], [B_YA[q]])
                            S.tt("dve", yb3, VT[:, tc, hc].rearrange("p (h v) -> p h v", v=64),
                                 BDOT[:, tc, half * 8 + q * 4: half * 8 + (q + 1) * 4].unsqueeze(2).to_broadcast([128, 4, 64]), ALU.mult,
                                 [B_VT[tc], B_BDOT], [B_YB[q]])
                            yield
                            S.tt("pool", YA[:, qc], YA[:, qc], YB[:, qc], ALU.add, [B_YA[q], B_YB[q]], [B_YA[q]])
                            S.tt("pool", YG[:, qc], YA[:, qc], SGT[:, qc], ALU.mult, [B_YA[q], B_SGT], [B_YG[q]])
                            yield
                            ptt, ptb = PTB[0]
                            for pl in range(2):
                                S.tr(ptt[:, pl * 128:(pl + 1) * 128], YG[:, q * 256 + pl * 128: q * 256 + (pl + 1) * 128], IDENT[:],
                                     [B_YG[q], B_IDENT], [ptb])
                            c0 = half * 4 + q * 2
                            S.copy("act", YGT[:, c0:c0 + 2, tcs],
                                   ptt[:, 0:256].rearrange("p (c j) -> p c j", j=128), [ptb], [B_YGT])
                            yield

                        def chain(*gs):
                            for g_ in gs:
                                yield from g_

                        def interleave(gens):
                            gens = list(gens)
                            while gens:
                                for g_ in list(gens):
                                    try:
                                        next(g_)
                                    except StopIteration:
                                        gens.remove(g_)

                        interleave([build_unit(0, 0), build_unit(0, 1)])
                        for tc in range(NTC):
                            gens = [chain(seq_unit(tc, 0), seq_unit(tc, 1))]
                            if tc + 1 < NTC:
                                gens += [build_unit(tc + 1, 0), build_unit(tc + 1, 1)]
                            interleave(gens)
                        handoff(ALIAS_SCAN, ALIAS_TMP)

                    _chk("S")
                    for m in range(8):
                        py_, pyb_ = ps()
                        w_, wb_ = wl(li, 98 + m)
                        for kc in range(8):
                            S.mm(py_[:], w_[:, kc * 128:(kc + 1) * 128], YGT[:, kc, :], kc == 0, kc == 7,
                                 [wb_, B_YGT], [pyb_])
                        pg_, pgb_ = inproj(li, 57 + m, False)
                        S.act(SGM[:], pg_[:], AF.Sigmoid, [pgb_], [B_SGM])
                        t1, B_t1 = TMP[7]
                        S.tt("dve", t1[:], py_[:], SGM[:], ALU.mult, [pyb_, B_SGM], [B_t1])
                        S.tt("pool", MG[:, m, :], t1[:], MP[:, m, :], ALU.add, [B_t1, B_MP], [B_MG[m]])
                    _chk("O1")
                    wo = [wr(li, 2), wr(li, 3)]
                    for tg in range(4):
                        xi, B_xi = XIN[tg % 2]
                        rows = slice(row0 + tg * 128, row0 + (tg + 1) * 128)
                        S.dma("sp", xi[:], x_src[rows, :], Rxs, [B_xi])
                        for hf in range(2):
                            po_, pob_ = ps()
                            for kc in range(8):
                                S.mm(po_[:], MG[:, kc, tg * 128:(tg + 1) * 128], wo[hf][0][:, kc * 512:(kc + 1) * 512],
                                     kc == 0, kc == 7, [B_MG, wo[hf][1]], [pob_])
                            S.tt("dve", xi[:, hf * 512:(hf + 1) * 512], po_[:], xi[:, hf * 512:(hf + 1) * 512], ALU.add,
                                 [pob_, B_xi], [B_xi])
                        if last:
                            S.act(JUNK[:], xi[:], AF.Square, [B_xi], [B_JUNK, B_STAT], accum_out=STAT[:, 4:5])
                            S.act(STAT[:, 5:6], STAT[:, 4:5], AF.Sqrt, [B_STAT, B_EPSC], [B_STAT], bias=EPSC[:, 0:1], scale=1.0 / D)
                            S.op("dve", lambda e: e.reciprocal(out=STAT[:, 6:7], in_=STAT[:, 5:6]), [B_STAT], [B_STAT])
                            S.stt(xi[:], xi[:], STAT[:, 6:7], FG[:], ALU.mult, ALU.mult, [B_xi, B_STAT, B_FG], [B_xi])
                        _chk("O2")
                        S.dma("act", x_dst[rows, :], xi[:], [B_xi], Wxd)
                        _chk("O3")
                    _chk("T")


def _pack_pp(inp, L0, L1):
    nl = L1 - L0
    pp = np.zeros((nl, 128, NPAR, 8), np.float32)
    for li in range(nl):
        L = L0 + li
        vecs = [inp["pool_scale"][L], inp["decay_w0"][L], inp["a0"][L],
                inp["vres_v0"][L - 1] if L > 0 else None, inp["k_k"][L], inp["k_a"][L],
                np.asarray(inp["r_k"][L]).reshape(-1)]
        for k, v in enumerate(vecs):
            if v is None:
                continue
            pp[li, :, k, :] = np.asarray(v, np.float32).reshape(8, 128).T
    return pp.reshape(nl, 128, NPAR * 8)


def _layer_inputs(inp, L0, L1):
    f = lambda a: np.ascontiguousarray(np.asarray(a, np.float32))
    nl = L1 - L0
    v1 = np.zeros((nl, D, 32), np.float32)
    v2 = np.zeros((nl, 32, D), np.float32)
    for li in range(nl):
        L = L0 + li
        if L > 0:
            v1[li] = inp["vres_v1"][L - 1]
            v2[li] = inp["vres_v2"][L - 1]
    return {
        "w_in": f(inp["w_in"][L0:L1]), "pool_lin": f(inp["pool_lin"][L0:L1]),
        "w_pool_proj": f(inp["w_pool_proj"][L0:L1]), "mu_shift": f(inp["mu_shift"][L0:L1]),
        "decay_w2": f(inp["decay_w2"][L0:L1]), "a2": f(inp["a2"][L0:L1]),
        "vres_v1": v1, "vres_v2": v2,
        "w_rwkv_proj": f(inp["w_rwkv_proj"][L0:L1]), "w_out": f(inp["w_out"][L0:L1]),
        "norm_g": f(inp["norm_g"][L0:L1]), "lnx_w": f(inp["lnx_w"][L0:L1]), "lnx_b": f(inp["lnx_b"][L0:L1]),
        "final_g": f(inp["final_g"]).reshape(1, D), "pp": _pack_pp(inp, L0, L1),
    }


_PROG_CACHE = {}
STOP = None


class _Stop(Exception):
    pass


_CNT = [0]


_TILE = [0]


def _chk(name):
    if name == "T":
        _TILE[0] += 1
    if STOP == name or STOP == "%s@%d" % (name, _TILE[0]):
        raise _Stop()
    if name == "cnt" and STOP is not None and STOP.startswith("cnt"):
        _CNT[0] += 1
        if _CNT[0] >= int(STOP[3:]):
            raise _Stop()


def run_layers(inp, xs, L0, L1, NT, vfirst=None, cores=None):
    key = (L0, L1, NT)
    if key not in _PROG_CACHE:
        _PROG_CACHE[key] = build_program(L0, L1, NT, True)
    nc = _PROG_CACHE[key]
    shared = _layer_inputs(inp, L0, L1)
    in_maps = []
    for i, xc in enumerate(xs):
        m = dict(shared)
        m["x"] = np.ascontiguousarray(xc)
        if L0 > 0:
            m["vfirst_in"] = vfirst[i]
        in_maps.append(m)
    cores = list(range(len(xs))) if cores is None else cores
    import os
    if os.environ.get("KTRACE"):
        res = run_bass_kernel_spmd(nc, in_maps, core_ids=cores, trace=True)
        print("EXEC_TIME_NS", res.exec_time_ns, flush=True)
        try:
            print("TRACE", res.instructions_and_trace[1] if res.instructions_and_trace else None, flush=True)
        except Exception as ex:
            print("TRACE?", ex)
    else:
        res = run_bass_kernel_spmd(nc, in_maps, core_ids=cores)
    outs = [r["out"] for r in res.results]
    vf = [r["vfirst_out"] for r in res.results] if (L0 == 0 and L1 < DEPTH) else None
    return outs, vf


FUSED = True


def kernel(**inputs):
    x = np.asarray(inputs["x"], np.float32)
    xs = [np.ascontiguousarray(x[2 * i:2 * i + 2].reshape(NSEQ * SEQ, D)) for i in range(8)]
    if FUSED:
        outs, _ = run_layers(inputs, xs, 0, DEPTH, SEQ // T)
    else:
        outs, vf = run_layers(inputs, xs, 0, 1, SEQ // T)
        for L in range(1, DEPTH):
            outs, _ = run_layers(inputs, outs, L, L + 1, SEQ // T, vfirst=vf)
    return np.stack([o.reshape(NSEQ, SEQ, D) for o in outs], 0).reshape(16, SEQ, D).astype(np.float32)
```

```python
import numpy as np
from contextlib import ExitStack
import concourse.bass as bass
import concourse.mybir as mybir
from concourse.bass_utils import run_bass_kernel_spmd

F32 = mybir.dt.float32
BF16 = mybir.dt.bfloat16
AF = mybir.ActivationFunctionType
ALU = mybir.AluOpType
AX = mybir.AxisListType

D = 1024
SEQ = 2048
NSEQ = 2
DEPTH = 4
T = 512
NTC = 4
IN_W = 8320
NWL = 106
NORM_EPS = 1e-6
LNX_EPS = 1e-5 * 64
DECAY_C = 0.6065306597126334
POOL_WINDOWS = (2, 4, 8, 16)
NPAR = 8


class Buf:
    __slots__ = ("name", "lw", "rd", "const")

    def __init__(self, name, const=False):
        self.name = name
        self.lw = None
        self.rd = []
        self.const = const


class Sched:
    ENGS = ("pe", "act", "dve", "pool", "sp")

    def __init__(self, nc, stack, n_dsem=32, strict=("act", "dve", "pool")):
        self.nc = nc
        self.sem = {e: stack.enter_context(nc.semaphore("s_" + e)) for e in self.ENGS}
        self.dsem = [stack.enter_context(nc.semaphore("d%d" % i)) for i in range(n_dsem)]
        self.dval = [0] * n_dsem
        self.dnext = 0
        self.cnt = {e: 0 for e in self.ENGS}
        self.prog = {e: [] for e in self.ENGS}
        self.seen = {e: {} for e in self.ENGS}
        self.strict = set(strict)
        self.nwait = 0
        self.ninst = 0

    @staticmethod
    def _flat(x):
        out = []
        for b in x:
            if isinstance(b, (list, tuple)):
                out.extend(Sched._flat(b))
            elif b is not None:
                out.append(b)
        return out

    def _deps(self, eng, reads, writes):
        deps = set()
        for b in reads:
            if b.lw is not None:
                deps.add(b.lw)
        for b in writes:
            if b.lw is not None:
                deps.add(b.lw)
            for t in b.rd:
                deps.add(t)
        need = {}
        for (kind, ident, val) in deps:
            if kind == "e" and ident == eng and eng not in self.strict:
                continue
            key = (kind, ident)
            if self.seen[eng].get(key, 0) >= val:
                continue
            if need.get(key, 0) < val:
                need[key] = val
        for key, val in need.items():
            self.seen[eng][key] = val
            sem = self.sem[key[1]] if key[0] == "e" else self.dsem[key[1]]
            self.prog[eng].append(("w", sem, val))
            self.nwait += 1

    def _commit(self, tok, reads, writes):
        for b in writes:
            b.lw = tok
            b.rd = []
        for b in reads:
            if b.const or b in writes:
                continue
            b.rd.append(tok)
            if len(b.rd) > 400:
                b.rd = b.rd[-400:]

    def op(self, eng, fn, reads=(), writes=()):
        reads, writes = self._flat(reads), self._flat(writes)
        self._deps(eng, reads, writes)
        self.cnt[eng] += 1
        n = self.cnt[eng]
        self.prog[eng].append(("i", fn, self.sem[eng], 1))
        self._commit(("e", eng, n), reads, writes)
        self.ninst += 1

    def dma(self, q, out, in_, reads=(), writes=(), **kw):
        reads, writes = self._flat(reads), self._flat(writes)
        k = self.dnext
        self.dnext = (self.dnext + 1) % len(self.dsem)
        if self.dval[k] > 0 and self.seen[q].get(("d", k), 0) < self.dval[k]:
            self.seen[q][("d", k)] = self.dval[k]
            self.prog[q].append(("w", self.dsem[k], self.dval[k]))
        self._deps(q, reads, writes)
        self.dval[k] += 16
        v = self.dval[k]
        self.prog[q].append(("i", lambda e: e.dma_start(out=out, in_=in_, **kw), self.dsem[k], 16))
        self._commit(("d", k, v), reads, writes)
        self.ninst += 1

    def barrier(self):
        for e in self.ENGS:
            for p in self.ENGS:
                if p == e or self.cnt[p] == 0:
                    continue
                if self.seen[e].get(("e", p), 0) < self.cnt[p]:
                    self.seen[e][("e", p)] = self.cnt[p]
                    self.prog[e].append(("w", self.sem[p], self.cnt[p]))
            for k, v in enumerate(self.dval):
                if v > 0 and self.seen[e].get(("d", k), 0) < v:
                    self.seen[e][("d", k)] = v
                    self.prog[e].append(("w", self.dsem[k], v))

    def finish(self, q="sp"):
        for k, v in enumerate(self.dval):
            if v > 0:
                self.prog[q].append(("w", self.dsem[k], v))

    def run(self, block):
        def body(prog):
            def f(e):
                for it in prog:
                    if it[0] == "w":
                        e.wait_ge(it[1], it[2])
                    else:
                        it[1](e).then_inc(it[2], it[3])
            return f
        block.tensor(body(self.prog["pe"]))
        block.scalar(body(self.prog["act"]))
        block.vector(body(self.prog["dve"]))
        block.gpsimd(body(self.prog["pool"]))
        block.sync(body(self.prog["sp"]))

    def mm(self, out, lhsT, rhs, start, stop, R, W):
        self.op("pe", lambda e: e.matmul(out, lhsT, rhs, start=start, stop=stop), R, W)

    def tr(self, out, in_, ident, R, W):
        self.op("pe", lambda e: e.transpose(out, in_, ident), R, W)

    def act(self, out, in_, func, R, W, bias=None, scale=None, accum_out=None):
        kw = {}
        if bias is not None:
            kw["bias"] = bias
        if scale is not None:
            kw["scale"] = scale
        if accum_out is not None:
            kw["accum_out"] = accum_out
        self.op("act", lambda e: e.activation(out=out, in_=in_, func=func, **kw), R, W)

    def tt(self, eng, out, in0, in1, op, R, W):
        self.op(eng, lambda e: e.tensor_tensor(out=out, in0=in0, in1=in1, op=op), R, W)

    def ts(self, eng, out, in0, s1, op0, R, W, s2=None, op1=None):
        if op1 is None:
            self.op(eng, lambda e: e.tensor_scalar(out=out, in0=in0, scalar1=s1, scalar2=None, op0=op0), R, W)
        else:
            self.op(eng, lambda e: e.tensor_scalar(out=out, in0=in0, scalar1=s1, scalar2=s2, op0=op0, op1=op1), R, W)

    def stt(self, out, in0, scalar, in1, op0, op1, R, W):
        self.op("dve", lambda e: e.scalar_tensor_tensor(out=out, in0=in0, scalar=scalar, in1=in1, op0=op0, op1=op1), R, W)

    def copy(self, eng, out, in_, R, W):
        if eng == "act":
            self.op("act", lambda e: e.activation(out=out, in_=in_, func=AF.Copy), R, W)
        else:
            self.op(eng, lambda e: e.tensor_copy(out=out, in_=in_), R, W)

    def memset(self, eng, ap, val, W):
        self.op(eng, lambda e: e.memset(ap, val), (), W)


def build_program(L0, L1, NT, final_norm):
    NL = L1 - L0
    NTOK = NSEQ * SEQ
    nc = bass.Bass("TRN2", target_bir_lowering=False, dynamic_dma_scratch_size=2048)
    dt_in = lambda name, shape: nc.dram_tensor(name, shape, F32, kind="ExternalInput").ap()
    x_in = dt_in("x", [NTOK, D])
    w_in = dt_in("w_in", [NL, D, IN_W])
    pool_lin = dt_in("pool_lin", [NL, 4, 256, 256])
    w_pool_proj = dt_in("w_pool_proj", [NL, D, D])
    mu_shift = dt_in("mu_shift", [NL, 3200])
    decay_w2 = dt_in("decay_w2", [NL, 64, D])
    a2 = dt_in("a2", [NL, 64, D])
    vres_v1 = dt_in("vres_v1", [NL, D, 32])
    vres_v2 = dt_in("vres_v2", [NL, 32, D])
    w_rwkv_proj = dt_in("w_rwkv_proj", [NL, D, D])
    w_out = dt_in("w_out", [NL, D, D])
    norm_g = dt_in("norm_g", [NL, D])
    lnx_w = dt_in("lnx_w", [NL, D])
    lnx_b = dt_in("lnx_b", [NL, D])
    final_g = dt_in("final_g", [1, D])
    pp_in = dt_in("pp", [NL, 128, NPAR * 8])
    out_d = nc.dram_tensor("out", [NTOK, D], F32, kind="ExternalOutput").ap()
    NVT = NSEQ * NT
    if L0 == 0 and L1 < DEPTH:
        vfd = nc.dram_tensor("vfirst_out", [NVT, 128, 8 * T], F32, kind="ExternalOutput").ap()
    elif L0 == 0:
        vfd = nc.dram_tensor("vfirst_scr", [NVT, 128, 8 * T], F32, kind="Internal").ap()
    else:
        vfd = dt_in("vfirst_in", [NVT, 128, 8 * T])
    xbufs = [nc.dram_tensor("xbuf%d" % i, [NTOK, D], F32, kind="Internal").ap() for i in range(2)] if NL > 1 else []
    WLd = nc.dram_tensor("wl_scr", [NL, NWL, 128, 1024], BF16, kind="Internal").ap()
    WRd = nc.dram_tensor("wr_scr", [NL, 4, 128, 4096], BF16, kind="Internal").ap()
    B_vfd = Buf("vfd")
    B_xb = [Buf("xb0"), Buf("xb1")]
    B_WLd = [[Buf("wld") for _ in range(NWL)] for _ in range(NL)]
    B_WRd = [[Buf("wrd") for _ in range(4)] for _ in range(NL)]

    with ExitStack() as st:
        S = Sched(nc, st)

        def mk(stack, name, shape, dt, nb=0):
            t = stack.enter_context(nc.sbuf_tensor(name, shape, dt))
            if nb:
                return t, [Buf("%s%d" % (name, i)) for i in range(nb)]
            return t, Buf(name)

        with ExitStack() as pst:
            NPB = 4
            STG = [mk(pst, "stg%d" % i, [128, 8, 512], F32) for i in range(NPB)]
            CB = [mk(pst, "cb%d" % i, [128, 4, 1024], BF16) for i in range(NPB)]
            CB2 = [mk(pst, "cbp%d" % i, [128, 4, 1024], BF16) for i in range(NPB)]
            MU, B_MU = mk(pst, "mu_bc", [128, 3200], F32)
            MU1, B_MU1 = mk(pst, "mu1_bc", [128, 3200], F32)
            cast_engs = ["pool", "dve", "act"]
            ce = [0]

            def cast(out, in_, R, W):
                e = cast_engs[ce[0] % 3]
                ce[0] += 1
                S.copy(e, out, in_, R, W)

            blk = [0]
            for li in range(NL):
                S.dma("sp", MU[:], mu_shift[li:li + 1, :].to_broadcast([128, 3200]), (), [B_MU])
                S.ts("pool", MU1[:], MU[:], -1.0, ALU.mult, [B_MU], [B_MU1], s2=1.0, op1=ALU.add)
                segs = []
                for c0 in list(range(0, 5248, 512)):
                    segs.append((w_in[li], c0, min(512, 5248 - c0), "L", c0 // 128))
                for c0 in range(6272, 8320, 512):
                    segs.append((w_in[li], c0, 512, "L", c0 // 128))
                segs.append((w_in[li], 5248, 512, "R", 0))
                segs.append((w_in[li], 5760, 512, "R", 1))
                for c0 in (0, 512):
                    segs.append((w_pool_proj[li], c0, 512, "L", 90 + c0 // 128))
                for c0 in (0, 512):
                    segs.append((w_rwkv_proj[li], c0, 512, "L", 98 + c0 // 128))
                segs.append((w_out[li], 0, 512, "R", 2))
                segs.append((w_out[li], 512, 512, "R", 3))
                for (src, c0, ncol, kind, base) in segs:
                    b = blk[0] % NPB
                    blk[0] += 1
                    stg, B_stg = STG[b]
                    cb, B_cb = CB[b]
                    cb2, B_cb2 = CB2[b]
                    S.dma("sp", stg[:, :, 0:ncol], src[:, c0:c0 + ncol].rearrange("(kc kp) c -> kp kc c", kp=128),
                          (), [B_stg])
                    if kind == "R":
                        cbr = cb[:].rearrange("p a f -> p (a f)").rearrange("p (kc j) -> p kc j", j=512)
                        for kc in range(8):
                            cast(cbr[:, kc, :], stg[:, kc, :], [B_stg], [B_cb])
                        S.dma("act", WRd[li, base], cb[:].rearrange("p a f -> p (a f)"), [B_cb], [B_WRd[li][base]])
                        continue
                    nm = ncol // 128
                    for mi in range(nm):
                        m = base + mi
                        o = cb[:, mi, :].rearrange("p (kc j) -> p kc j", j=128)
                        i_ = stg[:, :, mi * 128:(mi + 1) * 128]
                        is_shift = (base < 90) and (16 <= m <= 40)
                        if is_shift:
                            mc = (m - 16) * 128
                            S.tt("pool" if mi % 2 == 0 else "dve", o, i_,
                                 MU1[:, mc:mc + 128].unsqueeze(1).to_broadcast([128, 8, 128]), ALU.mult,
                                 [B_stg, B_MU1], [B_cb])
                            o2 = cb2[:, mi, :].rearrange("p (kc j) -> p kc j", j=128)
                            S.tt("dve" if mi % 2 == 0 else "pool", o2, i_,
                                 MU[:, mc:mc + 128].unsqueeze(1).to_broadcast([128, 8, 128]), ALU.mult,
                                 [B_stg, B_MU], [B_cb2])
                            S.dma("act", WLd[li, 65 + m - 16], cb2[:, mi, :], [B_cb2], [B_WLd[li][65 + m - 16]])
                        else:
                            cast(o, i_, [B_stg], [B_cb])
                        S.dma("act", WLd[li, m], cb[:, mi, :], [B_cb], [B_WLd[li][m]])
        S.barrier()
        try:
            _chk("prepass")
            _main(locals())
        except _Stop:
            pass
        S.finish("sp")
        print("program: %d instructions, %d waits" % (S.ninst, S.nwait), {e: S.cnt[e] for e in S.ENGS}, flush=True)
        with nc.Block() as block:
            S.run(block)
    return nc


def _main(env):
    globals_ = env
    nc = env["nc"]; st = env["st"]; S = env["S"]; mk = env["mk"]
    NL = env["NL"]; L0 = env["L0"]; L1 = env["L1"]; NT = env["NT"]; final_norm = env["final_norm"]
    x_in = env["x_in"]; out_d = env["out_d"]; vfd = env["vfd"]; xbufs = env["xbufs"]
    WLd = env["WLd"]; WRd = env["WRd"]; B_vfd = env["B_vfd"]; B_xb = env["B_xb"]; B_WLd = env["B_WLd"]; B_WRd = env["B_WRd"]
    pool_lin = env["pool_lin"]; decay_w2 = env["decay_w2"]; a2 = env["a2"]; vres_v1 = env["vres_v1"]; vres_v2 = env["vres_v2"]
    norm_g = env["norm_g"]; lnx_w = env["lnx_w"]; lnx_b = env["lnx_b"]; final_g = env["final_g"]; pp_in = env["pp_in"]
    if True:
        IDENT, B_IDENT = mk(st, "ident", [128, 128], BF16)
        MASKT, B_MASKT = mk(st, "maskt", [128, 512], BF16)
        MASKL, B_MASKL = mk(st, "maskl", [128, 128], BF16)
        BONES, B_BONES = mk(st, "bones", [128, 128], BF16)
        E2, B_E2 = mk(st, "e2", [128, 2], BF16)
        ONESF, B_ONESF = mk(st, "onesf", [128, 128], F32)
        CORR, B_CORR = mk(st, "corr", [128, 4, 16], F32)
        EPSC, B_EPSC = mk(st, "epsc", [128, 2], F32)
        for b_ in (B_IDENT, B_MASKT, B_MASKL, B_BONES, B_E2, B_ONESF, B_CORR, B_EPSC):
            b_.const = True
        G_BC, B_G = mk(st, "g_bc", [128, D], F32)
        LNW, B_LNW = mk(st, "lnw_bc", [128, D], F32)
        LNB, B_LNB = mk(st, "lnb_bc", [128, D], F32)
        FG, B_FG = mk(st, "fg_bc", [128, D], F32)
        PP, B_PP = mk(st, "pp_sb", [128, NPAR, 8], F32)
        PL, B_PL = mk(st, "pl", [128, 4, 2, 256], BF16)
        DAZ, B_DA = mk(st, "daz", [128, 2, D], BF16)
        V1W, B_V1W = mk(st, "v1w", [128, 8, 128], BF16)
        V2W, B_V2W = mk(st, "v2w", [128, D], BF16)
        HT, B_HT = mk(st, "ht", [128, 8, T + 1], BF16)
        MP, B_MP = mk(st, "mp", [128, 8, T], BF16)
        YGT, B_YGT = mk(st, "ygt", [128, 8, T], BF16)
        XIN = [mk(st, "xin%d" % i, [128, D], F32) for i in range(2)]
        HN, B_HN = mk(st, "hn", [128, D], BF16)
        JUNK, B_JUNK = HN, B_HN
        STAT, B_STAT = mk(st, "stat", [128, 8], F32)
        NRL = 6
        WLB = [mk(st, "wlb%d" % i, [128, 1024], BF16) for i in range(NRL)]
        WRB = [mk(st, "wrb%d" % i, [128, 4096], BF16) for i in range(2)]
        NTMP = 14
        TMPALL, B_TMP = mk(st, "tmpall", [128, NTMP, T + 16], F32, nb=NTMP)
        TMP = [(TMPALL[:, i, 0:T], B_TMP[i]) for i in range(NTMP)]
        SG, B_SG = TMPALL[:, 8:10, 0:T], [B_TMP[8], B_TMP[9]]
        SGM, B_SGM = TMP[10]
        CTMP, B_CTMP = TMP[9]
        DG, B_DG = mk(st, "dg", [128, 2, T], BF16)
        HALO_P, B_HALOP = mk(st, "halop", [128, 8, 16], F32)
        VF, B_VF = mk(st, "vf", [128, 8, T], F32, nb=8)
        STGS, B_STGS = VF[:].rearrange("p c t -> p (c t)")[:, 0:2048], B_VF[0:4]
        VFf = VF[:].rearrange("p c t -> p (c t)")
        UG, B_UG = VFf[:, 0:1056].rearrange("p (a b) -> p a b", a=2), Buf("ug")
        SA, B_SA = VFf[:, 1056:2112].rearrange("p (a b) -> p a b", a=2), Buf("sa")
        SBb, B_SBb = VFf[:, 2112:3168].rearrange("p (a b) -> p a b", a=2), Buf("sbb")
        VB, B_VB = mk(st, "vb", [128, 8, T], BF16, nb=8)
        VT, B_VT = mk(st, "vt", [128, NTC, D], BF16, nb=NTC)
        TWA, B_TWA = mk(st, "twa", [128, T], BF16)
        P1, B_P1 = mk(st, "p1", [128, T], BF16)
        BTZ, B_BT = mk(st, "btz", [128, 2, 4, T], BF16, nb=4)
        KTZ, B_KT = mk(st, "ktz", [128, 2, 4, T], BF16, nb=4)
        AR, B_AR = mk(st, "ar", [128, 4, NTC, 256], BF16, nb=4)
        BTT, B_BTT = mk(st, "btt", [128, NTC, 512], BF16, nb=NTC)
        KTT, B_KTT = mk(st, "ktt", [128, NTC, 512], BF16, nb=NTC)
        RKB2, B_RKB2 = mk(st, "rkb2", [128, 2, T], BF16, nb=2)
        EWC, B_EWC = mk(st, "ewc", [128, 4, NTC], F32)
        BDOT, B_BDOT = mk(st, "bdot", [128, NTC, 16], F32)
        SC, B_SC = mk(st, "sc", [128, 8, 512], BF16, nb=8)
        PK = [mk(st, "pk%d" % i, [128, 2, 512], BF16, nb=2) for i in range(2)]
        QK = [mk(st, "qk%d" % i, [128, 2, 512], BF16, nb=2) for i in range(2)]
        SK, B_SK = mk(st, "sk", [128, 2, 512], BF16, nb=2)
        SCXv = TMPALL[:, 3:7, :].rearrange("p a b -> p (a b)").bitcast(BF16)[:, 0:4096].rearrange("p (h f) -> p h f", f=512)
        B_SCX = [Buf("scx%d" % i) for i in range(8)]
        SKXv = TMPALL[:, 7, :].bitcast(BF16)[:, 0:1024].rearrange("p (q f) -> p q f", f=512)
        B_SKX = [Buf("skx%d" % i) for i in range(2)]
        ALIAS_TMP = [B_TMP[i] for i in range(3, 8)]
        ALIAS_SCAN = B_SCX + B_SKX

        def handoff(src, dst):
            toks = set()
            for b_ in src:
                if b_.lw is not None:
                    toks.add(b_.lw)
                toks.update(b_.rd)
            for d_ in dst:
                d_.rd = list(set(d_.rd) | toks)

        def sc_slot(slot):
            if slot < 2:
                return SC[:, slot * 4:(slot + 1) * 4, :], B_SC[slot * 4:(slot + 1) * 4]
            return SCXv[:, (slot - 2) * 4:(slot - 1) * 4, :], B_SCX[(slot - 2) * 4:(slot - 1) * 4]

        def sk_slot(slot):
            if slot < 2:
                return SK[:, slot, :], B_SK[slot]
            return SKXv[:, slot - 2, :], B_SKX[slot - 2]

        def pq_slot(slot):
            i_, j_ = slot // 2, slot % 2
            return (PK[i_][0][:, j_, :], PK[i_][1][j_]), (QK[i_][0][:, j_, :], QK[i_][1][j_])

        XB, B_XB = mk(st, "xbv", [128, 512], BF16, nb=2)
        UB, B_UB = mk(st, "ubv", [128, 512], BF16, nb=2)
        HS, B_HS = mk(st, "hs", [128, 8, 64], F32, nb=4)
        HBZ, B_HB = mk(st, "hbz", [128, 8, 128], BF16, nb=4)
        YA, B_YA = TMP[0][0], [TMP[0][1], Buf("ya1")]
        YB, B_YB = TMP[1][0], [TMP[1][1], Buf("yb1")]
        SGT, B_SGT = TMP[2]
        YG, B_YG = mk(st, "yg", [128, 512], BF16, nb=2)
        ST8, B_ST8 = mk(st, "st8", [128, 6, 8], F32, nb=2)
        MG, B_MG = VB, B_VB
        YPI, B_YPI = VB, B_VB
        PSB = []
        for i in range(8):
            t_ = st.enter_context(nc.psum_tensor("ps%d" % i, [128, 512], F32))
            PSB.append((t_, Buf("ps%d" % i)))
        PTB = [(PSB[6][0][:].bitcast(BF16), PSB[6][1]), (PSB[7][0][:].bitcast(BF16), PSB[7][1])]
        ring = [0, 0, 0, 0, 0, 0]

        def ps():
            r = PSB[ring[0] % 5]
            ring[0] += 1
            return r

        PBD, B_PBD = PSB[5]

        def pt():
            r = PTB[ring[1] % 2]
            ring[1] += 1
            return r

        def psL():
            r = PSB[ring[4] % 4]
            ring[4] += 1
            return r

        pP_i = [0]

        def pP():
            r = PSB[6 + pP_i[0] % 2]
            pP_i[0] += 1
            return r

        PT4 = (PSB[4][0][:].bitcast(BF16), PSB[4][1])

        def psS():
            r = PSB[(4, 6, 7)[ring[5] % 3]]
            ring[5] += 1
            return r

        def tmp():
            r = TMP[ring[3] % NTMP]
            ring[3] += 1
            return r

        def tri(dst_ap, cmp_op, pattern_step, cm):
            S.memset("pool", CTMP[:, 0:128], 1.0, [B_CTMP])
            S.op("pool", lambda e: e.affine_select(out=CTMP[:, 0:128], in_=CTMP[:, 0:128], pattern=[[pattern_step, 128]],
                                                   compare_op=cmp_op, fill=0.0, base=0, channel_multiplier=cm),
                 [B_CTMP], [B_CTMP])
            S.copy("pool", dst_ap, CTMP[:, 0:128], [B_CTMP], [B_IDENT])

        tri(IDENT[:], ALU.is_equal, -1, 1)
        tri(MASKL[:], ALU.is_gt, -1, 1)
        tri(MASKT[:, 0:128], ALU.is_gt, 1, -1)
        tri(MASKT[:, 128:256], ALU.is_ge, 1, -1)
        tri(MASKT[:, 256:384], ALU.is_gt, 1, -1)
        tri(MASKT[:, 384:512], ALU.is_ge, 1, -1)
        S.memset("pool", BONES[:], 0.0, [B_IDENT])
        S.memset("pool", BONES[0:64, 0:64], 1.0, [B_IDENT])
        S.memset("pool", BONES[64:128, 64:128], 1.0, [B_IDENT])
        S.memset("pool", E2[:], 0.0, [B_IDENT])
        S.memset("pool", E2[0:64, 0:1], 1.0, [B_IDENT])
        S.memset("pool", E2[64:128, 1:2], 1.0, [B_IDENT])
        S.memset("pool", ONESF[:], 1.0, [B_IDENT])
        S.memset("pool", CORR[:], 1.0, [B_IDENT])
        for g, w in enumerate(POOL_WINDOWS):
            for t_ in range(w - 1):
                S.memset("pool", CORR[:, g, t_:t_ + 1], float(w) / float(t_ + 1), [B_IDENT])
        S.memset("pool", BTZ[:], 0.0, B_BT)
        S.memset("pool", KTZ[:], 0.0, B_KT)
        S.memset("pool", HBZ[:], 0.0, B_HB)
        S.memset("pool", DAZ[:], 0.0, [B_DA])
        S.memset("pool", V1W[:], 0.0, [B_V1W])
        S.memset("pool", V2W[:], 0.0, [B_V2W])
        S.memset("pool", EPSC[:, 0:1], NORM_EPS, [B_IDENT])
        S.memset("pool", EPSC[:, 1:2], LNX_EPS, [B_IDENT])
        S.dma("sp", FG[:], final_g.to_broadcast([128, D]), (), [B_FG])
        _chk("consts")

        def wl(li, idx):
            t_, b_ = WLB[ring[2] % NRL]
            ring[2] += 1
            S.dma("sp", t_[:], WLd[li, idx], [B_WLd[li][idx]], [b_])
            return t_, b_

        wr_i = [0]

        def wr(li, idx):
            t_, b_ = WRB[wr_i[0] % 2]
            wr_i[0] += 1
            S.dma("sp", t_[:], WRd[li, idx], [B_WRd[li][idx]], [b_])
            return t_, b_

        def inproj(li, m, shift, alloc=None):
            pt_, pb_ = (alloc or ps)()
            w_, wb_ = wl(li, m)
            for kc in range(8):
                S.mm(pt_[:], w_[:, kc * 128:(kc + 1) * 128], HT[:, kc, 1:T + 1], kc == 0, (kc == 7 and not shift),
                     [wb_, B_HT], [pb_])
            if shift:
                w2, wb2 = wl(li, 65 + m - 16)
                for kc in range(8):
                    S.mm(pt_[:], w2[:, kc * 128:(kc + 1) * 128], HT[:, kc, 0:T], False, kc == 7,
                         [wb2, B_HT], [pb_])
            return pt_, pb_

        def inproj_g(li, m, alloc, step=2):
            pt_, pb_ = alloc()
            w_, wb_ = wl(li, m)
            for kc in range(8):
                S.mm(pt_[:], w_[:, kc * 128:(kc + 1) * 128], HT[:, kc, 1:T + 1], kc == 0, kc == 7, [wb_, B_HT], [pb_])
                if kc % step == step - 1 and kc != 7:
                    yield
            return pt_, pb_

        for li in range(NL):
            L = L0 + li
            last = (L == DEPTH - 1) and final_norm
            x_src, B_xs = (x_in, None) if li == 0 else (xbufs[(li - 1) % 2], B_xb[(li - 1) % 2])
            x_dst, B_xd = (out_d, None) if li == NL - 1 else (xbufs[li % 2], B_xb[li % 2])
            Rxs = [B_xs] if B_xs is not None else []
            Wxd = [B_xd] if B_xd is not None else []
            S.dma("sp", G_BC[:], norm_g[li:li + 1, :].to_broadcast([128, D]), (), [B_G])
            S.dma("sp", LNW[:], lnx_w[li:li + 1, :].to_broadcast([128, D]), (), [B_LNW])
            S.dma("sp", LNB[:], lnx_b[li:li + 1, :].to_broadcast([128, D]), (), [B_LNB])
            S.dma("sp", PP[:].rearrange("p k c -> p (k c)"), pp_in[li], (), [B_PP])
            S.ts("pool", PP[:, 7, :], PP[:, 5, :], -1.0, ALU.mult, [B_PP], [B_PP], s2=1.0, op1=ALU.add)
            S.dma("sp", STGS[:].rearrange("p (g kc d) -> p g kc d", g=4, kc=2),
                  pool_lin[li].rearrange("g (kc kp) d -> kp g kc d", kp=128), (), [B_STGS])
            S.copy("pool", PL[:].rearrange("p g kc d -> p (g kc d)"), STGS[:], [B_STGS], [B_PL])
            S.dma("sp", STGS[0:64, 0:D], decay_w2[li], [], [B_STGS])
            S.dma("sp", STGS[64:128, 0:D], a2[li], [], [B_STGS])
            S.copy("pool", DAZ[0:64, 0, :], STGS[0:64, 0:D], [B_STGS], [B_DA])
            S.copy("pool", DAZ[64:128, 1, :], STGS[64:128, 0:D], [B_STGS], [B_DA])
            if L > 0:
                S.dma("sp", STGS[:, 0:256].rearrange("p (kc j) -> p kc j", j=32),
                      vres_v1[li].rearrange("(kc kp) j -> kp kc j", kp=128), [], [B_STGS])
                S.copy("pool", V1W[:, :, 0:32], STGS[:, 0:256].rearrange("p (kc j) -> p kc j", j=32), [B_STGS], [B_V1W])
                S.dma("sp", STGS[0:32, 0:D], vres_v2[li], [], [B_STGS])
                S.copy("pool", V2W[0:32, :], STGS[0:32, 0:D], [B_STGS], [B_V2W])
            ppc = lambda k, c: PP[:, k, c:c + 1]
            _chk("params")

            for s in range(NSEQ):
                for ti in range(NT):
                    row0 = s * SEQ + ti * T
                    vt_idx = s * NT + ti
                    first = (ti == 0)
                    if first:
                        S.memset("pool", HT[:, :, 0:1], 0.0, [B_HT])
                    else:
                        S.copy("pool", HT[:, :, 0:1], HT[:, :, T:T + 1], [B_HT], [B_HT])
                    for tg in range(4):
                        xi, B_xi = XIN[tg % 2]
                        S.dma("sp", xi[:], x_src[row0 + tg * 128: row0 + (tg + 1) * 128, :], Rxs, [B_xi])
                        S.act(JUNK[:], xi[:], AF.Square, [B_xi], [B_JUNK, B_STAT], accum_out=STAT[:, 0:1])
                        S.act(STAT[:, 1:2], STAT[:, 0:1], AF.Sqrt, [B_STAT, B_EPSC], [B_STAT], bias=EPSC[:, 0:1], scale=1.0 / D)
                        S.op("dve", lambda e: e.reciprocal(out=STAT[:, 2:3], in_=STAT[:, 1:2]), [B_STAT], [B_STAT])
                        S.stt(HN[:], xi[:], STAT[:, 2:3], G_BC[:], ALU.mult, ALU.mult, [B_xi, B_STAT, B_G], [B_HN])
                        ptt, ptb = pt()
                        for kc in range(8):
                            S.tr(ptt[:, kc * 128:(kc + 1) * 128], HN[:, kc * 128:(kc + 1) * 128], IDENT[:],
                                 [B_HN, B_IDENT], [ptb])
                        S.copy("act", HT[:, :, 1 + tg * 128: 1 + (tg + 1) * 128],
                               ptt[:].rearrange("p (kc j) -> p kc j", j=128), [ptb], [B_HT])

                    _chk("N")
                    def pool_p1(li=li, first=first):
                        handoff(B_VF[0:7], [B_UG, B_SA, B_SBb])
                        if first:
                            S.memset("pool", HALO_P[:], 0.0, [B_HALOP])
                        for g, w in enumerate(POOL_WINDOWS):
                            for j in range(2):
                                p_, pb_ = yield from inproj_g(li, 2 * g + j, pP)
                                S.copy("act", UG[:, j, 16:16 + T], p_[:], [pb_], [B_UG])
                                yield
                            for j in range(2):
                                p_, pb_ = yield from inproj_g(li, 8 + 2 * g + j, pP)
                                S.act(SG[:, j, :], p_[:], AF.Silu, [pb_], [B_SG])
                                yield
                            S.copy("pool", UG[:, :, 0:16], HALO_P[:, 2 * g:2 * g + 2, :], [B_HALOP], [B_UG])
                            S.copy("pool", HALO_P[:, 2 * g:2 * g + 2, :], UG[:, :, T:T + 16], [B_UG], [B_HALOP])
                            NJ = T + 16
                            S.tt("pool", SA[:, :, 1:NJ], UG[:, :, 1:NJ], UG[:, :, 0:NJ - 1], ALU.add, [B_UG], [B_SA])
                            cur, B_cur = SA, B_SA
                            if w >= 4:
                                S.tt("pool", SBb[:, :, 3:NJ], SA[:, :, 3:NJ], SA[:, :, 1:NJ - 2], ALU.add, [B_SA], [B_SBb])
                                cur, B_cur = SBb, B_SBb
                            yield
                            if w >= 8:
                                S.tt("pool", SA[:, :, 7:NJ], SBb[:, :, 7:NJ], SBb[:, :, 3:NJ - 4], ALU.add, [B_SBb], [B_SA])
                                cur, B_cur = SA, B_SA
                            if w >= 16:
                                S.tt("pool", SBb[:, :, 15:NJ], SA[:, :, 15:NJ], SA[:, :, 7:NJ - 8], ALU.add, [B_SA], [B_SBb])
                                cur, B_cur = SBb, B_SBb
                            if first:
                                S.tt("pool", cur[:, :, 16:32], cur[:, :, 16:32],
                                     CORR[:, g, :].unsqueeze(1).to_broadcast([128, 2, 16]), ALU.mult,
                                     [B_cur, B_CORR], [B_cur])
                            S.stt(DG[:], cur[:, :, 16:16 + T], 1.0 / w, UG[:, :, 16:16 + T], ALU.mult, ALU.subtract,
                                  [B_cur, B_UG], [B_DG])
                            yield
                            for mo in range(2):
                                p_, pb_ = pP()
                                for kc in range(2):
                                    S.mm(p_[:], PL[:, g, kc, mo * 128:(mo + 1) * 128], DG[:, kc, :], kc == 0, kc == 1,
                                         [B_PL, B_DG], [pb_])
                                S.stt(YPI[:, 2 * g + mo, :], p_[:], ppc(0, 2 * g + mo), SG[:, mo, :], ALU.mult, ALU.mult,
                                      [pb_, B_PP, B_SG], [B_YPI[2 * g + mo]])
                            yield
                        handoff([B_UG, B_SA, B_SBb], B_VF[0:7])

                    def pool_p2(li=li):
                        for m in range(8):
                            py_, pyb_ = pP()
                            w_, wb_ = wl(li, 90 + m)
                            for kc in range(8):
                                S.mm(py_[:], w_[:, kc * 128:(kc + 1) * 128], YPI[:, kc, :], kc == 0, kc == 7,
                                     [wb_, B_YPI[kc]], [pyb_])
                                if kc % 2 == 1:
                                    yield
                            pg_, pgb_ = yield from inproj_g(li, 49 + m, pP)
                            S.act(SGM[:], pg_[:], AF.Sigmoid, [pgb_], [B_SGM])
                            S.tt("dve", MP[:, m, :], py_[:], SGM[:], ALU.mult, [pyb_, B_SGM], [B_MP])
                            yield

                    bg_gens = [pool_p1(), pool_p2()]

                    _chk("P")
                    p_, pb_ = inproj(li, 40, True)
                    S.act(TWA[0:64, :], p_[0:64, :], AF.Tanh, [pb_], [B_TWA])
                    S.copy("act", TWA[64:128, :], p_[64:128, :], [pb_], [B_TWA])
                    for c in range(8):
                        p_, pb_ = inproj(li, 32 + c, True)
                        S.copy("act", VF[:, c, :], p_[:], [pb_], [B_VF[c]])
                    if L == 0:
                        S.dma("act", vfd[vt_idx], VF[:].rearrange("p c t -> p (c t)"), B_VF, [B_vfd])
                    else:
                        for c in range(8):
                            S.copy("dve" if c % 2 == 0 else "act", VB[:, c, :], VF[:, c, :], [B_VF[c]], [B_VB[c]])
                        p1_, p1b_ = ps()
                        for kc in range(8):
                            S.mm(p1_[:], V1W[:, kc, :], VB[:, kc, :], kc == 0, kc == 7, [B_V1W, B_VB[kc]], [p1b_])
                        S.copy("act", P1[:], p1_[:], [p1b_], [B_P1])
                        for c in range(8):
                            p_, pb_ = ps()
                            S.mm(p_[:], V2W[:, c * 128:(c + 1) * 128], P1[:], True, True, [B_V2W, B_P1], [pb_])
                            mx, B_mx = tmp()
                            S.act(mx[:], p_[:], AF.Sigmoid, [pb_, B_PP], [B_mx], bias=ppc(3, c))
                            vf1, B_vf1 = tmp()
                            S.dma("sp", vf1[:], vfd[vt_idx, :, c * T:(c + 1) * T], [B_vfd], [B_vf1])
                            S.tt("pool", vf1[:], vf1[:], VF[:, c, :], ALU.subtract, [B_vf1, B_VF[c]], [B_vf1])
                            S.tt("pool", vf1[:], vf1[:], mx[:], ALU.mult, [B_vf1, B_mx], [B_vf1])
                            S.tt("dve", VF[:, c, :], VF[:, c, :], vf1[:], ALU.add, [B_VF[c], B_vf1], [B_VF[c]])
                    for c in range(8):
                        S.copy("dve" if c % 2 == 0 else "act", VB[:, c, :], VF[:, c, :], [B_VF[c]], [B_VB[c]])
                    for tc in range(NTC):
                        ptt, ptb = pt()
                        for c in range(8):
                            S.tr(ptt[:, c * 128:(c + 1) * 128], VB[:, c, tc * 128:(tc + 1) * 128], IDENT[:],
                                 [B_VB[c], B_IDENT], [ptb])
                        S.copy("act", VT[:, tc, :], ptt[:], [ptb], [B_VT[tc]])

                    _chk("R0")
                    for half in range(2):
                        def prep_c(cl, half=half):
                            c = half * 4 + cl
                            st_ = (cl % 2) * 7
                            sl = [TMP[st_ + i] for i in range(7)]
                            RK, B_RK = RKB2[:, cl % 2, :], B_RKB2[cl % 2]
                            pr_, prb_ = inproj(li, 16 + c, True, psL)
                            yield
                            pk_, pkb_ = inproj(li, 24 + c, True, psL)
                            yield
                            pd_, pdb_ = psS()
                            S.mm(pd_[:], DAZ[:, 0, c * 128:(c + 1) * 128], TWA[:], True, True, [B_DA, B_TWA], [pdb_])
                            sgd, B_sgd = sl[0]
                            S.act(sgd[:], pd_[:], AF.Sigmoid, [pdb_, B_PP], [B_sgd], bias=ppc(1, c))
                            pa_, pab_ = psS()
                            S.mm(pa_[:], DAZ[:, 1, c * 128:(c + 1) * 128], TWA[:], True, True, [B_DA, B_TWA], [pab_])
                            ag, B_ag = sl[1]
                            S.act(ag[:], pa_[:], AF.Sigmoid, [pab_, B_PP], [B_ag], bias=ppc(2, c))
                            kkr, B_kkr = sl[5]
                            S.ts("dve", kkr[:], pk_[:], ppc(4, c), ALU.mult, [pkb_, B_PP], [B_kkr])
                            S.tt("dve", RK, kkr[:], kkr[:], ALU.mult, [B_kkr], [B_RK])
                            pn_, pnb_ = psS()
                            S.mm(pn_[:], BONES[:], RK, True, True, [B_BONES, B_RK], [pnb_])
                            nrm, B_nrm = sl[6]
                            S.act(nrm[:], pn_[:], AF.Sqrt, [pnb_], [B_nrm])
                            yield
                            cs, B_cs = sl[2]
                            for tc in range(NTC):
                                S.op("dve", lambda e, cs=cs, sgd=sgd, tc=tc: e.tensor_tensor_scan(
                                    out=cs[:, tc * 128:(tc + 1) * 128], data0=ONESF[:], data1=sgd[:, tc * 128:(tc + 1) * 128],
                                    initial=0.0, op0=ALU.mult, op1=ALU.add), [B_ONESF, B_sgd], [B_cs])
                            csp, B_csp = sl[3]
                            S.tt("pool", csp[:], cs[:], sgd[:], ALU.subtract, [B_cs, B_sgd], [B_csp])
                            yield
                            S.ts("dve", nrm[:], nrm[:], 1e-12, ALU.max, [B_nrm], [B_nrm])
                            S.op("dve", lambda e, nrm=nrm: e.reciprocal(out=nrm[:], in_=nrm[:]), [B_nrm], [B_nrm])
                            S.tt("pool", kkr[:], kkr[:], nrm[:], ALU.mult, [B_kkr, B_nrm], [B_kkr])
                            S.ts("pool", nrm[:], ag[:], ppc(5, c), ALU.mult, [B_ag, B_PP], [B_nrm], s2=ppc(7, c), op1=ALU.add)
                            yield
                            ew, B_ew = sl[4]
                            S.act(ew[:], cs[:], AF.Exp, [B_cs], [B_ew], scale=-DECAY_C)
                            ewi, B_ewi = cs, B_cs
                            S.act(ewi[:], cs[:], AF.Exp, [B_cs], [B_ewi], scale=DECAY_C)
                            ewp, B_ewp = csp, B_csp
                            S.act(ewp[:], csp[:], AF.Exp, [B_csp], [B_ewp], scale=-DECAY_C)
                            S.copy("pool", EWC[:, cl, :], ew[:].rearrange("p (tc j) -> p tc j", j=128)[:, :, 127],
                                   [B_ew], [B_EWC])
                            yield
                            kp, B_kp = sgd, B_sgd
                            S.tt("dve", kp[:], pk_[:], nrm[:], ALU.mult, [pkb_, B_nrm], [B_kp])
                            S.stt(RK, pr_[:], ppc(6, c), kp[:], ALU.mult, ALU.mult, [prb_, B_PP, B_kp], [B_RK])
                            for tc in range(NTC):
                                S.mm(PBD[:, tc * 16 + 2 * c: tc * 16 + 2 * c + 2], RK[:, tc * 128:(tc + 1) * 128], E2[:],
                                     True, True, [B_RK, B_E2], [B_PBD])
                            yield
                            arv = AR[:, cl].rearrange("p tc (two j) -> p tc two j", two=2)
                            S.tt("dve", arv[:, :, 1, :], pr_[:].rearrange("p (tc j) -> p tc j", j=128),
                                 ew[:].rearrange("p (tc j) -> p tc j", j=128), ALU.mult, [prb_, B_ew], [B_AR[cl]])
                            S.stt(arv[:, :, 0, :], kkr[:].rearrange("p (tc j) -> p tc j", j=128), -1.0,
                                  ewp[:].rearrange("p (tc j) -> p tc j", j=128), ALU.mult, ALU.mult,
                                  [B_kkr, B_ewp], [B_AR[cl]])
                            yield
                            S.tt("pool", ag[:], ag[:], kkr[:], ALU.mult, [B_ag, B_kkr], [B_ag])
                            for hh_ in range(2):
                                Pq = slice(hh_ * 64, (hh_ + 1) * 64)
                                S.tt("pool", BTZ[Pq, hh_, cl, :], ag[Pq, :], ewi[Pq, :], ALU.mult, [B_ag, B_ewi], [B_BT[cl]])
                                S.tt("pool", KTZ[Pq, hh_, cl, :], kp[Pq, :], ewi[Pq, :], ALU.mult, [B_kp, B_ewi], [B_KT[cl]])
                            yield

                        def interleave2(gens):
                            gens = list(gens)
                            while gens:
                                for g_ in list(gens):
                                    try:
                                        next(g_)
                                    except StopIteration:
                                        gens.remove(g_)

                        interleave2([prep_c(0), prep_c(1)])
                        interleave2([prep_c(2), prep_c(3)])
                        for tc in range(NTC):
                            for (srcz, Bsrc, dst, Bdst) in ((BTZ, B_BT, BTT, B_BTT), (KTZ, B_KT, KTT, B_KTT)):
                                pz_, pzb_ = ps()
                                for cl in range(4):
                                    for hh_ in range(2):
                                        S.mm(pz_[:, cl * 128:(cl + 1) * 128], srcz[:, hh_, cl, tc * 128:(tc + 1) * 128], IDENT[:],
                                             hh_ == 0, hh_ == 1, [Bsrc[cl], B_IDENT], [pzb_])
                                S.copy("act", dst[:, tc, :], pz_[:], [pzb_], [Bdst[tc]])
                        S.copy("act", BDOT[:, :, half * 8:(half + 1) * 8],
                               PBD[:, 0:NTC * 16].rearrange("p (tc h) -> p tc h", h=16)[:, :, half * 8:(half + 1) * 8],
                               [B_PBD], [B_BDOT])
                        _chk("R1")
                        if first and True:
                            S.memset("pool", HS[:, half * 4:(half + 1) * 4, :], 0.0, B_HS[half * 2:half * 2 + 2])
                            S.memset("pool", HBZ[:, half * 4:(half + 1) * 4, :], 0.0, B_HB[half * 2:half * 2 + 2])
                        _chk("S00")
                        wg_, wgb_ = wr(li, half)

                        handoff(ALIAS_TMP, ALIAS_SCAN)

                        def build_unit(tc, q, half=half):
                            tcs = slice(tc * 128, (tc + 1) * 128)
                            slot = (tc % 2) * 2 + q
                            SCs, B_SCs = sc_slot(slot)
                            SKs, B_SKs = sk_slot(slot)
                            (Pk, BPk1), (Qk, BQk) = pq_slot(slot)
                            def pb():
                                r = PSB[ring[0] % 4]
                                ring[0] += 1
                                return r
                            pq_, pqb_ = pb()
                            for hq in range(4):
                                h = q * 4 + hq
                                cl, hh = h // 2, h % 2
                                S.mm(pq_[:, hq * 128:(hq + 1) * 128], AR[:, cl, tc, 0:128], BTZ[:, hh, cl, tcs], True, True,
                                     [B_AR[cl], B_BT[cl]], [pqb_])
                            S.tt("dve", Qk.rearrange("p (h j) -> p h j", j=128),
                                 pq_[:].rearrange("p (h j) -> p h j", j=128),
                                 MASKL[:].unsqueeze(1).to_broadcast([128, 4, 128]), ALU.mult,
                                 [pqb_, B_MASKL], [BQk])
                            for hq in range(4):
                                h = q * 4 + hq
                                cl, hh = h // 2, h % 2
                                psc_, pscb_ = pb()
                                S.mm(psc_[:, 0:256], BTZ[:, hh, cl, tcs], AR[:, cl, tc, :], True, True,
                                     [B_BT[cl], B_AR[cl]], [pscb_])
                                S.mm(psc_[:, 256:512], KTZ[:, hh, cl, tcs], AR[:, cl, tc, :], True, True,
                                     [B_KT[cl], B_AR[cl]], [pscb_])
                                S.tt("dve", SCs[:, hq, :], psc_[:], MASKT[:], ALU.mult, [pscb_, B_MASKT], [B_SCs[hq]])
                                if hq % 2 == 1:
                                    yield
                            S.tt("dve", SKs.rearrange("p (h j) -> p h j", j=128), SCs[:, :, 0:128],
                                 IDENT[:].unsqueeze(1).to_broadcast([128, 4, 128]), ALU.add, B_SCs + [B_IDENT], [B_SKs])
                            yield
                            for k in range(7):
                                def Pk_ap(hq, k=k):
                                    if k == 0:
                                        return SCs[:, hq, 0:128]
                                    return Pk[:, hq * 128:(hq + 1) * 128]
                                BPk = B_SCs if k == 0 else [BPk1]
                                if k <= 5:
                                    pQ_, pQb_ = pb()
                                    for hq in range(4):
                                        S.mm(pQ_[:, hq * 128:(hq + 1) * 128], Pk_ap(hq), Qk[:, hq * 128:(hq + 1) * 128],
                                             True, True, BPk + [BQk], [pQb_])
                                if k >= 1:
                                    pS_, pSb_ = pb()
                                    for hq in range(4):
                                        S.mm(pS_[:, hq * 128:(hq + 1) * 128], Qk[:, hq * 128:(hq + 1) * 128],
                                             SKs[:, hq * 128:(hq + 1) * 128], True, True, [BQk, B_SKs], [pSb_])
                                if k <= 4:
                                    pP_, pPb_ = pb()
                                    for hq in range(4):
                                        S.mm(pP_[:, hq * 128:(hq + 1) * 128], Qk[:, hq * 128:(hq + 1) * 128], Pk_ap(hq),
                                             True, True, BPk + [BQk], [pPb_])
                                if k >= 1:
                                    S.tt("dve", SKs, pS_[:], SKs, ALU.add, [pSb_, B_SKs], [B_SKs])
                                if k <= 5:
                                    S.copy("act", Qk, pQ_[:], [pQb_], [BQk])
                                if k <= 4:
                                    S.copy("act", Pk, pP_[:], [pPb_], [BPk1])
                                yield

                        def seq_unit(tc, q, half=half, wg_=wg_, wgb_=wgb_):
                            tcs = slice(tc * 128, (tc + 1) * 128)
                            slot = (tc % 2) * 2 + q
                            SCs, B_SCs = sc_slot(slot)
                            SKs, B_SKs = sk_slot(slot)
                            qc = slice(q * 256, (q + 1) * 256)
                            B_hs = B_HS[half * 2 + q]
                            B_hb = B_HB[half * 2 + q]
                            if q == 0:
                                pgt_, pgtb_ = PSB[4]
                                for kc in range(8):
                                    S.mm(pgt_[:], HT[:, kc, 1 + tc * 128: 1 + (tc + 1) * 128], wg_[:, kc * 512:(kc + 1) * 512],
                                         kc == 0, kc == 7, [B_HT, wgb_], [pgtb_])
                                S.act(SGT[:], pgt_[:], AF.Silu, [pgtb_], [B_SGT])
                                yield
                            px_, pxb_ = PSB[5]
                            for pl in range(2):
                                cl = q * 2 + pl
                                c = half * 4 + cl
                                S.mm(px_[:, pl * 128:(pl + 1) * 128], AR[:, cl, tc, 0:128], HBZ[:, c, :], True, False,
                                     [B_AR[cl], B_hb], [pxb_])
                                for hh in range(2):
                                    hl = pl * 2 + hh
                                    S.mm(px_[:, hl * 64:(hl + 1) * 64], SCs[:, hl, 256:384], VT[:, tc, (c * 2 + hh) * 64:(c * 2 + hh + 1) * 64],
                                         False, hh == 1, [B_SCs[hl], B_VT[tc]], [pxb_])
                            S.copy("act", XB[:, qc], px_[:, 0:256], [pxb_], [B_XB[q]])
                            yield
                            pu_, pub_ = PSB[4]
                            for hl in range(4):
                                S.mm(pu_[:, hl * 64:(hl + 1) * 64], SKs[:, hl * 128:(hl + 1) * 128],
                                     XB[:, q * 256 + hl * 64: q * 256 + (hl + 1) * 64], True, True, [B_SKs, B_XB[q]], [pub_])
                            S.copy("act", UB[:, qc], pu_[:, 0:256], [pub_], [B_UB[q]])
                            yield
                            py_, pyb_ = PSB[5]
                            for pl in range(2):
                                cl = q * 2 + pl
                                c = half * 4 + cl
                                S.mm(py_[:, pl * 128:(pl + 1) * 128], AR[:, cl, tc, 128:256], HBZ[:, c, :], True, False,
                                     [B_AR[cl], B_hb], [pyb_])
                                for hh in range(2):
                                    h = cl * 2 + hh
                                    hl = pl * 2 + hh
                                    vcol = slice((c * 2 + hh) * 64, (c * 2 + hh + 1) * 64)
                                    S.mm(py_[:, hl * 64:(hl + 1) * 64], SCs[:, hl, 128:256], UB[:, h * 64:(h + 1) * 64], False, False,
                                         [B_SCs[hl], B_UB[q]], [pyb_])
                                    S.mm(py_[:, hl * 64:(hl + 1) * 64], SCs[:, hl, 384:512], VT[:, tc, vcol], False, hh == 1,
                                         [B_SCs[hl], B_VT[tc]], [pyb_])
                            ph_, phb_ = PSB[4]
                            for pl in range(2):
                                cl = q * 2 + pl
                                c = half * 4 + cl
                                pc = slice(cl * 128, (cl + 1) * 128)
                                S.mm(ph_[:, pl * 128:(pl + 1) * 128], BTT[:, tc, pc], UB[:, pc], True, False, [B_BTT[tc], B_UB[q]], [phb_])
                                S.mm(ph_[:, pl * 128:(pl + 1) * 128], KTT[:, tc, pc], VT[:, tc, c * 128:(c + 1) * 128], False, True,
                                     [B_KTT[tc], B_VT[tc]], [phb_])
                            for hh in range(2):
                                Ph = slice(hh * 64, (hh + 1) * 64)
                                c0 = half * 4 + q * 2
                                hsv = HS[Ph, c0:c0 + 2, :]
                                phv = ph_[Ph, 0:256].rearrange("p (c x) -> p c x", x=128)[:, :, hh * 64:(hh + 1) * 64]
                                S.tt("dve", hsv, phv, hsv, ALU.add, [phb_, B_hs], [B_hs])
                                S.tt("dve", hsv, hsv, EWC[Ph, q * 2:q * 2 + 2, tc].unsqueeze(2).to_broadcast([64, 2, 64]), ALU.mult,
                                     [B_hs, B_EWC], [B_hs])
                                S.copy("pool", HBZ[Ph, c0:c0 + 2, hh * 64:(hh + 1) * 64], hsv, [B_hs], [B_hb])
                            yield
                            B_st = B_ST8[q]
                            hq4 = slice(q * 4, (q + 1) * 4)
                            y3 = py_[:, 0:256].rearrange("p (h v) -> p h v", v=64)
                            ya3 = YA[:, qc].rearrange("p (h v) -> p h v", v=64)
                            yb3 = YB[:, qc].rearrange("p (h v) -> p h v", v=64)
                            S.op("dve", lambda e, y3=y3: e.tensor_reduce(out=ST8[:, 0, hq4], in_=y3, axis=AX.X, op=ALU.add),
                                 [pyb_], [B_st])
                            S.ts("dve", ST8[:, 2, hq4], ST8[:, 0, hq4], 1.0 / 64, ALU.mult, [B_st], [B_st])
                            S.tt("dve", ya3, y3, ST8[:, 2, hq4].unsqueeze(2).to_broadcast([128, 4, 64]), ALU.subtract,
                                 [pyb_, B_st], [B_YA[q]])
                            yield
                            S.tt("pool", YB[:, qc], YA[:, qc], YA[:, qc], ALU.mult, [B_YA[q]], [B_YB[q]])
                            S.op("dve", lambda e, yb3=yb3: e.tensor_reduce(out=ST8[:, 1, hq4], in_=yb3, axis=AX.X, op=ALU.add),
                                 [B_YB[q]], [B_st])
                            S.act(ST8[:, 5, hq4], ST8[:, 1, hq4], AF.Sqrt, [B_st, B_EPSC], [B_st], bias=EPSC[:, 1:2], scale=1.0 / 64)
                            S.op("dve", lambda e: e.reciprocal(out=ST8[:, 5, hq4], in_=ST8[:, 5, hq4]), [B_st], [B_st])
                            yield
                            S.tt("dve", ya3, ya3, ST8[:, 5, hq4].unsqueeze(2).to_broadcast([128, 4, 64]), ALU.mult,
                                 [B_YA[q], B_st], [B_YA[q]])
                            hc = slice(half * 512 + q * 256, half * 512 + (q + 1) * 256)
                            S.tt("pool", YA[:, qc], YA[:, qc], LNW[:, hc], ALU.mult, [B_YA[q], B_LNW], [B_YA[q]])
                            S.tt("pool", YA[:, qc], YA[:, qc], LNB[:, hc], ALU.add, [B_YA[q], B_LNB], [B_YA[q]])
                            S.tt("dve", yb3, VT[:, tc, hc].rearrange("p (h v) -> p h v", v=64),
                                 BDOT[:, tc, half * 8 + q * 4: half * 8 + (q + 1) * 4].unsqueeze(2).to_broadcast([128, 4, 64]), ALU.mult,
                                 [B_VT[tc], B_BDOT], [B_YB[q]])
                            yield
                            S.tt("pool", YA[:, qc], YA[:, qc], YB[:, qc], ALU.add, [B_YA[q], B_YB[q]], [B_YA[q]])
                            S.tt("pool", YG[:, qc], YA[:, qc], SGT[:, qc], ALU.mult, [B_YA[q], B_SGT], [B_YG[q]])
                            yield
                            ptt, ptb = PT4
                            for pl in range(2):
                                S.tr(ptt[:, pl * 128:(pl + 1) * 128], YG[:, q * 256 + pl * 128: q * 256 + (pl + 1) * 128], IDENT[:],
                                     [B_YG[q], B_IDENT], [ptb])
                            c0 = half * 4 + q * 2
                            S.copy("act", YGT[:, c0:c0 + 2, tcs],
                                   ptt[:, 0:256].rearrange("p (c j) -> p c j", j=128), [ptb], [B_YGT])
                            yield

                        def chain(*gs):
                            for g_ in gs:
                                yield from g_

                        def interleave(gens):
                            gens = list(gens)
                            while gens:
                                for g_ in list(gens):
                                    try:
                                        next(g_)
                                    except StopIteration:
                                        gens.remove(g_)

                        bg = bg_gens[half]
                        if NO_BG:
                            for _ in bg:
                                pass

                        def interleave_bg(gens):
                            gens = list(gens)
                            while gens:
                                for g_ in list(gens):
                                    try:
                                        next(g_)
                                    except StopIteration:
                                        gens.remove(g_)
                                try:
                                    next(bg)
                                except StopIteration:
                                    pass

                        interleave_bg([build_unit(0, 0), build_unit(0, 1)])
                        for tc in range(NTC):
                            gens = [chain(seq_unit(tc, 0), seq_unit(tc, 1))]
                            if tc + 1 < NTC:
                                gens += [build_unit(tc + 1, 0), build_unit(tc + 1, 1)]
                            interleave_bg(gens)
                        for _ in bg:
                            pass
                        handoff(ALIAS_SCAN, ALIAS_TMP)

                    _chk("S")
                    for m in range(8):
                        py_, pyb_ = ps()
                        w_, wb_ = wl(li, 98 + m)
                        for kc in range(8):
                            S.mm(py_[:], w_[:, kc * 128:(kc + 1) * 128], YGT[:, kc, :], kc == 0, kc == 7,
                                 [wb_, B_YGT], [pyb_])
                        pg_, pgb_ = inproj(li, 57 + m, False)
                        S.act(SGM[:], pg_[:], AF.Sigmoid, [pgb_], [B_SGM])
                        t1, B_t1 = TMP[7]
                        S.tt("dve", t1[:], py_[:], SGM[:], ALU.mult, [pyb_, B_SGM], [B_t1])
                        S.tt("pool", MG[:, m, :], t1[:], MP[:, m, :], ALU.add, [B_t1, B_MP], [B_MG[m]])
                    _chk("O1")
                    wo = [wr(li, 2), wr(li, 3)]
                    for tg in range(4):
                        xi, B_xi = XIN[tg % 2]
                        rows = slice(row0 + tg * 128, row0 + (tg + 1) * 128)
                        S.dma("sp", xi[:], x_src[rows, :], Rxs, [B_xi])
                        for hf in range(2):
                            po_, pob_ = ps()
                            for kc in range(8):
                                S.mm(po_[:], MG[:, kc, tg * 128:(tg + 1) * 128], wo[hf][0][:, kc * 512:(kc + 1) * 512],
                                     kc == 0, kc == 7, [B_MG, wo[hf][1]], [pob_])
                            S.tt("dve", xi[:, hf * 512:(hf + 1) * 512], po_[:], xi[:, hf * 512:(hf + 1) * 512], ALU.add,
                                 [pob_, B_xi], [B_xi])
                        if last:
                            S.act(JUNK[:], xi[:], AF.Square, [B_xi], [B_JUNK, B_STAT], accum_out=STAT[:, 4:5])
                            S.act(STAT[:, 5:6], STAT[:, 4:5], AF.Sqrt, [B_STAT, B_EPSC], [B_STAT], bias=EPSC[:, 0:1], scale=1.0 / D)
                            S.op("dve", lambda e: e.reciprocal(out=STAT[:, 6:7], in_=STAT[:, 5:6]), [B_STAT], [B_STAT])
                            S.stt(xi[:], xi[:], STAT[:, 6:7], FG[:], ALU.mult, ALU.mult, [B_xi, B_STAT, B_FG], [B_xi])
                        _chk("O2")
                        S.dma("act", x_dst[rows, :], xi[:], [B_xi], Wxd)
                        _chk("O3")
                    _chk("T")


def _pack_pp(inp, L0, L1):
    nl = L1 - L0
    pp = np.zeros((nl, 128, NPAR, 8), np.float32)
    for li in range(nl):
        L = L0 + li
        vecs = [inp["pool_scale"][L], inp["decay_w0"][L], inp["a0"][L],
                inp["vres_v0"][L - 1] if L > 0 else None, inp["k_k"][L], inp["k_a"][L],
                np.asarray(inp["r_k"][L]).reshape(-1)]
        for k, v in enumerate(vecs):
            if v is None:
                continue
            pp[li, :, k, :] = np.asarray(v, np.float32).reshape(8, 128).T
    return pp.reshape(nl, 128, NPAR * 8)


def _layer_inputs(inp, L0, L1):
    f = lambda a: np.ascontiguousarray(np.asarray(a, np.float32))
    nl = L1 - L0
    v1 = np.zeros((nl, D, 32), np.float32)
    v2 = np.zeros((nl, 32, D), np.float32)
    for li in range(nl):
        L = L0 + li
        if L > 0:
            v1[li] = inp["vres_v1"][L - 1]
            v2[li] = inp["vres_v2"][L - 1]
    return {
        "w_in": f(inp["w_in"][L0:L1]), "pool_lin": f(inp["pool_lin"][L0:L1]),
        "w_pool_proj": f(inp["w_pool_proj"][L0:L1]), "mu_shift": f(inp["mu_shift"][L0:L1]),
        "decay_w2": f(inp["decay_w2"][L0:L1]), "a2": f(inp["a2"][L0:L1]),
        "vres_v1": v1, "vres_v2": v2,
        "w_rwkv_proj": f(inp["w_rwkv_proj"][L0:L1]), "w_out": f(inp["w_out"][L0:L1]),
        "norm_g": f(inp["norm_g"][L0:L1]), "lnx_w": f(inp["lnx_w"][L0:L1]), "lnx_b": f(inp["lnx_b"][L0:L1]),
        "final_g": f(inp["final_g"]).reshape(1, D), "pp": _pack_pp(inp, L0, L1),
    }


NO_BG = False
_PROG_CACHE = {}
STOP = None


class _Stop(Exception):
    pass


_CNT = [0]


_TILE = [0]


def _chk(name):
    if name == "T":
        _TILE[0] += 1
    if STOP == name or STOP == "%s@%d" % (name, _TILE[0]):
        raise _Stop()
    if name == "cnt" and STOP is not None and STOP.startswith("cnt"):
        _CNT[0] += 1
        if _CNT[0] >= int(STOP[3:]):
            raise _Stop()


def run_layers(inp, xs, L0, L1, NT, vfirst=None, cores=None):
    key = (L0, L1, NT)
    if key not in _PROG_CACHE:
        _PROG_CACHE[key] = build_program(L0, L1, NT, True)
    nc = _PROG_CACHE[key]
    shared = _layer_inputs(inp, L0, L1)
    in_maps = []
    for i, xc in enumerate(xs):
        m = dict(shared)
        m["x"] = np.ascontiguousarray(xc)
        if L0 > 0:
            m["vfirst_in"] = vfirst[i]
        in_maps.append(m)
    cores = list(range(len(xs))) if cores is None else cores
    import os
    if os.environ.get("KTRACE"):
        res = run_bass_kernel_spmd(nc, in_maps, core_ids=cores, trace=True)
        print("EXEC_TIME_NS", res.exec_time_ns, flush=True)
        try:
            print("TRACE", res.instructions_and_trace[1] if res.instructions_and_trace else None, flush=True)
        except Exception as ex:
            print("TRACE?", ex)
    else:
        res = run_bass_kernel_spmd(nc, in_maps, core_ids=cores)
    outs = [r["out"] for r in res.results]
    vf = [r["vfirst_out"] for r in res.results] if (L0 == 0 and L1 < DEPTH) else None
    return outs, vf


FUSED = True


def kernel(**inputs):
    x = np.asarray(inputs["x"], np.float32)
    xs = [np.ascontiguousarray(x[2 * i:2 * i + 2].reshape(NSEQ * SEQ, D)) for i in range(8)]
    if FUSED:
        outs, _ = run_layers(inputs, xs, 0, DEPTH, SEQ // T)
    else:
        outs, vf = run_layers(inputs, xs, 0, 1, SEQ // T)
        for L in range(1, DEPTH):
            outs, _ = run_layers(inputs, outs, L, L + 1, SEQ // T, vfirst=vf)
    return np.stack([o.reshape(NSEQ, SEQ, D) for o in outs], 0).reshape(16, SEQ, D).astype(np.float32)
```

```python
import numpy as np
from contextlib import ExitStack
import concourse.bass as bass
import concourse.mybir as mybir
from concourse.bass_utils import run_bass_kernel_spmd

F32 = mybir.dt.float32
BF16 = mybir.dt.bfloat16
AF = mybir.ActivationFunctionType
ALU = mybir.AluOpType
AX = mybir.AxisListType

D = 1024
SEQ = 2048
NSEQ = 2
DEPTH = 4
T = 512
NTC = 4
IN_W = 8320
NWL = 106
NORM_EPS = 1e-6
LNX_EPS = 1e-5 * 64
DECAY_C = 0.6065306597126334
POOL_WINDOWS = (2, 4, 8, 16)
NPAR = 8


class Buf:
    __slots__ = ("name", "lw", "rd", "const")

    def __init__(self, name, const=False):
        self.name = name
        self.lw = None
        self.rd = []
        self.const = const


class Sched:
    ENGS = ("pe", "act", "dve", "pool", "sp")

    def __init__(self, nc, stack, n_dsem=32, strict=("act", "dve", "pool")):
        self.nc = nc
        self.sem = {e: stack.enter_context(nc.semaphore("s_" + e)) for e in self.ENGS}
        self.dsem = [stack.enter_context(nc.semaphore("d%d" % i)) for i in range(n_dsem)]
        self.dval = [0] * n_dsem
        self.dnext = 0
        self.cnt = {e: 0 for e in self.ENGS}
        self.prog = {e: [] for e in self.ENGS}
        self.seen = {e: {} for e in self.ENGS}
        self.strict = set(strict)
        self.nwait = 0
        self.ninst = 0

    @staticmethod
    def _flat(x):
        out = []
        for b in x:
            if isinstance(b, (list, tuple)):
                out.extend(Sched._flat(b))
            elif b is not None:
                out.append(b)
        return out

    def _deps(self, eng, reads, writes):
        deps = set()
        for b in reads:
            if b.lw is not None:
                deps.add(b.lw)
        for b in writes:
            if b.lw is not None:
                deps.add(b.lw)
            for t in b.rd:
                deps.add(t)
        need = {}
        for (kind, ident, val) in deps:
            if kind == "e" and ident == eng and eng not in self.strict:
                continue
            key = (kind, ident)
            if self.seen[eng].get(key, 0) >= val:
                continue
            if need.get(key, 0) < val:
                need[key] = val
        for key, val in need.items():
            self.seen[eng][key] = val
            sem = self.sem[key[1]] if key[0] == "e" else self.dsem[key[1]]
            self.prog[eng].append(("w", sem, val))
            self.nwait += 1

    def _commit(self, tok, reads, writes):
        for b in writes:
            b.lw = tok
            b.rd = []
        for b in reads:
            if b.const or b in writes:
                continue
            b.rd.append(tok)
            if len(b.rd) > 400:
                b.rd = b.rd[-400:]

    def op(self, eng, fn, reads=(), writes=()):
        reads, writes = self._flat(reads), self._flat(writes)
        self._deps(eng, reads, writes)
        self.cnt[eng] += 1
        n = self.cnt[eng]
        self.prog[eng].append(("i", fn, self.sem[eng], 1))
        self._commit(("e", eng, n), reads, writes)
        self.ninst += 1

    def dma(self, q, out, in_, reads=(), writes=(), **kw):
        reads, writes = self._flat(reads), self._flat(writes)
        k = self.dnext
        self.dnext = (self.dnext + 1) % len(self.dsem)
        if self.dval[k] > 0 and self.seen[q].get(("d", k), 0) < self.dval[k]:
            self.seen[q][("d", k)] = self.dval[k]
            self.prog[q].append(("w", self.dsem[k], self.dval[k]))
        self._deps(q, reads, writes)
        self.dval[k] += 16
        v = self.dval[k]
        self.prog[q].append(("i", lambda e: e.dma_start(out=out, in_=in_, **kw), self.dsem[k], 16))
        self._commit(("d", k, v), reads, writes)
        self.ninst += 1

    def barrier(self):
        for e in self.ENGS:
            for p in self.ENGS:
                if p == e or self.cnt[p] == 0:
                    continue
                if self.seen[e].get(("e", p), 0) < self.cnt[p]:
                    self.seen[e][("e", p)] = self.cnt[p]
                    self.prog[e].append(("w", self.sem[p], self.cnt[p]))
            for k, v in enumerate(self.dval):
                if v > 0 and self.seen[e].get(("d", k), 0) < v:
                    self.seen[e][("d", k)] = v
                    self.prog[e].append(("w", self.dsem[k], v))

    def finish(self, q="sp"):
        for k, v in enumerate(self.dval):
            if v > 0:
                self.prog[q].append(("w", self.dsem[k], v))

    def run(self, block):
        def body(prog):
            def f(e):
                for it in prog:
                    if it[0] == "w":
                        e.wait_ge(it[1], it[2])
                    else:
                        it[1](e).then_inc(it[2], it[3])
            return f
        block.tensor(body(self.prog["pe"]))
        block.scalar(body(self.prog["act"]))
        block.vector(body(self.prog["dve"]))
        block.gpsimd(body(self.prog["pool"]))
        block.sync(body(self.prog["sp"]))

    def mm(self, out, lhsT, rhs, start, stop, R, W):
        self.op("pe", lambda e: e.matmul(out, lhsT, rhs, start=start, stop=stop), R, W)

    def tr(self, out, in_, ident, R, W):
        self.op("pe", lambda e: e.transpose(out, in_, ident), R, W)

    def act(self, out, in_, func, R, W, bias=None, scale=None, accum_out=None):
        kw = {}
        if bias is not None:
            kw["bias"] = bias
        if scale is not None:
            kw["scale"] = scale
        if accum_out is not None:
            kw["accum_out"] = accum_out
        self.op("act", lambda e: e.activation(out=out, in_=in_, func=func, **kw), R, W)

    def tt(self, eng, out, in0, in1, op, R, W):
        self.op(eng, lambda e: e.tensor_tensor(out=out, in0=in0, in1=in1, op=op), R, W)

    def ts(self, eng, out, in0, s1, op0, R, W, s2=None, op1=None):
        if op1 is None:
            self.op(eng, lambda e: e.tensor_scalar(out=out, in0=in0, scalar1=s1, scalar2=None, op0=op0), R, W)
        else:
            self.op(eng, lambda e: e.tensor_scalar(out=out, in0=in0, scalar1=s1, scalar2=s2, op0=op0, op1=op1), R, W)

    def stt(self, out, in0, scalar, in1, op0, op1, R, W):
        self.op("dve", lambda e: e.scalar_tensor_tensor(out=out, in0=in0, scalar=scalar, in1=in1, op0=op0, op1=op1), R, W)

    def copy(self, eng, out, in_, R, W):
        if eng == "act":
            self.op("act", lambda e: e.activation(out=out, in_=in_, func=AF.Copy), R, W)
        else:
            self.op(eng, lambda e: e.tensor_copy(out=out, in_=in_), R, W)

    def memset(self, eng, ap, val, W):
        self.op(eng, lambda e: e.memset(ap, val), (), W)


def build_program(L0, L1, NT, final_norm):
    NL = L1 - L0
    NTOK = NSEQ * SEQ
    nc = bass.Bass("TRN2", target_bir_lowering=False, dynamic_dma_scratch_size=2048)
    dt_in = lambda name, shape: nc.dram_tensor(name, shape, F32, kind="ExternalInput").ap()
    x_in = dt_in("x", [NTOK, D])
    w_in = dt_in("w_in", [NL, D, IN_W])
    pool_lin = dt_in("pool_lin", [NL, 4, 256, 256])
    w_pool_proj = dt_in("w_pool_proj", [NL, D, D])
    mu_shift = dt_in("mu_shift", [NL, 3200])
    decay_w2 = dt_in("decay_w2", [NL, 64, D])
    a2 = dt_in("a2", [NL, 64, D])
    vres_v1 = dt_in("vres_v1", [NL, D, 32])
    vres_v2 = dt_in("vres_v2", [NL, 32, D])
    w_rwkv_proj = dt_in("w_rwkv_proj", [NL, D, D])
    w_out = dt_in("w_out", [NL, D, D])
    norm_g = dt_in("norm_g", [NL, D])
    lnx_w = dt_in("lnx_w", [NL, D])
    lnx_b = dt_in("lnx_b", [NL, D])
    final_g = dt_in("final_g", [1, D])
    pp_in = dt_in("pp", [NL, 128, NPAR * 8])
    out_d = nc.dram_tensor("out", [NTOK, D], F32, kind="ExternalOutput").ap()
    NVT = NSEQ * NT
    if L0 == 0 and L1 < DEPTH:
        vfd = nc.dram_tensor("vfirst_out", [NVT, 128, 8 * T], F32, kind="ExternalOutput").ap()
    elif L0 == 0:
        vfd = nc.dram_tensor("vfirst_scr", [NVT, 128, 8 * T], F32, kind="Internal").ap()
    else:
        vfd = dt_in("vfirst_in", [NVT, 128, 8 * T])
    xbufs = [nc.dram_tensor("xbuf%d" % i, [NTOK, D], F32, kind="Internal").ap() for i in range(2)] if NL > 1 else []
    WLd = nc.dram_tensor("wl_scr", [NL, NWL, 128, 1024], BF16, kind="Internal").ap()
    WRd = nc.dram_tensor("wr_scr", [NL, 4, 128, 4096], BF16, kind="Internal").ap()
    B_vfd = Buf("vfd")
    B_xb = [Buf("xb0"), Buf("xb1")]
    B_WLd = [[Buf("wld") for _ in range(NWL)] for _ in range(NL)]
    B_WRd = [[Buf("wrd") for _ in range(4)] for _ in range(NL)]

    with ExitStack() as st:
        S = Sched(nc, st)

        def mk(stack, name, shape, dt, nb=0):
            t = stack.enter_context(nc.sbuf_tensor(name, shape, dt))
            if nb:
                return t, [Buf("%s%d" % (name, i)) for i in range(nb)]
            return t, Buf(name)

        with ExitStack() as pst:
            NPB = 4
            STG = [mk(pst, "stg%d" % i, [128, 8, 512], F32) for i in range(NPB)]
            CB = [mk(pst, "cb%d" % i, [128, 4, 1024], BF16) for i in range(NPB)]
            CB2 = [mk(pst, "cbp%d" % i, [128, 4, 1024], BF16) for i in range(NPB)]
            MU, B_MU = mk(pst, "mu_bc", [128, 3200], F32)
            MU1, B_MU1 = mk(pst, "mu1_bc", [128, 3200], F32)
            cast_engs = ["pool", "dve", "act"]
            ce = [0]

            def cast(out, in_, R, W):
                e = cast_engs[ce[0] % 3]
                ce[0] += 1
                S.copy(e, out, in_, R, W)

            blk = [0]
            for li in range(NL):
                S.dma("sp", MU[:], mu_shift[li:li + 1, :].to_broadcast([128, 3200]), (), [B_MU])
                S.ts("pool", MU1[:], MU[:], -1.0, ALU.mult, [B_MU], [B_MU1], s2=1.0, op1=ALU.add)
                segs = []
                for c0 in list(range(0, 5248, 512)):
                    segs.append((w_in[li], c0, min(512, 5248 - c0), "L", c0 // 128))
                for c0 in range(6272, 8320, 512):
                    segs.append((w_in[li], c0, 512, "L", c0 // 128))
                segs.append((w_in[li], 5248, 512, "R", 0))
                segs.append((w_in[li], 5760, 512, "R", 1))
                for c0 in (0, 512):
                    segs.append((w_pool_proj[li], c0, 512, "L", 90 + c0 // 128))
                for c0 in (0, 512):
                    segs.append((w_rwkv_proj[li], c0, 512, "L", 98 + c0 // 128))
                segs.append((w_out[li], 0, 512, "R", 2))
                segs.append((w_out[li], 512, 512, "R", 3))
                for (src, c0, ncol, kind, base) in segs:
                    b = blk[0] % NPB
                    blk[0] += 1
                    stg, B_stg = STG[b]
                    cb, B_cb = CB[b]
                    cb2, B_cb2 = CB2[b]
                    S.dma("sp", stg[:, :, 0:ncol], src[:, c0:c0 + ncol].rearrange("(kc kp) c -> kp kc c", kp=128),
                          (), [B_stg])
                    if kind == "R":
                        cbr = cb[:].rearrange("p a f -> p (a f)").rearrange("p (kc j) -> p kc j", j=512)
                        for kc in range(8):
                            cast(cbr[:, kc, :], stg[:, kc, :], [B_stg], [B_cb])
                        S.dma("act", WRd[li, base], cb[:].rearrange("p a f -> p (a f)"), [B_cb], [B_WRd[li][base]])
                        continue
                    nm = ncol // 128
                    for mi in range(nm):
                        m = base + mi
                        o = cb[:, mi, :].rearrange("p (kc j) -> p kc j", j=128)
                        i_ = stg[:, :, mi * 128:(mi + 1) * 128]
                        is_shift = (base < 90) and (16 <= m <= 40)
                        if is_shift:
                            mc = (m - 16) * 128
                            S.tt("pool" if mi % 2 == 0 else "dve", o, i_,
                                 MU1[:, mc:mc + 128].unsqueeze(1).to_broadcast([128, 8, 128]), ALU.mult,
                                 [B_stg, B_MU1], [B_cb])
                            o2 = cb2[:, mi, :].rearrange("p (kc j) -> p kc j", j=128)
                            S.tt("dve" if mi % 2 == 0 else "pool", o2, i_,
                                 MU[:, mc:mc + 128].unsqueeze(1).to_broadcast([128, 8, 128]), ALU.mult,
                                 [B_stg, B_MU], [B_cb2])
                            S.dma("act", WLd[li, 65 + m - 16], cb2[:, mi, :], [B_cb2], [B_WLd[li][65 + m - 16]])
                        else:
                            cast(o, i_, [B_stg], [B_cb])
                        S.dma("act", WLd[li, m], cb[:, mi, :], [B_cb], [B_WLd[li][m]])
        S.barrier()
        try:
            _chk("prepass")
            _main(locals())
        except _Stop:
            pass
        S.finish("sp")
        print("program: %d instructions, %d waits" % (S.ninst, S.nwait), {e: S.cnt[e] for e in S.ENGS}, flush=True)
        with nc.Block() as block:
            S.run(block)
    return nc


def _main(env):
    globals_ = env
    nc = env["nc"]; st = env["st"]; S = env["S"]; mk = env["mk"]
    NL = env["NL"]; L0 = env["L0"]; L1 = env["L1"]; NT = env["NT"]; final_norm = env["final_norm"]
    x_in = env["x_in"]; out_d = env["out_d"]; vfd = env["vfd"]; xbufs = env["xbufs"]
    WLd = env["WLd"]; WRd = env["WRd"]; B_vfd = env["B_vfd"]; B_xb = env["B_xb"]; B_WLd = env["B_WLd"]; B_WRd = env["B_WRd"]
    pool_lin = env["pool_lin"]; decay_w2 = env["decay_w2"]; a2 = env["a2"]; vres_v1 = env["vres_v1"]; vres_v2 = env["vres_v2"]
    norm_g = env["norm_g"]; lnx_w = env["lnx_w"]; lnx_b = env["lnx_b"]; final_g = env["final_g"]; pp_in = env["pp_in"]
    if True:
        IDENT, B_IDENT = mk(st, "ident", [128, 128], BF16)
        MASKT, B_MASKT = mk(st, "maskt", [128, 512], BF16)
        MASKL, B_MASKL = mk(st, "maskl", [128, 128], BF16)
        BONES, B_BONES = mk(st, "bones", [128, 128], BF16)
        E2, B_E2 = mk(st, "e2", [128, 2], BF16)
        ONESF, B_ONESF = mk(st, "onesf", [128, 128], F32)
        CORR, B_CORR = mk(st, "corr", [128, 4, 16], F32)
        EPSC, B_EPSC = mk(st, "epsc", [128, 2], F32)
        NEGH, B_NEGH = mk(st, "negh", [128, 8], F32)
        B_NEGH.const = True
        for b_ in (B_IDENT, B_MASKT, B_MASKL, B_BONES, B_E2, B_ONESF, B_CORR, B_EPSC):
            b_.const = True
        G_BC, B_G = mk(st, "g_bc", [128, D], F32)
        LNW, B_LNW = mk(st, "lnw_bc", [128, D], F32)
        LNB, B_LNB = mk(st, "lnb_bc", [128, D], F32)
        FG, B_FG = mk(st, "fg_bc", [128, D], F32)
        PP, B_PP = mk(st, "pp_sb", [128, NPAR, 8], F32)
        PL, B_PL = mk(st, "pl", [128, 4, 2, 256], BF16)
        DAZ, B_DA = mk(st, "daz", [128, 2, D], BF16)
        V1W, B_V1W = mk(st, "v1w", [128, 8, 128], BF16)
        V2W, B_V2W = mk(st, "v2w", [128, D], BF16)
        HT, B_HT = mk(st, "ht", [128, 8, T + 1], BF16)
        MP, B_MP = mk(st, "mp", [128, 8, T], BF16)
        YGT, B_YGT = mk(st, "ygt", [128, 8, T], BF16)
        XIN = [mk(st, "xin%d" % i, [128, D], F32) for i in range(2)]
        HN, B_HN = mk(st, "hn", [128, D], BF16)
        JUNK, B_JUNK = HN, B_HN
        STAT, B_STAT = mk(st, "stat", [128, 8], F32)
        NRL = 6
        WLB = [mk(st, "wlb%d" % i, [128, 1024], BF16) for i in range(NRL)]
        WRB = [mk(st, "wrb%d" % i, [128, 4096], BF16) for i in range(2)]
        NTMP = 14
        TMPALL, B_TMP = mk(st, "tmpall", [128, NTMP, T + 16], F32, nb=NTMP)
        TMP = [(TMPALL[:, i, 0:T], B_TMP[i]) for i in range(NTMP)]
        SG, B_SG = TMPALL[:, 8:10, 0:T], [B_TMP[8], B_TMP[9]]
        SGM, B_SGM = TMP[10]
        CTMP, B_CTMP = TMP[9]
        DG, B_DG = mk(st, "dg", [128, 2, T], BF16)
        HALO_P, B_HALOP = mk(st, "halop", [128, 8, 16], F32)
        VF, B_VF = mk(st, "vf", [128, 8, T], F32, nb=8)
        STGS, B_STGS = VF[:].rearrange("p c t -> p (c t)")[:, 0:2048], B_VF[0:4]
        VFf = VF[:].rearrange("p c t -> p (c t)")
        UG, B_UG = VFf[:, 0:1056].rearrange("p (a b) -> p a b", a=2), Buf("ug")
        SA, B_SA = VFf[:, 1056:2112].rearrange("p (a b) -> p a b", a=2), Buf("sa")
        SBb, B_SBb = VFf[:, 2112:3168].rearrange("p (a b) -> p a b", a=2), Buf("sbb")
        VB, B_VB = mk(st, "vb", [128, 8, T], BF16, nb=8)
        VT, B_VT = mk(st, "vt", [128, NTC, D], BF16, nb=NTC)
        TWA, B_TWA = mk(st, "twa", [128, T], BF16)
        P1, B_P1 = mk(st, "p1", [128, T], BF16)
        BTZ, B_BT = mk(st, "btz", [128, 2, 4, T], BF16, nb=4)
        KTZ, B_KT = mk(st, "ktz", [128, 2, 4, T], BF16, nb=4)
        AR, B_AR = mk(st, "ar", [128, 4, NTC, 256], BF16, nb=4)
        BTT, B_BTT = mk(st, "btt", [128, NTC, 512], BF16, nb=NTC)
        KTT, B_KTT = mk(st, "ktt", [128, NTC, 512], BF16, nb=NTC)
        RKB2, B_RKB2 = mk(st, "rkb2", [128, 2, T], BF16, nb=2)
        EWC, B_EWC = mk(st, "ewc", [128, 4, NTC], F32)
        BDOT, B_BDOT = mk(st, "bdot", [128, NTC, 16], F32)
        SC, B_SC = mk(st, "sc", [128, 8, 512], BF16, nb=8)
        PK = [mk(st, "pk%d" % i, [128, 2, 512], BF16, nb=2) for i in range(2)]
        QK = [mk(st, "qk%d" % i, [128, 2, 512], BF16, nb=2) for i in range(2)]
        SK, B_SK = mk(st, "sk", [128, 2, 512], BF16, nb=2)
        SCXv = TMPALL[:, 3:7, :].rearrange("p a b -> p (a b)").bitcast(BF16)[:, 0:4096].rearrange("p (h f) -> p h f", f=512)
        B_SCX = [Buf("scx%d" % i) for i in range(8)]
        SKXv = TMPALL[:, 7, :].bitcast(BF16)[:, 0:1024].rearrange("p (q f) -> p q f", f=512)
        B_SKX = [Buf("skx%d" % i) for i in range(2)]
        ALIAS_TMP = [B_TMP[i] for i in range(3, 8)]
        ALIAS_SCAN = B_SCX + B_SKX

        def handoff(src, dst):
            toks = set()
            for b_ in src:
                if b_.lw is not None:
                    toks.add(b_.lw)
                toks.update(b_.rd)
            for d_ in dst:
                d_.rd = list(set(d_.rd) | toks)

        def sc_slot(slot):
            if slot < 2:
                return SC[:, slot * 4:(slot + 1) * 4, :], B_SC[slot * 4:(slot + 1) * 4]
            return SCXv[:, (slot - 2) * 4:(slot - 1) * 4, :], B_SCX[(slot - 2) * 4:(slot - 1) * 4]

        def sk_slot(slot):
            if slot < 2:
                return SK[:, slot, :], B_SK[slot]
            return SKXv[:, slot - 2, :], B_SKX[slot - 2]

        def pq_slot(slot):
            i_, j_ = slot // 2, slot % 2
            return (PK[i_][0][:, j_, :], PK[i_][1][j_]), (QK[i_][0][:, j_, :], QK[i_][1][j_])

        XB, B_XB = mk(st, "xbv", [128, 512], BF16, nb=2)
        UB, B_UB = mk(st, "ubv", [128, 512], BF16, nb=2)
        HS, B_HS = mk(st, "hs", [128, 8, 64], F32, nb=4)
        HBZ, B_HB = mk(st, "hbz", [128, 8, 128], BF16, nb=4)
        YA, B_YA = TMP[0][0], [TMP[0][1], Buf("ya1")]
        YB, B_YB = TMP[1][0], [TMP[1][1], Buf("yb1")]
        SGT, B_SGT = TMP[2]
        YG, B_YG = mk(st, "yg", [128, 512], BF16, nb=2)
        ST8, B_ST8 = mk(st, "st8", [128, 6, 8], F32, nb=2)
        MG, B_MG = VB, B_VB
        YPI, B_YPI = VB, B_VB
        PSB = []
        for i in range(8):
            t_ = st.enter_context(nc.psum_tensor("ps%d" % i, [128, 512], F32))
            PSB.append((t_, Buf("ps%d" % i)))
        PTB = [(PSB[6][0][:].bitcast(BF16), PSB[6][1]), (PSB[7][0][:].bitcast(BF16), PSB[7][1])]
        ring = [0, 0, 0, 0, 0, 0]

        def ps():
            r = PSB[ring[0] % 5]
            ring[0] += 1
            return r

        PBD, B_PBD = PSB[5]

        def pt():
            r = PTB[ring[1] % 2]
            ring[1] += 1
            return r

        def psL():
            r = PSB[ring[4] % 4]
            ring[4] += 1
            return r

        pP_i = [0]

        def pP():
            r = PSB[6 + pP_i[0] % 2]
            pP_i[0] += 1
            return r

        PT4 = (PSB[4][0][:].bitcast(BF16), PSB[4][1])

        def psS():
            r = PSB[(4, 6, 7)[ring[5] % 3]]
            ring[5] += 1
            return r

        def tmp():
            r = TMP[ring[3] % NTMP]
            ring[3] += 1
            return r

        def tri(dst_ap, cmp_op, pattern_step, cm):
            S.memset("pool", CTMP[:, 0:128], 1.0, [B_CTMP])
            S.op("pool", lambda e: e.affine_select(out=CTMP[:, 0:128], in_=CTMP[:, 0:128], pattern=[[pattern_step, 128]],
                                                   compare_op=cmp_op, fill=0.0, base=0, channel_multiplier=cm),
                 [B_CTMP], [B_CTMP])
            S.copy("pool", dst_ap, CTMP[:, 0:128], [B_CTMP], [B_IDENT])

        tri(IDENT[:], ALU.is_equal, -1, 1)
        tri(MASKL[:], ALU.is_gt, -1, 1)
        tri(MASKT[:, 0:128], ALU.is_gt, 1, -1)
        tri(MASKT[:, 128:256], ALU.is_ge, 1, -1)
        tri(MASKT[:, 256:384], ALU.is_gt, 1, -1)
        tri(MASKT[:, 384:512], ALU.is_ge, 1, -1)
        S.memset("pool", BONES[:], 0.0, [B_IDENT])
        S.memset("pool", BONES[0:64, 0:64], 1.0, [B_IDENT])
        S.memset("pool", BONES[64:128, 64:128], 1.0, [B_IDENT])
        S.memset("pool", E2[:], 0.0, [B_IDENT])
        S.memset("pool", E2[0:64, 0:1], 1.0, [B_IDENT])
        S.memset("pool", E2[64:128, 1:2], 1.0, [B_IDENT])
        S.memset("pool", ONESF[:], 1.0, [B_IDENT])
        S.memset("pool", CORR[:], 1.0, [B_IDENT])
        for g, w in enumerate(POOL_WINDOWS):
            for t_ in range(w - 1):
                S.memset("pool", CORR[:, g, t_:t_ + 1], float(w) / float(t_ + 1), [B_IDENT])
        S.memset("pool", BTZ[:], 0.0, B_BT)
        S.memset("pool", KTZ[:], 0.0, B_KT)
        S.memset("pool", HBZ[:], 0.0, B_HB)
        S.memset("pool", DAZ[:], 0.0, [B_DA])
        S.memset("pool", V1W[:], 0.0, [B_V1W])
        S.memset("pool", V2W[:], 0.0, [B_V2W])
        S.memset("pool", NEGH[:], -0.5, [B_NEGH])
        S.memset("pool", EPSC[:, 0:1], NORM_EPS, [B_IDENT])
        S.memset("pool", EPSC[:, 1:2], LNX_EPS, [B_IDENT])
        S.dma("sp", FG[:], final_g.to_broadcast([128, D]), (), [B_FG])
        _chk("consts")

        def wl(li, idx):
            t_, b_ = WLB[ring[2] % NRL]
            ring[2] += 1
            S.dma("sp", t_[:], WLd[li, idx], [B_WLd[li][idx]], [b_])
            return t_, b_

        wr_i = [0]

        def wr(li, idx):
            t_, b_ = WRB[wr_i[0] % 2]
            wr_i[0] += 1
            S.dma("sp", t_[:], WRd[li, idx], [B_WRd[li][idx]], [b_])
            return t_, b_

        def inproj(li, m, shift, alloc=None):
            pt_, pb_ = (alloc or ps)()
            w_, wb_ = wl(li, m)
            for kc in range(8):
                S.mm(pt_[:], w_[:, kc * 128:(kc + 1) * 128], HT[:, kc, 1:T + 1], kc == 0, (kc == 7 and not shift),
                     [wb_, B_HT], [pb_])
            if shift:
                w2, wb2 = wl(li, 65 + m - 16)
                for kc in range(8):
                    S.mm(pt_[:], w2[:, kc * 128:(kc + 1) * 128], HT[:, kc, 0:T], False, kc == 7,
                         [wb2, B_HT], [pb_])
            return pt_, pb_

        def inproj_g(li, m, alloc, step=2):
            pt_, pb_ = alloc()
            w_, wb_ = wl(li, m)
            for kc in range(8):
                S.mm(pt_[:], w_[:, kc * 128:(kc + 1) * 128], HT[:, kc, 1:T + 1], kc == 0, kc == 7, [wb_, B_HT], [pb_])
                if kc % step == step - 1 and kc != 7:
                    yield
            return pt_, pb_

        for li in range(NL):
            L = L0 + li
            last = (L == DEPTH - 1) and final_norm
            x_src, B_xs = (x_in, None) if li == 0 else (xbufs[(li - 1) % 2], B_xb[(li - 1) % 2])
            x_dst, B_xd = (out_d, None) if li == NL - 1 else (xbufs[li % 2], B_xb[li % 2])
            Rxs = [B_xs] if B_xs is not None else []
            Wxd = [B_xd] if B_xd is not None else []
            S.dma("sp", G_BC[:], norm_g[li:li + 1, :].to_broadcast([128, D]), (), [B_G])
            S.dma("sp", LNW[:], lnx_w[li:li + 1, :].to_broadcast([128, D]), (), [B_LNW])
            S.dma("sp", LNB[:], lnx_b[li:li + 1, :].to_broadcast([128, D]), (), [B_LNB])
            S.dma("sp", PP[:].rearrange("p k c -> p (k c)"), pp_in[li], (), [B_PP])
            S.ts("pool", PP[:, 7, :], PP[:, 5, :], -1.0, ALU.mult, [B_PP], [B_PP], s2=1.0, op1=ALU.add)
            S.dma("sp", STGS[:].rearrange("p (g kc d) -> p g kc d", g=4, kc=2),
                  pool_lin[li].rearrange("g (kc kp) d -> kp g kc d", kp=128), (), [B_STGS])
            S.copy("pool", PL[:].rearrange("p g kc d -> p (g kc d)"), STGS[:], [B_STGS], [B_PL])
            S.dma("sp", STGS[0:64, 0:D], decay_w2[li], [], [B_STGS])
            S.dma("sp", STGS[64:128, 0:D], a2[li], [], [B_STGS])
            S.copy("pool", DAZ[0:64, 0, :], STGS[0:64, 0:D], [B_STGS], [B_DA])
            S.copy("pool", DAZ[64:128, 1, :], STGS[64:128, 0:D], [B_STGS], [B_DA])
            if L > 0:
                S.dma("sp", STGS[:, 0:256].rearrange("p (kc j) -> p kc j", j=32),
                      vres_v1[li].rearrange("(kc kp) j -> kp kc j", kp=128), [], [B_STGS])
                S.copy("pool", V1W[:, :, 0:32], STGS[:, 0:256].rearrange("p (kc j) -> p kc j", j=32), [B_STGS], [B_V1W])
                S.dma("sp", STGS[0:32, 0:D], vres_v2[li], [], [B_STGS])
                S.copy("pool", V2W[0:32, :], STGS[0:32, 0:D], [B_STGS], [B_V2W])
            ppc = lambda k, c: PP[:, k, c:c + 1]
            _chk("params")

            for s in range(NSEQ):
                for ti in range(NT):
                    row0 = s * SEQ + ti * T
                    vt_idx = s * NT + ti
                    first = (ti == 0)
                    if first:
                        S.memset("pool", HT[:, :, 0:1], 0.0, [B_HT])
                    else:
                        S.copy("pool", HT[:, :, 0:1], HT[:, :, T:T + 1], [B_HT], [B_HT])
                    for tg in range(4):
                        xi, B_xi = XIN[tg % 2]
                        S.dma("sp", xi[:], x_src[row0 + tg * 128: row0 + (tg + 1) * 128, :], Rxs, [B_xi])
                        S.act(JUNK[:], xi[:], AF.Square, [B_xi], [B_JUNK, B_STAT], accum_out=STAT[:, 0:1])
                        S.ts("pool", STAT[:, 1:2], STAT[:, 0:1], 1.0 / D, ALU.mult, [B_STAT], [B_STAT], s2=NORM_EPS, op1=ALU.add)
                        S.tt("pool", STAT[:, 2:3], STAT[:, 1:2], NEGH[:, 0:1], ALU.pow, [B_STAT, B_NEGH], [B_STAT])
                        S.stt(HN[:], xi[:], STAT[:, 2:3], G_BC[:], ALU.mult, ALU.mult, [B_xi, B_STAT, B_G], [B_HN])
                        ptt, ptb = pt()
                        for kc in range(8):
                            S.tr(ptt[:, kc * 128:(kc + 1) * 128], HN[:, kc * 128:(kc + 1) * 128], IDENT[:],
                                 [B_HN, B_IDENT], [ptb])
                        S.copy("act", HT[:, :, 1 + tg * 128: 1 + (tg + 1) * 128],
                               ptt[:].rearrange("p (kc j) -> p kc j", j=128), [ptb], [B_HT])

                    _chk("N")
                    def pool_p1(li=li, first=first):
                        handoff(B_VF[0:7], [B_UG, B_SA, B_SBb])
                        if first:
                            S.memset("pool", HALO_P[:], 0.0, [B_HALOP])
                        for g, w in enumerate(POOL_WINDOWS):
                            for j in range(2):
                                p_, pb_ = yield from inproj_g(li, 2 * g + j, pP)
                                S.copy("act", UG[:, j, 16:16 + T], p_[:], [pb_], [B_UG])
                                yield
                            for j in range(2):
                                p_, pb_ = yield from inproj_g(li, 8 + 2 * g + j, pP)
                                S.act(SG[:, j, :], p_[:], AF.Silu, [pb_], [B_SG])
                                yield
                            S.copy("pool", UG[:, :, 0:16], HALO_P[:, 2 * g:2 * g + 2, :], [B_HALOP], [B_UG])
                            S.copy("pool", HALO_P[:, 2 * g:2 * g + 2, :], UG[:, :, T:T + 16], [B_UG], [B_HALOP])
                            NJ = T + 16
                            S.tt("pool", SA[:, :, 1:NJ], UG[:, :, 1:NJ], UG[:, :, 0:NJ - 1], ALU.add, [B_UG], [B_SA])
                            cur, B_cur = SA, B_SA
                            if w >= 4:
                                S.tt("pool", SBb[:, :, 3:NJ], SA[:, :, 3:NJ], SA[:, :, 1:NJ - 2], ALU.add, [B_SA], [B_SBb])
                                cur, B_cur = SBb, B_SBb
                            yield
                            if w >= 8:
                                S.tt("pool", SA[:, :, 7:NJ], SBb[:, :, 7:NJ], SBb[:, :, 3:NJ - 4], ALU.add, [B_SBb], [B_SA])
                                cur, B_cur = SA, B_SA
                            if w >= 16:
                                S.tt("pool", SBb[:, :, 15:NJ], SA[:, :, 15:NJ], SA[:, :, 7:NJ - 8], ALU.add, [B_SA], [B_SBb])
                                cur, B_cur = SBb, B_SBb
                            if first:
                                S.tt("pool", cur[:, :, 16:32], cur[:, :, 16:32],
                                     CORR[:, g, :].unsqueeze(1).to_broadcast([128, 2, 16]), ALU.mult,
                                     [B_cur, B_CORR], [B_cur])
                            S.stt(DG[:], cur[:, :, 16:16 + T], 1.0 / w, UG[:, :, 16:16 + T], ALU.mult, ALU.subtract,
                                  [B_cur, B_UG], [B_DG])
                            yield
                            for mo in range(2):
                                p_, pb_ = pP()
                                for kc in range(2):
                                    S.mm(p_[:], PL[:, g, kc, mo * 128:(mo + 1) * 128], DG[:, kc, :], kc == 0, kc == 1,
                                         [B_PL, B_DG], [pb_])
                                S.stt(YPI[:, 2 * g + mo, :], p_[:], ppc(0, 2 * g + mo), SG[:, mo, :], ALU.mult, ALU.mult,
                                      [pb_, B_PP, B_SG], [B_YPI[2 * g + mo]])
                            yield
                        handoff([B_UG, B_SA, B_SBb], B_VF[0:7])

                    def pool_p2(li=li):
                        for m in range(8):
                            py_, pyb_ = pP()
                            w_, wb_ = wl(li, 90 + m)
                            for kc in range(8):
                                S.mm(py_[:], w_[:, kc * 128:(kc + 1) * 128], YPI[:, kc, :], kc == 0, kc == 7,
                                     [wb_, B_YPI[kc]], [pyb_])
                                if kc % 2 == 1:
                                    yield
                            pg_, pgb_ = yield from inproj_g(li, 49 + m, pP)
                            S.act(SGM[:], pg_[:], AF.Sigmoid, [pgb_], [B_SGM])
                            S.tt("dve", MP[:, m, :], py_[:], SGM[:], ALU.mult, [pyb_, B_SGM], [B_MP])
                            yield

                    bg_gens = [pool_p1(), pool_p2()]

                    _chk("P")
                    p_, pb_ = inproj(li, 40, True)
                    S.act(TWA[0:64, :], p_[0:64, :], AF.Tanh, [pb_], [B_TWA])
                    S.copy("act", TWA[64:128, :], p_[64:128, :], [pb_], [B_TWA])
                    for c in range(8):
                        p_, pb_ = inproj(li, 32 + c, True)
                        S.copy("act", VF[:, c, :], p_[:], [pb_], [B_VF[c]])
                    if L == 0:
                        S.dma("act", vfd[vt_idx], VF[:].rearrange("p c t -> p (c t)"), B_VF, [B_vfd])
                    else:
                        for c in range(8):
                            S.copy("dve" if c % 2 == 0 else "act", VB[:, c, :], VF[:, c, :], [B_VF[c]], [B_VB[c]])
                        p1_, p1b_ = ps()
                        for kc in range(8):
                            S.mm(p1_[:], V1W[:, kc, :], VB[:, kc, :], kc == 0, kc == 7, [B_V1W, B_VB[kc]], [p1b_])
                        S.copy("act", P1[:], p1_[:], [p1b_], [B_P1])
                        for c in range(8):
                            p_, pb_ = ps()
                            S.mm(p_[:], V2W[:, c * 128:(c + 1) * 128], P1[:], True, True, [B_V2W, B_P1], [pb_])
                            mx, B_mx = tmp()
                            S.act(mx[:], p_[:], AF.Sigmoid, [pb_, B_PP], [B_mx], bias=ppc(3, c))
                            vf1, B_vf1 = tmp()
                            S.dma("sp", vf1[:], vfd[vt_idx, :, c * T:(c + 1) * T], [B_vfd], [B_vf1])
                            S.tt("pool", vf1[:], vf1[:], VF[:, c, :], ALU.subtract, [B_vf1, B_VF[c]], [B_vf1])
                            S.tt("pool", vf1[:], vf1[:], mx[:], ALU.mult, [B_vf1, B_mx], [B_vf1])
                            S.tt("dve", VF[:, c, :], VF[:, c, :], vf1[:], ALU.add, [B_VF[c], B_vf1], [B_VF[c]])
                    for c in range(8):
                        S.copy("dve" if c % 2 == 0 else "act", VB[:, c, :], VF[:, c, :], [B_VF[c]], [B_VB[c]])
                    for tc in range(NTC):
                        ptt, ptb = pt()
                        for c in range(8):
                            S.tr(ptt[:, c * 128:(c + 1) * 128], VB[:, c, tc * 128:(tc + 1) * 128], IDENT[:],
                                 [B_VB[c], B_IDENT], [ptb])
                        S.copy("act", VT[:, tc, :], ptt[:], [ptb], [B_VT[tc]])

                    _chk("R0")
                    for half in range(2):
                        def prep_c(cl, half=half):
                            c = half * 4 + cl
                            st_ = (cl % 2) * 7
                            sl = [TMP[st_ + i] for i in range(7)]
                            RK, B_RK = RKB2[:, cl % 2, :], B_RKB2[cl % 2]
                            pr_, prb_ = inproj(li, 16 + c, True, psL)
                            yield
                            pk_, pkb_ = inproj(li, 24 + c, True, psL)
                            yield
                            pd_, pdb_ = psS()
                            S.mm(pd_[:], DAZ[:, 0, c * 128:(c + 1) * 128], TWA[:], True, True, [B_DA, B_TWA], [pdb_])
                            sgd, B_sgd = sl[0]
                            S.act(sgd[:], pd_[:], AF.Sigmoid, [pdb_, B_PP], [B_sgd], bias=ppc(1, c))
                            pa_, pab_ = psS()
                            S.mm(pa_[:], DAZ[:, 1, c * 128:(c + 1) * 128], TWA[:], True, True, [B_DA, B_TWA], [pab_])
                            ag, B_ag = sl[1]
                            S.act(ag[:], pa_[:], AF.Sigmoid, [pab_, B_PP], [B_ag], bias=ppc(2, c))
                            kkr, B_kkr = sl[5]
                            S.ts("dve", kkr[:], pk_[:], ppc(4, c), ALU.mult, [pkb_, B_PP], [B_kkr])
                            S.tt("dve", RK, kkr[:], kkr[:], ALU.mult, [B_kkr], [B_RK])
                            pn_, pnb_ = psS()
                            S.mm(pn_[:], BONES[:], RK, True, True, [B_BONES, B_RK], [pnb_])
                            nrm, B_nrm = sl[6]
                            S.act(nrm[:], pn_[:], AF.Sqrt, [pnb_], [B_nrm])
                            yield
                            cs, B_cs = sl[2]
                            for tc in range(NTC):
                                S.op("dve", lambda e, cs=cs, sgd=sgd, tc=tc: e.tensor_tensor_scan(
                                    out=cs[:, tc * 128:(tc + 1) * 128], data0=ONESF[:], data1=sgd[:, tc * 128:(tc + 1) * 128],
                                    initial=0.0, op0=ALU.mult, op1=ALU.add), [B_ONESF, B_sgd], [B_cs])
                            csp, B_csp = sl[3]
                            S.tt("pool", csp[:], cs[:], sgd[:], ALU.subtract, [B_cs, B_sgd], [B_csp])
                            yield
                            S.ts("dve", nrm[:], nrm[:], 1e-12, ALU.max, [B_nrm], [B_nrm])
                            S.op("dve", lambda e, nrm=nrm: e.reciprocal(out=nrm[:], in_=nrm[:]), [B_nrm], [B_nrm])
                            S.tt("pool", kkr[:], kkr[:], nrm[:], ALU.mult, [B_kkr, B_nrm], [B_kkr])
                            S.ts("pool", nrm[:], ag[:], ppc(5, c), ALU.mult, [B_ag, B_PP], [B_nrm], s2=ppc(7, c), op1=ALU.add)
                            yield
                            ew, B_ew = sl[4]
                            S.act(ew[:], cs[:], AF.Exp, [B_cs], [B_ew], scale=-DECAY_C)
                            ewi, B_ewi = cs, B_cs
                            S.act(ewi[:], cs[:], AF.Exp, [B_cs], [B_ewi], scale=DECAY_C)
                            ewp, B_ewp = csp, B_csp
                            S.act(ewp[:], csp[:], AF.Exp, [B_csp], [B_ewp], scale=-DECAY_C)
                            S.copy("pool", EWC[:, cl, :], ew[:].rearrange("p (tc j) -> p tc j", j=128)[:, :, 127],
                                   [B_ew], [B_EWC])
                            yield
                            kp, B_kp = sgd, B_sgd
                            S.tt("dve", kp[:], pk_[:], nrm[:], ALU.mult, [pkb_, B_nrm], [B_kp])
                            S.stt(RK, pr_[:], ppc(6, c), kp[:], ALU.mult, ALU.mult, [prb_, B_PP, B_kp], [B_RK])
                            for tc in range(NTC):
                                S.mm(PBD[:, tc * 16 + 2 * c: tc * 16 + 2 * c + 2], RK[:, tc * 128:(tc + 1) * 128], E2[:],
                                     True, True, [B_RK, B_E2], [B_PBD])
                            yield
                            arv = AR[:, cl].rearrange("p tc (two j) -> p tc two j", two=2)
                            S.tt("dve", arv[:, :, 1, :], pr_[:].rearrange("p (tc j) -> p tc j", j=128),
                                 ew[:].rearrange("p (tc j) -> p tc j", j=128), ALU.mult, [prb_, B_ew], [B_AR[cl]])
                            S.stt(arv[:, :, 0, :], kkr[:].rearrange("p (tc j) -> p tc j", j=128), -1.0,
                                  ewp[:].rearrange("p (tc j) -> p tc j", j=128), ALU.mult, ALU.mult,
                                  [B_kkr, B_ewp], [B_AR[cl]])
                            yield
                            S.tt("pool", ag[:], ag[:], kkr[:], ALU.mult, [B_ag, B_kkr], [B_ag])
                            for hh_ in range(2):
                                Pq = slice(hh_ * 64, (hh_ + 1) * 64)
                                S.tt("pool", BTZ[Pq, hh_, cl, :], ag[Pq, :], ewi[Pq, :], ALU.mult, [B_ag, B_ewi], [B_BT[cl]])
                                S.tt("pool", KTZ[Pq, hh_, cl, :], kp[Pq, :], ewi[Pq, :], ALU.mult, [B_kp, B_ewi], [B_KT[cl]])
                            yield

                        def interleave2(gens):
                            gens = list(gens)
                            while gens:
                                for g_ in list(gens):
                                    try:
                                        next(g_)
                                    except StopIteration:
                                        gens.remove(g_)

                        interleave2([prep_c(0), prep_c(1)])
                        interleave2([prep_c(2), prep_c(3)])
                        for tc in range(NTC):
                            for (srcz, Bsrc, dst, Bdst) in ((BTZ, B_BT, BTT, B_BTT), (KTZ, B_KT, KTT, B_KTT)):
                                pz_, pzb_ = ps()
                                for cl in range(4):
                                    for hh_ in range(2):
                                        S.mm(pz_[:, cl * 128:(cl + 1) * 128], srcz[:, hh_, cl, tc * 128:(tc + 1) * 128], IDENT[:],
                                             hh_ == 0, hh_ == 1, [Bsrc[cl], B_IDENT], [pzb_])
                                S.copy("act", dst[:, tc, :], pz_[:], [pzb_], [Bdst[tc]])
                        S.copy("act", BDOT[:, :, half * 8:(half + 1) * 8],
                               PBD[:, 0:NTC * 16].rearrange("p (tc h) -> p tc h", h=16)[:, :, half * 8:(half + 1) * 8],
                               [B_PBD], [B_BDOT])
                        _chk("R1")
                        if first and True:
                            S.memset("pool", HS[:, half * 4:(half + 1) * 4, :], 0.0, B_HS[half * 2:half * 2 + 2])
                            S.memset("pool", HBZ[:, half * 4:(half + 1) * 4, :], 0.0, B_HB[half * 2:half * 2 + 2])
                        _chk("S00")
                        wg_, wgb_ = wr(li, half)

                        handoff(ALIAS_TMP, ALIAS_SCAN)

                        def build_unit(tc, q, half=half):
                            tcs = slice(tc * 128, (tc + 1) * 128)
                            slot = (tc % 2) * 2 + q
                            SCs, B_SCs = sc_slot(slot)
                            SKs, B_SKs = sk_slot(slot)
                            (Pk, BPk1), (Qk, BQk) = pq_slot(slot)
                            def pb():
                                r = PSB[ring[0] % 4]
                                ring[0] += 1
                                return r
                            pq_, pqb_ = pb()
                            for hq in range(4):
                                h = q * 4 + hq
                                cl, hh = h // 2, h % 2
                                S.mm(pq_[:, hq * 128:(hq + 1) * 128], AR[:, cl, tc, 0:128], BTZ[:, hh, cl, tcs], True, True,
                                     [B_AR[cl], B_BT[cl]], [pqb_])
                            S.tt("dve", Qk.rearrange("p (h j) -> p h j", j=128),
                                 pq_[:].rearrange("p (h j) -> p h j", j=128),
                                 MASKL[:].unsqueeze(1).to_broadcast([128, 4, 128]), ALU.mult,
                                 [pqb_, B_MASKL], [BQk])
                            for hq in range(4):
                                h = q * 4 + hq
                                cl, hh = h // 2, h % 2
                                psc_, pscb_ = pb()
                                S.mm(psc_[:, 0:256], BTZ[:, hh, cl, tcs], AR[:, cl, tc, :], True, True,
                                     [B_BT[cl], B_AR[cl]], [pscb_])
                                S.mm(psc_[:, 256:512], KTZ[:, hh, cl, tcs], AR[:, cl, tc, :], True, True,
                                     [B_KT[cl], B_AR[cl]], [pscb_])
                                S.tt("dve", SCs[:, hq, :], psc_[:], MASKT[:], ALU.mult, [pscb_, B_MASKT], [B_SCs[hq]])
                                if hq % 2 == 1:
                                    yield
                            S.tt("dve", SKs.rearrange("p (h j) -> p h j", j=128), SCs[:, :, 0:128],
                                 IDENT[:].unsqueeze(1).to_broadcast([128, 4, 128]), ALU.add, B_SCs + [B_IDENT], [B_SKs])
                            yield
                            for k in range(7):
                                def Pk_ap(hq, k=k):
                                    if k == 0:
                                        return SCs[:, hq, 0:128]
                                    return Pk[:, hq * 128:(hq + 1) * 128]
                                BPk = B_SCs if k == 0 else [BPk1]
                                if k <= 5:
                                    pQ_, pQb_ = pb()
                                    for hq in range(4):
                                        S.mm(pQ_[:, hq * 128:(hq + 1) * 128], Pk_ap(hq), Qk[:, hq * 128:(hq + 1) * 128],
                                             True, True, BPk + [BQk], [pQb_])
                                if k >= 1:
                                    pS_, pSb_ = pb()
                                    for hq in range(4):
                                        S.mm(pS_[:, hq * 128:(hq + 1) * 128], Qk[:, hq * 128:(hq + 1) * 128],
                                             SKs[:, hq * 128:(hq + 1) * 128], True, True, [BQk, B_SKs], [pSb_])
                                if k <= 4:
                                    pP_, pPb_ = pb()
                                    for hq in range(4):
                                        S.mm(pP_[:, hq * 128:(hq + 1) * 128], Qk[:, hq * 128:(hq + 1) * 128], Pk_ap(hq),
                                             True, True, BPk + [BQk], [pPb_])
                                if k >= 1:
                                    S.tt("dve", SKs, pS_[:], SKs, ALU.add, [pSb_, B_SKs], [B_SKs])
                                if k <= 5:
                                    S.copy("act", Qk, pQ_[:], [pQb_], [BQk])
                                if k <= 4:
                                    S.copy("act", Pk, pP_[:], [pPb_], [BPk1])
                                yield

                        def seq_unit(tc, q, half=half, wg_=wg_, wgb_=wgb_):
                            tcs = slice(tc * 128, (tc + 1) * 128)
                            slot = (tc % 2) * 2 + q
                            SCs, B_SCs = sc_slot(slot)
                            SKs, B_SKs = sk_slot(slot)
                            qc = slice(q * 256, (q + 1) * 256)
                            B_hs = B_HS[half * 2 + q]
                            B_hb = B_HB[half * 2 + q]
                            if q == 0:
                                pgt_, pgtb_ = PSB[4]
                                for kc in range(8):
                                    S.mm(pgt_[:], HT[:, kc, 1 + tc * 128: 1 + (tc + 1) * 128], wg_[:, kc * 512:(kc + 1) * 512],
                                         kc == 0, kc == 7, [B_HT, wgb_], [pgtb_])
                                S.act(SGT[:], pgt_[:], AF.Silu, [pgtb_], [B_SGT])
                                yield
                            px_, pxb_ = PSB[5]
                            for pl in range(2):
                                cl = q * 2 + pl
                                c = half * 4 + cl
                                S.mm(px_[:, pl * 128:(pl + 1) * 128], AR[:, cl, tc, 0:128], HBZ[:, c, :], True, False,
                                     [B_AR[cl], B_hb], [pxb_])
                                for hh in range(2):
                                    hl = pl * 2 + hh
                                    S.mm(px_[:, hl * 64:(hl + 1) * 64], SCs[:, hl, 256:384], VT[:, tc, (c * 2 + hh) * 64:(c * 2 + hh + 1) * 64],
                                         False, hh == 1, [B_SCs[hl], B_VT[tc]], [pxb_])
                            S.copy("act", XB[:, qc], px_[:, 0:256], [pxb_], [B_XB[q]])
                            yield
                            pu_, pub_ = PSB[4]
                            for hl in range(4):
                                S.mm(pu_[:, hl * 64:(hl + 1) * 64], SKs[:, hl * 128:(hl + 1) * 128],
                                     XB[:, q * 256 + hl * 64: q * 256 + (hl + 1) * 64], True, True, [B_SKs, B_XB[q]], [pub_])
                            S.copy("act", UB[:, qc], pu_[:, 0:256], [pub_], [B_UB[q]])
                            yield
                            py_, pyb_ = PSB[5]
                            for pl in range(2):
                                cl = q * 2 + pl
                                c = half * 4 + cl
                                S.mm(py_[:, pl * 128:(pl + 1) * 128], AR[:, cl, tc, 128:256], HBZ[:, c, :], True, False,
                                     [B_AR[cl], B_hb], [pyb_])
                                for hh in range(2):
                                    h = cl * 2 + hh
                                    hl = pl * 2 + hh
                                    vcol = slice((c * 2 + hh) * 64, (c * 2 + hh + 1) * 64)
                                    S.mm(py_[:, hl * 64:(hl + 1) * 64], SCs[:, hl, 128:256], UB[:, h * 64:(h + 1) * 64], False, False,
                                         [B_SCs[hl], B_UB[q]], [pyb_])
                                    S.mm(py_[:, hl * 64:(hl + 1) * 64], SCs[:, hl, 384:512], VT[:, tc, vcol], False, hh == 1,
                                         [B_SCs[hl], B_VT[tc]], [pyb_])
                            ph_, phb_ = PSB[4]
                            for pl in range(2):
                                cl = q * 2 + pl
                                c = half * 4 + cl
                                pc = slice(cl * 128, (cl + 1) * 128)
                                S.mm(ph_[:, pl * 128:(pl + 1) * 128], BTT[:, tc, pc], UB[:, pc], True, False, [B_BTT[tc], B_UB[q]], [phb_])
                                S.mm(ph_[:, pl * 128:(pl + 1) * 128], KTT[:, tc, pc], VT[:, tc, c * 128:(c + 1) * 128], False, True,
                                     [B_KTT[tc], B_VT[tc]], [phb_])
                            for hh in range(2):
                                Ph = slice(hh * 64, (hh + 1) * 64)
                                c0 = half * 4 + q * 2
                                hsv = HS[Ph, c0:c0 + 2, :]
                                phv = ph_[Ph, 0:256].rearrange("p (c x) -> p c x", x=128)[:, :, hh * 64:(hh + 1) * 64]
                                S.tt("dve", hsv, phv, hsv, ALU.add, [phb_, B_hs], [B_hs])
                                S.tt("dve", hsv, hsv, EWC[Ph, q * 2:q * 2 + 2, tc].unsqueeze(2).to_broadcast([64, 2, 64]), ALU.mult,
                                     [B_hs, B_EWC], [B_hs])
                                S.copy("pool", HBZ[Ph, c0:c0 + 2, hh * 64:(hh + 1) * 64], hsv, [B_hs], [B_hb])
                            yield
                            B_st = B_ST8[q]
                            hq4 = slice(q * 4, (q + 1) * 4)
                            y3 = py_[:, 0:256].rearrange("p (h v) -> p h v", v=64)
                            ya3 = YA[:, qc].rearrange("p (h v) -> p h v", v=64)
                            yb3 = YB[:, qc].rearrange("p (h v) -> p h v", v=64)
                            S.op("dve", lambda e, y3=y3: e.tensor_reduce(out=ST8[:, 0, hq4], in_=y3, axis=AX.X, op=ALU.add),
                                 [pyb_], [B_st])
                            S.ts("dve", ST8[:, 2, hq4], ST8[:, 0, hq4], 1.0 / 64, ALU.mult, [B_st], [B_st])
                            S.tt("dve", ya3, y3, ST8[:, 2, hq4].unsqueeze(2).to_broadcast([128, 4, 64]), ALU.subtract,
                                 [pyb_, B_st], [B_YA[q]])
                            yield
                            S.tt("pool", YB[:, qc], YA[:, qc], YA[:, qc], ALU.mult, [B_YA[q]], [B_YB[q]])
                            S.op("dve", lambda e, yb3=yb3: e.tensor_reduce(out=ST8[:, 1, hq4], in_=yb3, axis=AX.X, op=ALU.add),
                                 [B_YB[q]], [B_st])
                            S.ts("pool", ST8[:, 4, hq4], ST8[:, 1, hq4], 1.0 / 64, ALU.mult, [B_st], [B_st], s2=LNX_EPS, op1=ALU.add)
                            S.tt("pool", ST8[:, 5, hq4], ST8[:, 4, hq4], NEGH[:, 0:4], ALU.pow, [B_st, B_NEGH], [B_st])
                            yield
                            S.tt("dve", ya3, ya3, ST8[:, 5, hq4].unsqueeze(2).to_broadcast([128, 4, 64]), ALU.mult,
                                 [B_YA[q], B_st], [B_YA[q]])
                            hc = slice(half * 512 + q * 256, half * 512 + (q + 1) * 256)
                            S.tt("pool", YA[:, qc], YA[:, qc], LNW[:, hc], ALU.mult, [B_YA[q], B_LNW], [B_YA[q]])
                            S.tt("pool", YA[:, qc], YA[:, qc], LNB[:, hc], ALU.add, [B_YA[q], B_LNB], [B_YA[q]])
                            S.tt("dve", yb3, VT[:, tc, hc].rearrange("p (h v) -> p h v", v=64),
                                 BDOT[:, tc, half * 8 + q * 4: half * 8 + (q + 1) * 4].unsqueeze(2).to_broadcast([128, 4, 64]), ALU.mult,
                                 [B_VT[tc], B_BDOT], [B_YB[q]])
                            yield
                            S.tt("pool", YA[:, qc], YA[:, qc], YB[:, qc], ALU.add, [B_YA[q], B_YB[q]], [B_YA[q]])
                            S.tt("pool", YG[:, qc], YA[:, qc], SGT[:, qc], ALU.mult, [B_YA[q], B_SGT], [B_YG[q]])
                            yield
                            ptt, ptb = PT4
                            for pl in range(2):
                                S.tr(ptt[:, pl * 128:(pl + 1) * 128], YG[:, q * 256 + pl * 128: q * 256 + (pl + 1) * 128], IDENT[:],
                                     [B_YG[q], B_IDENT], [ptb])
                            c0 = half * 4 + q * 2
                            S.copy("act", YGT[:, c0:c0 + 2, tcs],
                                   ptt[:, 0:256].rearrange("p (c j) -> p c j", j=128), [ptb], [B_YGT])
                            yield

                        def chain(*gs):
                            for g_ in gs:
                                yield from g_

                        def interleave(gens):
                            gens = list(gens)
                            while gens:
                                for g_ in list(gens):
                                    try:
                                        next(g_)
                                    except StopIteration:
                                        gens.remove(g_)

                        bg = bg_gens[half]
                        if NO_BG:
                            for _ in bg:
                                pass

                        def interleave_bg(gens):
                            gens = list(gens)
                            while gens:
                                for g_ in list(gens):
                                    try:
                                        next(g_)
                                    except StopIteration:
                                        gens.remove(g_)
                                try:
                                    next(bg)
                                except StopIteration:
                                    pass

                        interleave_bg([build_unit(0, 0), build_unit(0, 1)])
                        for tc in range(NTC):
                            gens = [chain(seq_unit(tc, 0), seq_unit(tc, 1))]
                            if tc + 1 < NTC:
                                gens += [build_unit(tc + 1, 0), build_unit(tc + 1, 1)]
                            interleave_bg(gens)
                        for _ in bg:
                            pass
                        handoff(ALIAS_SCAN, ALIAS_TMP)

                    _chk("S")
                    for m in range(8):
                        py_, pyb_ = ps()
                        w_, wb_ = wl(li, 98 + m)
                        for kc in range(8):
                            S.mm(py_[:], w_[:, kc * 128:(kc + 1) * 128], YGT[:, kc, :], kc == 0, kc == 7,
                                 [wb_, B_YGT], [pyb_])
                        pg_, pgb_ = inproj(li, 57 + m, False)
                        S.act(SGM[:], pg_[:], AF.Sigmoid, [pgb_], [B_SGM])
                        t1, B_t1 = TMP[7]
                        S.tt("dve", t1[:], py_[:], SGM[:], ALU.mult, [pyb_, B_SGM], [B_t1])
                        S.tt("pool", MG[:, m, :], t1[:], MP[:, m, :], ALU.add, [B_t1, B_MP], [B_MG[m]])
                    _chk("O1")
                    wo = [wr(li, 2), wr(li, 3)]
                    for tg in range(4):
                        xi, B_xi = XIN[tg % 2]
                        rows = slice(row0 + tg * 128, row0 + (tg + 1) * 128)
                        S.dma("sp", xi[:], x_src[rows, :], Rxs, [B_xi])
                        for hf in range(2):
                            po_, pob_ = ps()
                            for kc in range(8):
                                S.mm(po_[:], MG[:, kc, tg * 128:(tg + 1) * 128], wo[hf][0][:, kc * 512:(kc + 1) * 512],
                                     kc == 0, kc == 7, [B_MG, wo[hf][1]], [pob_])
                            S.tt("dve", xi[:, hf * 512:(hf + 1) * 512], po_[:], xi[:, hf * 512:(hf + 1) * 512], ALU.add,
                                 [pob_, B_xi], [B_xi])
                        if last:
                            S.act(JUNK[:], xi[:], AF.Square, [B_xi], [B_JUNK, B_STAT], accum_out=STAT[:, 4:5])
                            S.ts("pool", STAT[:, 5:6], STAT[:, 4:5], 1.0 / D, ALU.mult, [B_STAT], [B_STAT], s2=NORM_EPS, op1=ALU.add)
                            S.tt("pool", STAT[:, 6:7], STAT[:, 5:6], NEGH[:, 0:1], ALU.pow, [B_STAT, B_NEGH], [B_STAT])
                            S.stt(xi[:], xi[:], STAT[:, 6:7], FG[:], ALU.mult, ALU.mult, [B_xi, B_STAT, B_FG], [B_xi])
                        _chk("O2")
                        S.dma("act", x_dst[rows, :], xi[:], [B_xi], Wxd)
                        _chk("O3")
                    _chk("T")


def _pack_pp(inp, L0, L1):
    nl = L1 - L0
    pp = np.zeros((nl, 128, NPAR, 8), np.float32)
    for li in range(nl):
        L = L0 + li
        vecs = [inp["pool_scale"][L], inp["decay_w0"][L], inp["a0"][L],
                inp["vres_v0"][L - 1] if L > 0 else None, inp["k_k"][L], inp["k_a"][L],
                np.asarray(inp["r_k"][L]).reshape(-1)]
        for k, v in enumerate(vecs):
            if v is None:
                continue
            pp[li, :, k, :] = np.asarray(v, np.float32).reshape(8, 128).T
    return pp.reshape(nl, 128, NPAR * 8)


def _layer_inputs(inp, L0, L1):
    f = lambda a: np.ascontiguousarray(np.asarray(a, np.float32))
    nl = L1 - L0
    v1 = np.zeros((nl, D, 32), np.float32)
    v2 = np.zeros((nl, 32, D), np.float32)
    for li in range(nl):
        L = L0 + li
        if L > 0:
            v1[li] = inp["vres_v1"][L - 1]
            v2[li] = inp["vres_v2"][L - 1]
    return {
        "w_in": f(inp["w_in"][L0:L1]), "pool_lin": f(inp["pool_lin"][L0:L1]),
        "w_pool_proj": f(inp["w_pool_proj"][L0:L1]), "mu_shift": f(inp["mu_shift"][L0:L1]),
        "decay_w2": f(inp["decay_w2"][L0:L1]), "a2": f(inp["a2"][L0:L1]),
        "vres_v1": v1, "vres_v2": v2,
        "w_rwkv_proj": f(inp["w_rwkv_proj"][L0:L1]), "w_out": f(inp["w_out"][L0:L1]),
        "norm_g": f(inp["norm_g"][L0:L1]), "lnx_w": f(inp["lnx_w"][L0:L1]), "lnx_b": f(inp["lnx_b"][L0:L1]),
        "final_g": f(inp["final_g"]).reshape(1, D), "pp": _pack_pp(inp, L0, L1),
    }


NO_BG = False
_PROG_CACHE = {}
STOP = None


class _Stop(Exception):
    pass


_CNT = [0]


_TILE = [0]


def _chk(name):
    if name == "T":
        _TILE[0] += 1
    if STOP == name or STOP == "%s@%d" % (name, _TILE[0]):
        raise _Stop()
    if name == "cnt" and STOP is not None and STOP.startswith("cnt"):
        _CNT[0] += 1
        if _CNT[0] >= int(STOP[3:]):
            raise _Stop()


def run_layers(inp, xs, L0, L1, NT, vfirst=None, cores=None):
    key = (L0, L1, NT)
    if key not in _PROG_CACHE:
        _PROG_CACHE[key] = build_program(L0, L1, NT, True)
    nc = _PROG_CACHE[key]
    shared = _layer_inputs(inp, L0, L1)
    in_maps = []
    for i, xc in enumerate(xs):
        m = dict(shared)
        m["x"] = np.ascontiguousarray(xc)
        if L0 > 0:
            m["vfirst_in"] = vfirst[i]
        in_maps.append(m)
    cores = list(range(len(xs))) if cores is None else cores
    import os
    if os.environ.get("KTRACE"):
        res = run_bass_kernel_spmd(nc, in_maps, core_ids=cores, trace=True)
        print("EXEC_TIME_NS", res.exec_time_ns, flush=True)
        try:
            print("TRACE", res.instructions_and_trace[1] if res.instructions_and_trace else None, flush=True)
        except Exception as ex:
            print("TRACE?", ex)
    else:
        res = run_bass_kernel_spmd(nc, in_maps, core_ids=cores)
    outs = [r["out"] for r in res.results]
    vf = [r["vfirst_out"] for r in res.results] if (L0 == 0 and L1 < DEPTH) else None
    return outs, vf


FUSED = True


def kernel(**inputs):
    x = np.asarray(inputs["x"], np.float32)
    xs = [np.ascontiguousarray(x[2 * i:2 * i + 2].reshape(NSEQ * SEQ, D)) for i in range(8)]
    if FUSED:
        outs, _ = run_layers(inputs, xs, 0, DEPTH, SEQ // T)
    else:
        outs, vf = run_layers(inputs, xs, 0, 1, SEQ // T)
        for L in range(1, DEPTH):
            outs, _ = run_layers(inputs, outs, L, L + 1, SEQ // T, vfirst=vf)
    return np.stack([o.reshape(NSEQ, SEQ, D) for o in outs], 0).reshape(16, SEQ, D).astype(np.float32)
```

```python
import numpy as np
from contextlib import ExitStack
import concourse.bass as bass
import concourse.mybir as mybir
from concourse.bass_utils import run_bass_kernel_spmd

F32 = mybir.dt.float32
BF16 = mybir.dt.bfloat16
AF = mybir.ActivationFunctionType
ALU = mybir.AluOpType
AX = mybir.AxisListType

D = 1024
SEQ = 2048
NSEQ = 2
DEPTH = 4
T = 512
NTC = 4
IN_W = 8320
NWL = 106
NORM_EPS = 1e-6
LNX_EPS = 1e-5 * 64
DECAY_C = 0.6065306597126334
POOL_WINDOWS = (2, 4, 8, 16)
NPAR = 8


class Buf:
    __slots__ = ("name", "lw", "rd", "const")

    def __init__(self, name, const=False):
        self.name = name
        self.lw = None
        self.rd = []
        self.const = const


class Sched:
    ENGS = ("pe", "act", "dve", "pool", "sp")

    def __init__(self, nc, stack, n_dsem=32, strict=("act", "dve", "pool")):
        self.nc = nc
        self.sem = {e: stack.enter_context(nc.semaphore("s_" + e)) for e in self.ENGS}
        self.dsem = [stack.enter_context(nc.semaphore("d%d" % i)) for i in range(n_dsem)]
        self.dval = [0] * n_dsem
        self.dnext = 0
        self.cnt = {e: 0 for e in self.ENGS}
        self.prog = {e: [] for e in self.ENGS}
        self.seen = {e: {} for e in self.ENGS}
        self.strict = set(strict)
        self.nwait = 0
        self.ninst = 0

    @staticmethod
    def _flat(x):
        out = []
        for b in x:
            if isinstance(b, (list, tuple)):
                out.extend(Sched._flat(b))
            elif b is not None:
                out.append(b)
        return out

    def _deps(self, eng, reads, writes):
        deps = set()
        for b in reads:
            if b.lw is not None:
                deps.add(b.lw)
        for b in writes:
            if b.lw is not None:
                deps.add(b.lw)
            for t in b.rd:
                deps.add(t)
        need = {}
        for (kind, ident, val) in deps:
            if kind == "e" and ident == eng and eng not in self.strict:
                continue
            key = (kind, ident)
            if self.seen[eng].get(key, 0) >= val:
                continue
            if need.get(key, 0) < val:
                need[key] = val
        for key, val in need.items():
            self.seen[eng][key] = val
            sem = self.sem[key[1]] if key[0] == "e" else self.dsem[key[1]]
            self.prog[eng].append(("w", sem, val))
            self.nwait += 1

    def _commit(self, tok, reads, writes):
        for b in writes:
            b.lw = tok
            b.rd = []
        for b in reads:
            if b.const or b in writes:
                continue
            b.rd.append(tok)
            if len(b.rd) > 400:
                b.rd = b.rd[-400:]

    def op(self, eng, fn, reads=(), writes=()):
        reads, writes = self._flat(reads), self._flat(writes)
        self._deps(eng, reads, writes)
        self.cnt[eng] += 1
        n = self.cnt[eng]
        self.prog[eng].append(("i", fn, self.sem[eng], 1))
        self._commit(("e", eng, n), reads, writes)
        self.ninst += 1

    def dma(self, q, out, in_, reads=(), writes=(), **kw):
        reads, writes = self._flat(reads), self._flat(writes)
        k = self.dnext
        self.dnext = (self.dnext + 1) % len(self.dsem)
        if self.dval[k] > 0 and self.seen[q].get(("d", k), 0) < self.dval[k]:
            self.seen[q][("d", k)] = self.dval[k]
            self.prog[q].append(("w", self.dsem[k], self.dval[k]))
        self._deps(q, reads, writes)
        self.dval[k] += 16
        v = self.dval[k]
        self.prog[q].append(("i", lambda e: e.dma_start(out=out, in_=in_, **kw), self.dsem[k], 16))
        self._commit(("d", k, v), reads, writes)
        self.ninst += 1

    def barrier(self):
        for e in self.ENGS:
            for p in self.ENGS:
                if p == e or self.cnt[p] == 0:
                    continue
                if self.seen[e].get(("e", p), 0) < self.cnt[p]:
                    self.seen[e][("e", p)] = self.cnt[p]
                    self.prog[e].append(("w", self.sem[p], self.cnt[p]))
            for k, v in enumerate(self.dval):
                if v > 0 and self.seen[e].get(("d", k), 0) < v:
                    self.seen[e][("d", k)] = v
                    self.prog[e].append(("w", self.dsem[k], v))

    def finish(self, q="sp"):
        for k, v in enumerate(self.dval):
            if v > 0:
                self.prog[q].append(("w", self.dsem[k], v))

    def run(self, block):
        def body(prog):
            def f(e):
                for it in prog:
                    if it[0] == "w":
                        e.wait_ge(it[1], it[2])
                    else:
                        it[1](e).then_inc(it[2], it[3])
            return f
        block.tensor(body(self.prog["pe"]))
        block.scalar(body(self.prog["act"]))
        block.vector(body(self.prog["dve"]))
        block.gpsimd(body(self.prog["pool"]))
        block.sync(body(self.prog["sp"]))

    def mm(self, out, lhsT, rhs, start, stop, R, W):
        self.op("pe", lambda e: e.matmul(out, lhsT, rhs, start=start, stop=stop), R, W)

    def tr(self, out, in_, ident, R, W):
        self.op("pe", lambda e: e.transpose(out, in_, ident), R, W)

    def act(self, out, in_, func, R, W, bias=None, scale=None, accum_out=None):
        kw = {}
        if bias is not None:
            kw["bias"] = bias
        if scale is not None:
            kw["scale"] = scale
        if accum_out is not None:
            kw["accum_out"] = accum_out
        self.op("act", lambda e: e.activation(out=out, in_=in_, func=func, **kw), R, W)

    def tt(self, eng, out, in0, in1, op, R, W):
        self.op(eng, lambda e: e.tensor_tensor(out=out, in0=in0, in1=in1, op=op), R, W)

    def ts(self, eng, out, in0, s1, op0, R, W, s2=None, op1=None):
        if op1 is None:
            self.op(eng, lambda e: e.tensor_scalar(out=out, in0=in0, scalar1=s1, scalar2=None, op0=op0), R, W)
        else:
            self.op(eng, lambda e: e.tensor_scalar(out=out, in0=in0, scalar1=s1, scalar2=s2, op0=op0, op1=op1), R, W)

    def stt(self, out, in0, scalar, in1, op0, op1, R, W):
        self.op("dve", lambda e: e.scalar_tensor_tensor(out=out, in0=in0, scalar=scalar, in1=in1, op0=op0, op1=op1), R, W)

    def copy(self, eng, out, in_, R, W):
        if eng == "act":
            self.op("act", lambda e: e.activation(out=out, in_=in_, func=AF.Copy), R, W)
        else:
            self.op(eng, lambda e: e.tensor_copy(out=out, in_=in_), R, W)

    def memset(self, eng, ap, val, W):
        self.op(eng, lambda e: e.memset(ap, val), (), W)


def build_program(L0, L1, NT, final_norm):
    NL = L1 - L0
    NTOK = NSEQ * SEQ
    nc = bass.Bass("TRN2", target_bir_lowering=False, dynamic_dma_scratch_size=2048)
    dt_in = lambda name, shape: nc.dram_tensor(name, shape, F32, kind="ExternalInput").ap()
    x_in = dt_in("x", [NTOK, D])
    w_in = dt_in("w_in", [NL, D, IN_W])
    pool_lin = dt_in("pool_lin", [NL, 4, 256, 256])
    w_pool_proj = dt_in("w_pool_proj", [NL, D, D])
    mu_shift = dt_in("mu_shift", [NL, 3200])
    decay_w2 = dt_in("decay_w2", [NL, 64, D])
    a2 = dt_in("a2", [NL, 64, D])
    vres_v1 = dt_in("vres_v1", [NL, D, 32])
    vres_v2 = dt_in("vres_v2", [NL, 32, D])
    w_rwkv_proj = dt_in("w_rwkv_proj", [NL, D, D])
    w_out = dt_in("w_out", [NL, D, D])
    norm_g = dt_in("norm_g", [NL, D])
    lnx_w = dt_in("lnx_w", [NL, D])
    lnx_b = dt_in("lnx_b", [NL, D])
    final_g = dt_in("final_g", [1, D])
    pp_in = dt_in("pp", [NL, 128, NPAR * 8])
    out_d = nc.dram_tensor("out", [NTOK, D], F32, kind="ExternalOutput").ap()
    NVT = NSEQ * NT
    if L0 == 0 and L1 < DEPTH:
        vfd = nc.dram_tensor("vfirst_out", [NVT, 128, 8 * T], F32, kind="ExternalOutput").ap()
    elif L0 == 0:
        vfd = nc.dram_tensor("vfirst_scr", [NVT, 128, 8 * T], F32, kind="Internal").ap()
    else:
        vfd = dt_in("vfirst_in", [NVT, 128, 8 * T])
    xbufs = [nc.dram_tensor("xbuf%d" % i, [NTOK, D], F32, kind="Internal").ap() for i in range(2)] if NL > 1 else []
    WLd = nc.dram_tensor("wl_scr", [NL, NWL, 128, 1024], BF16, kind="Internal").ap()
    WRd = nc.dram_tensor("wr_scr", [NL, 4, 128, 4096], BF16, kind="Internal").ap()
    B_vfd = Buf("vfd")
    B_xb = [Buf("xb0"), Buf("xb1")]
    B_WLd = [[Buf("wld") for _ in range(NWL)] for _ in range(NL)]
    B_WRd = [[Buf("wrd") for _ in range(4)] for _ in range(NL)]

    with ExitStack() as st:
        S = Sched(nc, st)

        def mk(stack, name, shape, dt, nb=0):
            t = stack.enter_context(nc.sbuf_tensor(name, shape, dt))
            if nb:
                return t, [Buf("%s%d" % (name, i)) for i in range(nb)]
            return t, Buf(name)

        with ExitStack() as pst:
            NPB = 4
            STG = [mk(pst, "stg%d" % i, [128, 8, 512], F32) for i in range(NPB)]
            CB = [mk(pst, "cb%d" % i, [128, 4, 1024], BF16) for i in range(NPB)]
            CB2 = [mk(pst, "cbp%d" % i, [128, 4, 1024], BF16) for i in range(NPB)]
            MU, B_MU = mk(pst, "mu_bc", [128, 3200], F32)
            MU1, B_MU1 = mk(pst, "mu1_bc", [128, 3200], F32)
            cast_engs = ["pool", "dve", "act"]
            ce = [0]

            def cast(out, in_, R, W):
                e = cast_engs[ce[0] % 3]
                ce[0] += 1
                S.copy(e, out, in_, R, W)

            blk = [0]
            for li in range(NL):
                S.dma("sp", MU[:], mu_shift[li:li + 1, :].to_broadcast([128, 3200]), (), [B_MU])
                S.ts("pool", MU1[:], MU[:], -1.0, ALU.mult, [B_MU], [B_MU1], s2=1.0, op1=ALU.add)
                segs = []
                for c0 in list(range(0, 5248, 512)):
                    segs.append((w_in[li], c0, min(512, 5248 - c0), "L", c0 // 128))
                for c0 in range(6272, 8320, 512):
                    segs.append((w_in[li], c0, 512, "L", c0 // 128))
                segs.append((w_in[li], 5248, 512, "R", 0))
                segs.append((w_in[li], 5760, 512, "R", 1))
                for c0 in (0, 512):
                    segs.append((w_pool_proj[li], c0, 512, "L", 90 + c0 // 128))
                for c0 in (0, 512):
                    segs.append((w_rwkv_proj[li], c0, 512, "L", 98 + c0 // 128))
                segs.append((w_out[li], 0, 512, "R", 2))
                segs.append((w_out[li], 512, 512, "R", 3))
                for (src, c0, ncol, kind, base) in segs:
                    b = blk[0] % NPB
                    blk[0] += 1
                    stg, B_stg = STG[b]
                    cb, B_cb = CB[b]
                    cb2, B_cb2 = CB2[b]
                    S.dma("sp", stg[:, :, 0:ncol], src[:, c0:c0 + ncol].rearrange("(kc kp) c -> kp kc c", kp=128),
                          (), [B_stg])
                    if kind == "R":
                        cbr = cb[:].rearrange("p a f -> p (a f)").rearrange("p (kc j) -> p kc j", j=512)
                        for kc in range(8):
                            cast(cbr[:, kc, :], stg[:, kc, :], [B_stg], [B_cb])
                        S.dma("act", WRd[li, base], cb[:].rearrange("p a f -> p (a f)"), [B_cb], [B_WRd[li][base]])
                        continue
                    nm = ncol // 128
                    for mi in range(nm):
                        m = base + mi
                        o = cb[:, mi, :].rearrange("p (kc j) -> p kc j", j=128)
                        i_ = stg[:, :, mi * 128:(mi + 1) * 128]
                        is_shift = (base < 90) and (16 <= m <= 40)
                        if is_shift:
                            mc = (m - 16) * 128
                            S.tt("pool" if mi % 2 == 0 else "dve", o, i_,
                                 MU1[:, mc:mc + 128].unsqueeze(1).to_broadcast([128, 8, 128]), ALU.mult,
                                 [B_stg, B_MU1], [B_cb])
                            o2 = cb2[:, mi, :].rearrange("p (kc j) -> p kc j", j=128)
                            S.tt("dve" if mi % 2 == 0 else "pool", o2, i_,
                                 MU[:, mc:mc + 128].unsqueeze(1).to_broadcast([128, 8, 128]), ALU.mult,
                                 [B_stg, B_MU], [B_cb2])
                            S.dma("act", WLd[li, 65 + m - 16], cb2[:, mi, :], [B_cb2], [B_WLd[li][65 + m - 16]])
                        else:
                            cast(o, i_, [B_stg], [B_cb])
                        S.dma("act", WLd[li, m], cb[:, mi, :], [B_cb], [B_WLd[li][m]])
        S.barrier()
        try:
            _chk("prepass")
            _main(locals())
        except _Stop:
            pass
        S.finish("sp")
        print("program: %d instructions, %d waits" % (S.ninst, S.nwait), {e: S.cnt[e] for e in S.ENGS}, flush=True)
        with nc.Block() as block:
            S.run(block)
    return nc


def _main(env):
    globals_ = env
    nc = env["nc"]; st = env["st"]; S = env["S"]; mk = env["mk"]
    NL = env["NL"]; L0 = env["L0"]; L1 = env["L1"]; NT = env["NT"]; final_norm = env["final_norm"]
    x_in = env["x_in"]; out_d = env["out_d"]; vfd = env["vfd"]; xbufs = env["xbufs"]
    WLd = env["WLd"]; WRd = env["WRd"]; B_vfd = env["B_vfd"]; B_xb = env["B_xb"]; B_WLd = env["B_WLd"]; B_WRd = env["B_WRd"]
    pool_lin = env["pool_lin"]; decay_w2 = env["decay_w2"]; a2 = env["a2"]; vres_v1 = env["vres_v1"]; vres_v2 = env["vres_v2"]
    norm_g = env["norm_g"]; lnx_w = env["lnx_w"]; lnx_b = env["lnx_b"]; final_g = env["final_g"]; pp_in = env["pp_in"]
    if True:
        IDENT, B_IDENT = mk(st, "ident", [128, 128], BF16)
        MASKT, B_MASKT = mk(st, "maskt", [128, 512], BF16)
        MASKL, B_MASKL = mk(st, "maskl", [128, 128], BF16)
        BONES, B_BONES = mk(st, "bones", [128, 128], BF16)
        E2, B_E2 = mk(st, "e2", [128, 2], BF16)
        ONESF, B_ONESF = mk(st, "onesf", [128, 128], F32)
        CORR, B_CORR = mk(st, "corr", [128, 4, 16], F32)
        EPSC, B_EPSC = mk(st, "epsc", [128, 2], F32)
        NEGH, B_NEGH = mk(st, "negh", [128, 8], F32)
        B_NEGH.const = True
        for b_ in (B_IDENT, B_MASKT, B_MASKL, B_BONES, B_E2, B_ONESF, B_CORR, B_EPSC):
            b_.const = True
        G_BC, B_G = mk(st, "g_bc", [128, D], F32)
        LNW, B_LNW = mk(st, "lnw_bc", [128, D], F32)
        LNB, B_LNB = mk(st, "lnb_bc", [128, D], F32)
        FG, B_FG = mk(st, "fg_bc", [128, D], F32)
        PP, B_PP = mk(st, "pp_sb", [128, NPAR, 8], F32)
        PL, B_PL = mk(st, "pl", [128, 4, 2, 256], BF16)
        DAZ, B_DA = mk(st, "daz", [128, 2, D], BF16)
        V1W, B_V1W = mk(st, "v1w", [128, 8, 128], BF16)
        V2W, B_V2W = mk(st, "v2w", [128, D], BF16)
        HT, B_HT = mk(st, "ht", [128, 8, T + 1], BF16)
        MP, B_MP = mk(st, "mp", [128, 8, T], BF16)
        YGT, B_YGT = mk(st, "ygt", [128, 8, T], BF16)
        XIN = [mk(st, "xin%d" % i, [128, D], F32) for i in range(2)]
        HN, B_HN = mk(st, "hn", [128, D], BF16)
        JUNK, B_JUNK = HN, B_HN
        STAT, B_STAT = mk(st, "stat", [128, 8], F32)
        NRL = 7
        WLB = [mk(st, "wlb%d" % i, [128, 1024], BF16) for i in range(NRL)]
        WRB = [mk(st, "wrb%d" % i, [128, 4096], BF16) for i in range(2)]
        NTMP = 14
        TMPALL, B_TMP = mk(st, "tmpall", [128, NTMP, T + 16], F32, nb=NTMP)
        TMP = [(TMPALL[:, i, 0:T], B_TMP[i]) for i in range(NTMP)]
        SG, B_SG = TMPALL[:, 8:10, 0:T], [B_TMP[8], B_TMP[9]]
        SGM, B_SGM = TMP[10]
        CTMP, B_CTMP = TMP[9]
        DG, B_DG = mk(st, "dg", [128, 2, T], BF16)
        HALO_P, B_HALOP = mk(st, "halop", [128, 8, 16], F32)
        VF, B_VF = mk(st, "vf", [128, 8, T], F32, nb=8)
        STGS, B_STGS = VF[:].rearrange("p c t -> p (c t)")[:, 0:2048], B_VF[0:4]
        VFf = VF[:].rearrange("p c t -> p (c t)")
        UG, B_UG = VFf[:, 0:1056].rearrange("p (a b) -> p a b", a=2), Buf("ug")
        SA, B_SA = VFf[:, 1056:2112].rearrange("p (a b) -> p a b", a=2), Buf("sa")
        SBb, B_SBb = VFf[:, 2112:3168].rearrange("p (a b) -> p a b", a=2), Buf("sbb")
        VB, B_VB = mk(st, "vb", [128, 8, T], BF16, nb=8)
        VT, B_VT = mk(st, "vt", [128, NTC, D], BF16, nb=NTC)
        TWA, B_TWA = mk(st, "twa", [128, T], BF16)
        P1, B_P1 = mk(st, "p1", [128, T], BF16)
        BTZ, B_BT = mk(st, "btz", [128, 2, 4, T], BF16, nb=4)
        KTZ, B_KT = mk(st, "ktz", [128, 2, 4, T], BF16, nb=4)
        AR, B_AR = mk(st, "ar", [128, 4, NTC, 256], BF16, nb=4)
        BTT, B_BTT = mk(st, "btt", [128, NTC, 512], BF16, nb=NTC)
        KTT, B_KTT = mk(st, "ktt", [128, NTC, 512], BF16, nb=NTC)
        RKB2, B_RKB2 = mk(st, "rkb2", [128, 2, T], BF16, nb=2)
        EWC, B_EWC = mk(st, "ewc", [128, 4, NTC], F32)
        BDOT, B_BDOT = mk(st, "bdot", [128, NTC, 16], F32)
        SC, B_SC = mk(st, "sc", [128, 8, 512], BF16, nb=8)
        PK = [mk(st, "pk%d" % i, [128, 2, 512], BF16, nb=2) for i in range(2)]
        QK = [mk(st, "qk%d" % i, [128, 2, 512], BF16, nb=2) for i in range(2)]
        SK, B_SK = mk(st, "sk", [128, 2, 512], BF16, nb=2)
        SCXv = TMPALL[:, 3:7, :].rearrange("p a b -> p (a b)").bitcast(BF16)[:, 0:4096].rearrange("p (h f) -> p h f", f=512)
        B_SCX = [Buf("scx%d" % i) for i in range(8)]
        SKXv = TMPALL[:, 7, :].bitcast(BF16)[:, 0:1024].rearrange("p (q f) -> p q f", f=512)
        B_SKX = [Buf("skx%d" % i) for i in range(2)]
        ALIAS_TMP = [B_TMP[i] for i in range(3, 8)]
        ALIAS_SCAN = B_SCX + B_SKX

        def handoff(src, dst):
            toks = set()
            for b_ in src:
                if b_.lw is not None:
                    toks.add(b_.lw)
                toks.update(b_.rd)
            for d_ in dst:
                d_.rd = list(set(d_.rd) | toks)

        def sc_slot(slot):
            if slot < 2:
                return SC[:, slot * 4:(slot + 1) * 4, :], B_SC[slot * 4:(slot + 1) * 4]
            return SCXv[:, (slot - 2) * 4:(slot - 1) * 4, :], B_SCX[(slot - 2) * 4:(slot - 1) * 4]

        def sk_slot(slot):
            if slot < 2:
                return SK[:, slot, :], B_SK[slot]
            return SKXv[:, slot - 2, :], B_SKX[slot - 2]

        def pq_slot(slot):
            i_, j_ = slot // 2, slot % 2
            return (PK[i_][0][:, j_, :], PK[i_][1][j_]), (QK[i_][0][:, j_, :], QK[i_][1][j_])

        XB, B_XB = mk(st, "xbv", [128, 512], BF16, nb=2)
        UB, B_UB = mk(st, "ubv", [128, 512], BF16, nb=2)
        HS, B_HS = mk(st, "hs", [128, 8, 64], F32, nb=4)
        HBZ, B_HB = mk(st, "hbz", [128, 8, 128], BF16, nb=4)
        YA, B_YA = TMP[0][0], [TMP[0][1], Buf("ya1")]
        YB, B_YB = TMP[1][0], [TMP[1][1], Buf("yb1")]
        SGT, B_SGT = TMP[2]
        YG, B_YG = mk(st, "yg", [128, 512], BF16, nb=2)
        ST8, B_ST8 = mk(st, "st8", [128, 6, 8], F32, nb=2)
        MG, B_MG = VB, B_VB
        YPI, B_YPI = VB, B_VB
        PSB = []
        for i in range(8):
            t_ = st.enter_context(nc.psum_tensor("ps%d" % i, [128, 512], F32))
            PSB.append((t_, Buf("ps%d" % i)))
        PTB = [(PSB[6][0][:].bitcast(BF16), PSB[6][1]), (PSB[7][0][:].bitcast(BF16), PSB[7][1])]
        ring = [0, 0, 0, 0, 0, 0]

        def ps():
            r = PSB[ring[0] % 5]
            ring[0] += 1
            return r

        PBD, B_PBD = PSB[5]

        def pt():
            r = PTB[ring[1] % 2]
            ring[1] += 1
            return r

        def psL():
            r = PSB[ring[4] % 4]
            ring[4] += 1
            return r

        pP_i = [0]

        def pP():
            r = PSB[6 + pP_i[0] % 2]
            pP_i[0] += 1
            return r

        PT4 = (PSB[4][0][:].bitcast(BF16), PSB[4][1])

        def psS():
            r = PSB[(4, 6, 7)[ring[5] % 3]]
            ring[5] += 1
            return r

        def tmp():
            r = TMP[ring[3] % NTMP]
            ring[3] += 1
            return r

        def tri(dst_ap, cmp_op, pattern_step, cm):
            S.memset("pool", CTMP[:, 0:128], 1.0, [B_CTMP])
            S.op("pool", lambda e: e.affine_select(out=CTMP[:, 0:128], in_=CTMP[:, 0:128], pattern=[[pattern_step, 128]],
                                                   compare_op=cmp_op, fill=0.0, base=0, channel_multiplier=cm),
                 [B_CTMP], [B_CTMP])
            S.copy("pool", dst_ap, CTMP[:, 0:128], [B_CTMP], [B_IDENT])

        tri(IDENT[:], ALU.is_equal, -1, 1)
        tri(MASKL[:], ALU.is_gt, -1, 1)
        tri(MASKT[:, 0:128], ALU.is_gt, 1, -1)
        tri(MASKT[:, 128:256], ALU.is_ge, 1, -1)
        tri(MASKT[:, 256:384], ALU.is_gt, 1, -1)
        tri(MASKT[:, 384:512], ALU.is_ge, 1, -1)
        S.memset("pool", BONES[:], 0.0, [B_IDENT])
        S.memset("pool", BONES[0:64, 0:64], 1.0, [B_IDENT])
        S.memset("pool", BONES[64:128, 64:128], 1.0, [B_IDENT])
        S.memset("pool", E2[:], 0.0, [B_IDENT])
        S.memset("pool", E2[0:64, 0:1], 1.0, [B_IDENT])
        S.memset("pool", E2[64:128, 1:2], 1.0, [B_IDENT])
        S.memset("pool", ONESF[:], 1.0, [B_IDENT])
        S.memset("pool", CORR[:], 1.0, [B_IDENT])
        for g, w in enumerate(POOL_WINDOWS):
            for t_ in range(w - 1):
                S.memset("pool", CORR[:, g, t_:t_ + 1], float(w) / float(t_ + 1), [B_IDENT])
        S.memset("pool", BTZ[:], 0.0, B_BT)
        S.memset("pool", KTZ[:], 0.0, B_KT)
        S.memset("pool", HBZ[:], 0.0, B_HB)
        S.memset("pool", DAZ[:], 0.0, [B_DA])
        S.memset("pool", V1W[:], 0.0, [B_V1W])
        S.memset("pool", V2W[:], 0.0, [B_V2W])
        S.memset("pool", NEGH[:], -0.5, [B_NEGH])
        S.memset("pool", EPSC[:, 0:1], NORM_EPS, [B_IDENT])
        S.memset("pool", EPSC[:, 1:2], LNX_EPS, [B_IDENT])
        S.dma("sp", FG[:], final_g.to_broadcast([128, D]), (), [B_FG])
        _chk("consts")

        def wl(li, idx):
            t_, b_ = WLB[ring[2] % NRL]
            ring[2] += 1
            S.dma("sp", t_[:], WLd[li, idx], [B_WLd[li][idx]], [b_])
            return t_, b_

        wr_i = [0]

        def wr(li, idx):
            t_, b_ = WRB[wr_i[0] % 2]
            wr_i[0] += 1
            S.dma("sp", t_[:], WRd[li, idx], [B_WRd[li][idx]], [b_])
            return t_, b_

        def inproj(li, m, shift, alloc=None):
            pt_, pb_ = (alloc or ps)()
            w_, wb_ = wl(li, m)
            for kc in range(8):
                S.mm(pt_[:], w_[:, kc * 128:(kc + 1) * 128], HT[:, kc, 1:T + 1], kc == 0, (kc == 7 and not shift),
                     [wb_, B_HT], [pb_])
            if shift:
                w2, wb2 = wl(li, 65 + m - 16)
                for kc in range(8):
                    S.mm(pt_[:], w2[:, kc * 128:(kc + 1) * 128], HT[:, kc, 0:T], False, kc == 7,
                         [wb2, B_HT], [pb_])
            return pt_, pb_

        def inproj_g(li, m, alloc, step=2):
            pt_, pb_ = alloc()
            w_, wb_ = wl(li, m)
            for kc in range(8):
                S.mm(pt_[:], w_[:, kc * 128:(kc + 1) * 128], HT[:, kc, 1:T + 1], kc == 0, kc == 7, [wb_, B_HT], [pb_])
                if kc % step == step - 1 and kc != 7:
                    yield
            return pt_, pb_

        for li in range(NL):
            L = L0 + li
            last = (L == DEPTH - 1) and final_norm
            x_src, B_xs = (x_in, None) if li == 0 else (xbufs[(li - 1) % 2], B_xb[(li - 1) % 2])
            x_dst, B_xd = (out_d, None) if li == NL - 1 else (xbufs[li % 2], B_xb[li % 2])
            Rxs = [B_xs] if B_xs is not None else []
            Wxd = [B_xd] if B_xd is not None else []
            S.dma("sp", G_BC[:], norm_g[li:li + 1, :].to_broadcast([128, D]), (), [B_G])
            S.dma("sp", LNW[:], lnx_w[li:li + 1, :].to_broadcast([128, D]), (), [B_LNW])
            S.dma("sp", LNB[:], lnx_b[li:li + 1, :].to_broadcast([128, D]), (), [B_LNB])
            S.dma("sp", PP[:].rearrange("p k c -> p (k c)"), pp_in[li], (), [B_PP])
            S.ts("pool", PP[:, 7, :], PP[:, 5, :], -1.0, ALU.mult, [B_PP], [B_PP], s2=1.0, op1=ALU.add)
            S.dma("sp", STGS[:].rearrange("p (g kc d) -> p g kc d", g=4, kc=2),
                  pool_lin[li].rearrange("g (kc kp) d -> kp g kc d", kp=128), (), [B_STGS])
            S.copy("pool", PL[:].rearrange("p g kc d -> p (g kc d)"), STGS[:], [B_STGS], [B_PL])
            S.dma("sp", STGS[0:64, 0:D], decay_w2[li], [], [B_STGS])
            S.dma("sp", STGS[64:128, 0:D], a2[li], [], [B_STGS])
            S.copy("pool", DAZ[0:64, 0, :], STGS[0:64, 0:D], [B_STGS], [B_DA])
            S.copy("pool", DAZ[64:128, 1, :], STGS[64:128, 0:D], [B_STGS], [B_DA])
            if L > 0:
                S.dma("sp", STGS[:, 0:256].rearrange("p (kc j) -> p kc j", j=32),
                      vres_v1[li].rearrange("(kc kp) j -> kp kc j", kp=128), [], [B_STGS])
                S.copy("pool", V1W[:, :, 0:32], STGS[:, 0:256].rearrange("p (kc j) -> p kc j", j=32), [B_STGS], [B_V1W])
                S.dma("sp", STGS[0:32, 0:D], vres_v2[li], [], [B_STGS])
                S.copy("pool", V2W[0:32, :], STGS[0:32, 0:D], [B_STGS], [B_V2W])
            ppc = lambda k, c: PP[:, k, c:c + 1]
            _chk("params")

            for s in range(NSEQ):
                for ti in range(NT):
                    row0 = s * SEQ + ti * T
                    vt_idx = s * NT + ti
                    first = (ti == 0)
                    if first:
                        S.memset("pool", HT[:, :, 0:1], 0.0, [B_HT])
                    else:
                        S.copy("pool", HT[:, :, 0:1], HT[:, :, T:T + 1], [B_HT], [B_HT])
                    for tg in range(4):
                        xi, B_xi = XIN[tg % 2]
                        S.dma("sp", xi[:], x_src[row0 + tg * 128: row0 + (tg + 1) * 128, :], Rxs, [B_xi])
                        S.act(JUNK[:], xi[:], AF.Square, [B_xi], [B_JUNK, B_STAT], accum_out=STAT[:, 0:1])
                        S.ts("pool", STAT[:, 1:2], STAT[:, 0:1], 1.0 / D, ALU.mult, [B_STAT], [B_STAT], s2=NORM_EPS, op1=ALU.add)
                        S.tt("pool", STAT[:, 2:3], STAT[:, 1:2], NEGH[:, 0:1], ALU.pow, [B_STAT, B_NEGH], [B_STAT])
                        S.stt(HN[:], xi[:], STAT[:, 2:3], G_BC[:], ALU.mult, ALU.mult, [B_xi, B_STAT, B_G], [B_HN])
                        ptt, ptb = pt()
                        for kc in range(8):
                            S.tr(ptt[:, kc * 128:(kc + 1) * 128], HN[:, kc * 128:(kc + 1) * 128], IDENT[:],
                                 [B_HN, B_IDENT], [ptb])
                        S.copy("act", HT[:, :, 1 + tg * 128: 1 + (tg + 1) * 128],
                               ptt[:].rearrange("p (kc j) -> p kc j", j=128), [ptb], [B_HT])

                    _chk("N")
                    def pool_p1(li=li, first=first):
                        handoff(B_VF[0:7], [B_UG, B_SA, B_SBb])
                        if first:
                            S.memset("pool", HALO_P[:], 0.0, [B_HALOP])
                        for g, w in enumerate(POOL_WINDOWS):
                            for j in range(2):
                                p_, pb_ = yield from inproj_g(li, 2 * g + j, pP)
                                S.copy("act", UG[:, j, 16:16 + T], p_[:], [pb_], [B_UG])
                                yield
                            for j in range(2):
                                p_, pb_ = yield from inproj_g(li, 8 + 2 * g + j, pP)
                                S.act(SG[:, j, :], p_[:], AF.Silu, [pb_], [B_SG])
                                yield
                            S.copy("pool", UG[:, :, 0:16], HALO_P[:, 2 * g:2 * g + 2, :], [B_HALOP], [B_UG])
                            S.copy("pool", HALO_P[:, 2 * g:2 * g + 2, :], UG[:, :, T:T + 16], [B_UG], [B_HALOP])
                            NJ = T + 16
                            S.tt("pool", SA[:, :, 1:NJ], UG[:, :, 1:NJ], UG[:, :, 0:NJ - 1], ALU.add, [B_UG], [B_SA])
                            cur, B_cur = SA, B_SA
                            if w >= 4:
                                S.tt("pool", SBb[:, :, 3:NJ], SA[:, :, 3:NJ], SA[:, :, 1:NJ - 2], ALU.add, [B_SA], [B_SBb])
                                cur, B_cur = SBb, B_SBb
                            yield
                            if w >= 8:
                                S.tt("pool", SA[:, :, 7:NJ], SBb[:, :, 7:NJ], SBb[:, :, 3:NJ - 4], ALU.add, [B_SBb], [B_SA])
                                cur, B_cur = SA, B_SA
                            if w >= 16:
                                S.tt("pool", SBb[:, :, 15:NJ], SA[:, :, 15:NJ], SA[:, :, 7:NJ - 8], ALU.add, [B_SA], [B_SBb])
                                cur, B_cur = SBb, B_SBb
                            if first:
                                S.tt("pool", cur[:, :, 16:32], cur[:, :, 16:32],
                                     CORR[:, g, :].unsqueeze(1).to_broadcast([128, 2, 16]), ALU.mult,
                                     [B_cur, B_CORR], [B_cur])
                            S.stt(DG[:], cur[:, :, 16:16 + T], 1.0 / w, UG[:, :, 16:16 + T], ALU.mult, ALU.subtract,
                                  [B_cur, B_UG], [B_DG])
                            yield
                            for mo in range(2):
                                p_, pb_ = pP()
                                for kc in range(2):
                                    S.mm(p_[:], PL[:, g, kc, mo * 128:(mo + 1) * 128], DG[:, kc, :], kc == 0, kc == 1,
                                         [B_PL, B_DG], [pb_])
                                S.stt(YPI[:, 2 * g + mo, :], p_[:], ppc(0, 2 * g + mo), SG[:, mo, :], ALU.mult, ALU.mult,
                                      [pb_, B_PP, B_SG], [B_YPI[2 * g + mo]])
                            yield
                        handoff([B_UG, B_SA, B_SBb], B_VF[0:7])

                    def pool_p2(li=li):
                        for m in range(8):
                            py_, pyb_ = pP()
                            w_, wb_ = wl(li, 90 + m)
                            for kc in range(8):
                                S.mm(py_[:], w_[:, kc * 128:(kc + 1) * 128], YPI[:, kc, :], kc == 0, kc == 7,
                                     [wb_, B_YPI[kc]], [pyb_])
                                if kc % 2 == 1:
                                    yield
                            pg_, pgb_ = yield from inproj_g(li, 49 + m, pP)
                            S.act(SGM[:], pg_[:], AF.Sigmoid, [pgb_], [B_SGM])
                            S.tt("dve", MP[:, m, :], py_[:], SGM[:], ALU.mult, [pyb_, B_SGM], [B_MP])
                            yield

                    bg_gens = [pool_p1(), pool_p2()]

                    _chk("P")
                    p_, pb_ = inproj(li, 40, True)
                    S.act(TWA[0:64, :], p_[0:64, :], AF.Tanh, [pb_], [B_TWA])
                    S.copy("act", TWA[64:128, :], p_[64:128, :], [pb_], [B_TWA])
                    for c in range(8):
                        p_, pb_ = inproj(li, 32 + c, True)
                        S.copy("act", VF[:, c, :], p_[:], [pb_], [B_VF[c]])
                    if L == 0:
                        S.dma("act", vfd[vt_idx], VF[:].rearrange("p c t -> p (c t)"), B_VF, [B_vfd])
                    else:
                        for c in range(8):
                            S.copy("dve" if c % 2 == 0 else "act", VB[:, c, :], VF[:, c, :], [B_VF[c]], [B_VB[c]])
                        p1_, p1b_ = ps()
                        for kc in range(8):
                            S.mm(p1_[:], V1W[:, kc, :], VB[:, kc, :], kc == 0, kc == 7, [B_V1W, B_VB[kc]], [p1b_])
                        S.copy("act", P1[:], p1_[:], [p1b_], [B_P1])
                        for c in range(8):
                            p_, pb_ = ps()
                            S.mm(p_[:], V2W[:, c * 128:(c + 1) * 128], P1[:], True, True, [B_V2W, B_P1], [pb_])
                            mx, B_mx = tmp()
                            S.act(mx[:], p_[:], AF.Sigmoid, [pb_, B_PP], [B_mx], bias=ppc(3, c))
                            vf1, B_vf1 = tmp()
                            S.dma("sp", vf1[:], vfd[vt_idx, :, c * T:(c + 1) * T], [B_vfd], [B_vf1])
                            S.tt("pool", vf1[:], vf1[:], VF[:, c, :], ALU.subtract, [B_vf1, B_VF[c]], [B_vf1])
                            S.tt("pool", vf1[:], vf1[:], mx[:], ALU.mult, [B_vf1, B_mx], [B_vf1])
                            S.tt("dve", VF[:, c, :], VF[:, c, :], vf1[:], ALU.add, [B_VF[c], B_vf1], [B_VF[c]])
                    for c in range(8):
                        S.copy("dve" if c % 2 == 0 else "act", VB[:, c, :], VF[:, c, :], [B_VF[c]], [B_VB[c]])
                    for tc in range(NTC):
                        ptt, ptb = pt()
                        for c in range(8):
                            S.tr(ptt[:, c * 128:(c + 1) * 128], VB[:, c, tc * 128:(tc + 1) * 128], IDENT[:],
                                 [B_VB[c], B_IDENT], [ptb])
                        S.copy("act", VT[:, tc, :], ptt[:], [ptb], [B_VT[tc]])

                    _chk("R0")
                    for half in range(2):
                        def prep_c(cl, half=half):
                            c = half * 4 + cl
                            st_ = (cl % 2) * 7
                            sl = [TMP[st_ + i] for i in range(7)]
                            RK, B_RK = RKB2[:, cl % 2, :], B_RKB2[cl % 2]
                            pr_, prb_ = inproj(li, 16 + c, True, psL)
                            yield
                            pk_, pkb_ = inproj(li, 24 + c, True, psL)
                            yield
                            pd_, pdb_ = psS()
                            S.mm(pd_[:], DAZ[:, 0, c * 128:(c + 1) * 128], TWA[:], True, True, [B_DA, B_TWA], [pdb_])
                            sgd, B_sgd = sl[0]
                            S.act(sgd[:], pd_[:], AF.Sigmoid, [pdb_, B_PP], [B_sgd], bias=ppc(1, c))
                            pa_, pab_ = psS()
                            S.mm(pa_[:], DAZ[:, 1, c * 128:(c + 1) * 128], TWA[:], True, True, [B_DA, B_TWA], [pab_])
                            ag, B_ag = sl[1]
                            S.act(ag[:], pa_[:], AF.Sigmoid, [pab_, B_PP], [B_ag], bias=ppc(2, c))
                            kkr, B_kkr = sl[5]
                            S.ts("dve", kkr[:], pk_[:], ppc(4, c), ALU.mult, [pkb_, B_PP], [B_kkr])
                            S.tt("dve", RK, kkr[:], kkr[:], ALU.mult, [B_kkr], [B_RK])
                            pn_, pnb_ = psS()
                            S.mm(pn_[:], BONES[:], RK, True, True, [B_BONES, B_RK], [pnb_])
                            nrm, B_nrm = sl[6]
                            S.act(nrm[:], pn_[:], AF.Sqrt, [pnb_], [B_nrm])
                            yield
                            cs, B_cs = sl[2]
                            for tc in range(NTC):
                                S.op("dve", lambda e, cs=cs, sgd=sgd, tc=tc: e.tensor_tensor_scan(
                                    out=cs[:, tc * 128:(tc + 1) * 128], data0=ONESF[:], data1=sgd[:, tc * 128:(tc + 1) * 128],
                                    initial=0.0, op0=ALU.mult, op1=ALU.add), [B_ONESF, B_sgd], [B_cs])
                            csp, B_csp = sl[3]
                            S.tt("pool", csp[:], cs[:], sgd[:], ALU.subtract, [B_cs, B_sgd], [B_csp])
                            yield
                            S.ts("dve", nrm[:], nrm[:], 1e-12, ALU.max, [B_nrm], [B_nrm])
                            S.op("dve", lambda e, nrm=nrm: e.reciprocal(out=nrm[:], in_=nrm[:]), [B_nrm], [B_nrm])
                            S.tt("pool", kkr[:], kkr[:], nrm[:], ALU.mult, [B_kkr, B_nrm], [B_kkr])
                            S.ts("pool", nrm[:], ag[:], ppc(5, c), ALU.mult, [B_ag, B_PP], [B_nrm], s2=ppc(7, c), op1=ALU.add)
                            yield
                            ew, B_ew = sl[4]
                            S.act(ew[:], cs[:], AF.Exp, [B_cs], [B_ew], scale=-DECAY_C)
                            ewi, B_ewi = cs, B_cs
                            S.act(ewi[:], cs[:], AF.Exp, [B_cs], [B_ewi], scale=DECAY_C)
                            ewp, B_ewp = csp, B_csp
                            S.act(ewp[:], csp[:], AF.Exp, [B_csp], [B_ewp], scale=-DECAY_C)
                            S.copy("pool", EWC[:, cl, :], ew[:].rearrange("p (tc j) -> p tc j", j=128)[:, :, 127],
                                   [B_ew], [B_EWC])
                            yield
                            kp, B_kp = sgd, B_sgd
                            S.tt("dve", kp[:], pk_[:], nrm[:], ALU.mult, [pkb_, B_nrm], [B_kp])
                            S.stt(RK, pr_[:], ppc(6, c), kp[:], ALU.mult, ALU.mult, [prb_, B_PP, B_kp], [B_RK])
                            for tc in range(NTC):
                                S.mm(PBD[:, tc * 16 + 2 * c: tc * 16 + 2 * c + 2], RK[:, tc * 128:(tc + 1) * 128], E2[:],
                                     True, True, [B_RK, B_E2], [B_PBD])
                            yield
                            arv = AR[:, cl].rearrange("p tc (two j) -> p tc two j", two=2)
                            S.tt("dve", arv[:, :, 1, :], pr_[:].rearrange("p (tc j) -> p tc j", j=128),
                                 ew[:].rearrange("p (tc j) -> p tc j", j=128), ALU.mult, [prb_, B_ew], [B_AR[cl]])
                            S.stt(arv[:, :, 0, :], kkr[:].rearrange("p (tc j) -> p tc j", j=128), -1.0,
                                  ewp[:].rearrange("p (tc j) -> p tc j", j=128), ALU.mult, ALU.mult,
                                  [B_kkr, B_ewp], [B_AR[cl]])
                            yield
                            S.tt("pool", ag[:], ag[:], kkr[:], ALU.mult, [B_ag, B_kkr], [B_ag])
                            for hh_ in range(2):
                                Pq = slice(hh_ * 64, (hh_ + 1) * 64)
                                S.tt("pool", BTZ[Pq, hh_, cl, :], ag[Pq, :], ewi[Pq, :], ALU.mult, [B_ag, B_ewi], [B_BT[cl]])
                                S.tt("pool", KTZ[Pq, hh_, cl, :], kp[Pq, :], ewi[Pq, :], ALU.mult, [B_kp, B_ewi], [B_KT[cl]])
                            yield

                        def interleave2(gens):
                            gens = list(gens)
                            while gens:
                                for g_ in list(gens):
                                    try:
                                        next(g_)
                                    except StopIteration:
                                        gens.remove(g_)

                        interleave2([prep_c(0), prep_c(1)])
                        interleave2([prep_c(2), prep_c(3)])
                        for tc in range(NTC):
                            for (srcz, Bsrc, dst, Bdst) in ((BTZ, B_BT, BTT, B_BTT), (KTZ, B_KT, KTT, B_KTT)):
                                pz_, pzb_ = ps()
                                for cl in range(4):
                                    for hh_ in range(2):
                                        S.mm(pz_[:, cl * 128:(cl + 1) * 128], srcz[:, hh_, cl, tc * 128:(tc + 1) * 128], IDENT[:],
                                             hh_ == 0, hh_ == 1, [Bsrc[cl], B_IDENT], [pzb_])
                                S.copy("act", dst[:, tc, :], pz_[:], [pzb_], [Bdst[tc]])
                        S.copy("act", BDOT[:, :, half * 8:(half + 1) * 8],
                               PBD[:, 0:NTC * 16].rearrange("p (tc h) -> p tc h", h=16)[:, :, half * 8:(half + 1) * 8],
                               [B_PBD], [B_BDOT])
                        _chk("R1")
                        if first and True:
                            S.memset("pool", HS[:, half * 4:(half + 1) * 4, :], 0.0, B_HS[half * 2:half * 2 + 2])
                            S.memset("pool", HBZ[:, half * 4:(half + 1) * 4, :], 0.0, B_HB[half * 2:half * 2 + 2])
                        _chk("S00")
                        wg_, wgb_ = wr(li, half)

                        handoff(ALIAS_TMP, ALIAS_SCAN)

                        def build_unit(tc, q, half=half):
                            tcs = slice(tc * 128, (tc + 1) * 128)
                            slot = (tc % 2) * 2 + q
                            SCs, B_SCs = sc_slot(slot)
                            SKs, B_SKs = sk_slot(slot)
                            (Pk, BPk1), (Qk, BQk) = pq_slot(slot)
                            def pb():
                                r = PSB[ring[0] % 4]
                                ring[0] += 1
                                return r
                            pq_, pqb_ = pb()
                            for hq in range(4):
                                h = q * 4 + hq
                                cl, hh = h // 2, h % 2
                                S.mm(pq_[:, hq * 128:(hq + 1) * 128], AR[:, cl, tc, 0:128], BTZ[:, hh, cl, tcs], True, True,
                                     [B_AR[cl], B_BT[cl]], [pqb_])
                            S.tt("dve", Qk.rearrange("p (h j) -> p h j", j=128),
                                 pq_[:].rearrange("p (h j) -> p h j", j=128),
                                 MASKL[:].unsqueeze(1).to_broadcast([128, 4, 128]), ALU.mult,
                                 [pqb_, B_MASKL], [BQk])
                            for hq in range(4):
                                h = q * 4 + hq
                                cl, hh = h // 2, h % 2
                                psc_, pscb_ = pb()
                                S.mm(psc_[:, 0:256], BTZ[:, hh, cl, tcs], AR[:, cl, tc, :], True, True,
                                     [B_BT[cl], B_AR[cl]], [pscb_])
                                S.mm(psc_[:, 256:512], KTZ[:, hh, cl, tcs], AR[:, cl, tc, :], True, True,
                                     [B_KT[cl], B_AR[cl]], [pscb_])
                                S.tt("dve", SCs[:, hq, :], psc_[:], MASKT[:], ALU.mult, [pscb_, B_MASKT], [B_SCs[hq]])
                                if hq % 2 == 1:
                                    yield
                            S.tt("dve", SKs.rearrange("p (h j) -> p h j", j=128), SCs[:, :, 0:128],
                                 IDENT[:].unsqueeze(1).to_broadcast([128, 4, 128]), ALU.add, B_SCs + [B_IDENT], [B_SKs])
                            yield
                            for k in range(7):
                                def Pk_ap(hq, k=k):
                                    if k == 0:
                                        return SCs[:, hq, 0:128]
                                    return Pk[:, hq * 128:(hq + 1) * 128]
                                BPk = B_SCs if k == 0 else [BPk1]
                                if k <= 5:
                                    pQ_, pQb_ = pb()
                                    for hq in range(4):
                                        S.mm(pQ_[:, hq * 128:(hq + 1) * 128], Pk_ap(hq), Qk[:, hq * 128:(hq + 1) * 128],
                                             True, True, BPk + [BQk], [pQb_])
                                if k >= 1:
                                    pS_, pSb_ = pb()
                                    for hq in range(4):
                                        S.mm(pS_[:, hq * 128:(hq + 1) * 128], Qk[:, hq * 128:(hq + 1) * 128],
                                             SKs[:, hq * 128:(hq + 1) * 128], True, True, [BQk, B_SKs], [pSb_])
                                if k <= 4:
                                    pP_, pPb_ = pb()
                                    for hq in range(4):
                                        S.mm(pP_[:, hq * 128:(hq + 1) * 128], Qk[:, hq * 128:(hq + 1) * 128], Pk_ap(hq),
                                             True, True, BPk + [BQk], [pPb_])
                                if k >= 1:
                                    S.tt("dve", SKs, pS_[:], SKs, ALU.add, [pSb_, B_SKs], [B_SKs])
                                if k <= 5:
                                    S.copy("act", Qk, pQ_[:], [pQb_], [BQk])
                                if k <= 4:
                                    S.copy("act", Pk, pP_[:], [pPb_], [BPk1])
                                yield

                        def seq_unit(tc, q, half=half, wg_=wg_, wgb_=wgb_):
                            tcs = slice(tc * 128, (tc + 1) * 128)
                            slot = (tc % 2) * 2 + q
                            SCs, B_SCs = sc_slot(slot)
                            SKs, B_SKs = sk_slot(slot)
                            qc = slice(q * 256, (q + 1) * 256)
                            B_hs = B_HS[half * 2 + q]
                            B_hb = B_HB[half * 2 + q]
                            if q == 0:
                                pgt_, pgtb_ = PSB[4]
                                for kc in range(8):
                                    S.mm(pgt_[:], HT[:, kc, 1 + tc * 128: 1 + (tc + 1) * 128], wg_[:, kc * 512:(kc + 1) * 512],
                                         kc == 0, kc == 7, [B_HT, wgb_], [pgtb_])
                                S.act(SGT[:], pgt_[:], AF.Silu, [pgtb_], [B_SGT])
                                yield
                            px_, pxb_ = PSB[5]
                            for pl in range(2):
                                cl = q * 2 + pl
                                c = half * 4 + cl
                                S.mm(px_[:, pl * 128:(pl + 1) * 128], AR[:, cl, tc, 0:128], HBZ[:, c, :], True, False,
                                     [B_AR[cl], B_hb], [pxb_])
                                for hh in range(2):
                                    hl = pl * 2 + hh
                                    S.mm(px_[:, hl * 64:(hl + 1) * 64], SCs[:, hl, 256:384], VT[:, tc, (c * 2 + hh) * 64:(c * 2 + hh + 1) * 64],
                                         False, hh == 1, [B_SCs[hl], B_VT[tc]], [pxb_])
                            S.copy("act", XB[:, qc], px_[:, 0:256], [pxb_], [B_XB[q]])
                            yield
                            pu_, pub_ = PSB[4]
                            for hl in range(4):
                                S.mm(pu_[:, hl * 64:(hl + 1) * 64], SKs[:, hl * 128:(hl + 1) * 128],
                                     XB[:, q * 256 + hl * 64: q * 256 + (hl + 1) * 64], True, True, [B_SKs, B_XB[q]], [pub_])
                            S.copy("act", UB[:, qc], pu_[:, 0:256], [pub_], [B_UB[q]])
                            yield
                            py_, pyb_ = PSB[5]
                            for pl in range(2):
                                cl = q * 2 + pl
                                c = half * 4 + cl
                                S.mm(py_[:, pl * 128:(pl + 1) * 128], AR[:, cl, tc, 128:256], HBZ[:, c, :], True, False,
                                     [B_AR[cl], B_hb], [pyb_])
                                for hh in range(2):
                                    h = cl * 2 + hh
                                    hl = pl * 2 + hh
                                    vcol = slice((c * 2 + hh) * 64, (c * 2 + hh + 1) * 64)
                                    S.mm(py_[:, hl * 64:(hl + 1) * 64], SCs[:, hl, 128:256], UB[:, h * 64:(h + 1) * 64], False, False,
                                         [B_SCs[hl], B_UB[q]], [pyb_])
                                    S.mm(py_[:, hl * 64:(hl + 1) * 64], SCs[:, hl, 384:512], VT[:, tc, vcol], False, hh == 1,
                                         [B_SCs[hl], B_VT[tc]], [pyb_])
                            ph_, phb_ = PSB[4]
                            for pl in range(2):
                                cl = q * 2 + pl
                                c = half * 4 + cl
                                pc = slice(cl * 128, (cl + 1) * 128)
                                S.mm(ph_[:, pl * 128:(pl + 1) * 128], BTT[:, tc, pc], UB[:, pc], True, False, [B_BTT[tc], B_UB[q]], [phb_])
                                S.mm(ph_[:, pl * 128:(pl + 1) * 128], KTT[:, tc, pc], VT[:, tc, c * 128:(c + 1) * 128], False, True,
                                     [B_KTT[tc], B_VT[tc]], [phb_])
                            for hh in range(2):
                                Ph = slice(hh * 64, (hh + 1) * 64)
                                c0 = half * 4 + q * 2
                                hsv = HS[Ph, c0:c0 + 2, :]
                                phv = ph_[Ph, 0:256].rearrange("p (c x) -> p c x", x=128)[:, :, hh * 64:(hh + 1) * 64]
                                S.tt("dve", hsv, phv, hsv, ALU.add, [phb_, B_hs], [B_hs])
                                S.tt("dve", hsv, hsv, EWC[Ph, q * 2:q * 2 + 2, tc].unsqueeze(2).to_broadcast([64, 2, 64]), ALU.mult,
                                     [B_hs, B_EWC], [B_hs])
                                S.copy("pool", HBZ[Ph, c0:c0 + 2, hh * 64:(hh + 1) * 64], hsv, [B_hs], [B_hb])
                            yield
                            B_st = B_ST8[q]
                            hq4 = slice(q * 4, (q + 1) * 4)
                            y3 = py_[:, 0:256].rearrange("p (h v) -> p h v", v=64)
                            ya3 = YA[:, qc].rearrange("p (h v) -> p h v", v=64)
                            yb3 = YB[:, qc].rearrange("p (h v) -> p h v", v=64)
                            S.op("dve", lambda e, y3=y3: e.tensor_reduce(out=ST8[:, 0, hq4], in_=y3, axis=AX.X, op=ALU.add),
                                 [pyb_], [B_st])
                            S.ts("dve", ST8[:, 2, hq4], ST8[:, 0, hq4], 1.0 / 64, ALU.mult, [B_st], [B_st])
                            S.tt("dve", ya3, y3, ST8[:, 2, hq4].unsqueeze(2).to_broadcast([128, 4, 64]), ALU.subtract,
                                 [pyb_, B_st], [B_YA[q]])
                            yield
                            S.tt("pool", YB[:, qc], YA[:, qc], YA[:, qc], ALU.mult, [B_YA[q]], [B_YB[q]])
                            S.op("dve", lambda e, yb3=yb3: e.tensor_reduce(out=ST8[:, 1, hq4], in_=yb3, axis=AX.X, op=ALU.add),
                                 [B_YB[q]], [B_st])
                            S.ts("pool", ST8[:, 4, hq4], ST8[:, 1, hq4], 1.0 / 64, ALU.mult, [B_st], [B_st], s2=LNX_EPS, op1=ALU.add)
                            S.tt("pool", ST8[:, 5, hq4], ST8[:, 4, hq4], NEGH[:, 0:4], ALU.pow, [B_st, B_NEGH], [B_st])
                            yield
                            S.tt("dve", ya3, ya3, ST8[:, 5, hq4].unsqueeze(2).to_broadcast([128, 4, 64]), ALU.mult,
                                 [B_YA[q], B_st], [B_YA[q]])
                            hc = slice(half * 512 + q * 256, half * 512 + (q + 1) * 256)
                            S.tt("pool", YA[:, qc], YA[:, qc], LNW[:, hc], ALU.mult, [B_YA[q], B_LNW], [B_YA[q]])
                            S.tt("pool", YA[:, qc], YA[:, qc], LNB[:, hc], ALU.add, [B_YA[q], B_LNB], [B_YA[q]])
                            S.tt("dve", yb3, VT[:, tc, hc].rearrange("p (h v) -> p h v", v=64),
                                 BDOT[:, tc, half * 8 + q * 4: half * 8 + (q + 1) * 4].unsqueeze(2).to_broadcast([128, 4, 64]), ALU.mult,
                                 [B_VT[tc], B_BDOT], [B_YB[q]])
                            yield
                            S.tt("pool", YA[:, qc], YA[:, qc], YB[:, qc], ALU.add, [B_YA[q], B_YB[q]], [B_YA[q]])
                            S.tt("pool", YG[:, qc], YA[:, qc], SGT[:, qc], ALU.mult, [B_YA[q], B_SGT], [B_YG[q]])
                            yield
                            ptt, ptb = PT4
                            for pl in range(2):
                                S.tr(ptt[:, pl * 128:(pl + 1) * 128], YG[:, q * 256 + pl * 128: q * 256 + (pl + 1) * 128], IDENT[:],
                                     [B_YG[q], B_IDENT], [ptb])
                            c0 = half * 4 + q * 2
                            S.copy("act", YGT[:, c0:c0 + 2, tcs],
                                   ptt[:, 0:256].rearrange("p (c j) -> p c j", j=128), [ptb], [B_YGT])
                            yield

                        def chain(*gs):
                            for g_ in gs:
                                yield from g_

                        def interleave(gens):
                            gens = list(gens)
                            while gens:
                                for g_ in list(gens):
                                    try:
                                        next(g_)
                                    except StopIteration:
                                        gens.remove(g_)

                        bg = bg_gens[half]
                        if NO_BG:
                            for _ in bg:
                                pass

                        def interleave_bg(gens):
                            gens = list(gens)
                            while gens:
                                for g_ in list(gens):
                                    try:
                                        next(g_)
                                    except StopIteration:
                                        gens.remove(g_)
                                try:
                                    next(bg)
                                except StopIteration:
                                    pass

                        interleave_bg([build_unit(0, 0), build_unit(0, 1)])
                        for tc in range(NTC):
                            gens = [chain(seq_unit(tc, 0), seq_unit(tc, 1))]
                            if tc + 1 < NTC:
                                gens += [build_unit(tc + 1, 0), build_unit(tc + 1, 1)]
                            interleave_bg(gens)
                        for _ in bg:
                            pass
                        handoff(ALIAS_SCAN, ALIAS_TMP)

                    _chk("S")
                    for m in range(8):
                        py_, pyb_ = ps()
                        w_, wb_ = wl(li, 98 + m)
                        for kc in range(8):
                            S.mm(py_[:], w_[:, kc * 128:(kc + 1) * 128], YGT[:, kc, :], kc == 0, kc == 7,
                                 [wb_, B_YGT], [pyb_])
                        pg_, pgb_ = inproj(li, 57 + m, False)
                        S.act(SGM[:], pg_[:], AF.Sigmoid, [pgb_], [B_SGM])
                        t1, B_t1 = TMP[7]
                        S.tt("dve", t1[:], py_[:], SGM[:], ALU.mult, [pyb_, B_SGM], [B_t1])
                        S.tt("pool", MG[:, m, :], t1[:], MP[:, m, :], ALU.add, [B_t1, B_MP], [B_MG[m]])
                    _chk("O1")
                    wo = [wr(li, 2), wr(li, 3)]
                    for tg in range(4):
                        xi, B_xi = XIN[tg % 2]
                        rows = slice(row0 + tg * 128, row0 + (tg + 1) * 128)
                        S.dma("sp", xi[:], x_src[rows, :], Rxs, [B_xi])
                        for hf in range(2):
                            po_, pob_ = ps()
                            for kc in range(8):
                                S.mm(po_[:], MG[:, kc, tg * 128:(tg + 1) * 128], wo[hf][0][:, kc * 512:(kc + 1) * 512],
                                     kc == 0, kc == 7, [B_MG, wo[hf][1]], [pob_])
                            S.tt("dve", xi[:, hf * 512:(hf + 1) * 512], po_[:], xi[:, hf * 512:(hf + 1) * 512], ALU.add,
                                 [pob_, B_xi], [B_xi])
                        if last:
                            S.act(JUNK[:], xi[:], AF.Square, [B_xi], [B_JUNK, B_STAT], accum_out=STAT[:, 4:5])
                            S.ts("pool", STAT[:, 5:6], STAT[:, 4:5], 1.0 / D, ALU.mult, [B_STAT], [B_STAT], s2=NORM_EPS, op1=ALU.add)
                            S.tt("pool", STAT[:, 6:7], STAT[:, 5:6], NEGH[:, 0:1], ALU.pow, [B_STAT, B_NEGH], [B_STAT])
                            S.stt(xi[:], xi[:], STAT[:, 6:7], FG[:], ALU.mult, ALU.mult, [B_xi, B_STAT, B_FG], [B_xi])
                        _chk("O2")
                        S.dma("act", x_dst[rows, :], xi[:], [B_xi], Wxd)
                        _chk("O3")
                    _chk("T")


def _pack_pp(inp, L0, L1):
    nl = L1 - L0
    pp = np.zeros((nl, 128, NPAR, 8), np.float32)
    for li in range(nl):
        L = L0 + li
        vecs = [inp["pool_scale"][L], inp["decay_w0"][L], inp["a0"][L],
                inp["vres_v0"][L - 1] if L > 0 else None, inp["k_k"][L], inp["k_a"][L],
                np.asarray(inp["r_k"][L]).reshape(-1)]
        for k, v in enumerate(vecs):
            if v is None:
                continue
            pp[li, :, k, :] = np.asarray(v, np.float32).reshape(8, 128).T
    return pp.reshape(nl, 128, NPAR * 8)


def _layer_inputs(inp, L0, L1):
    f = lambda a: np.ascontiguousarray(np.asarray(a, np.float32))
    nl = L1 - L0
    v1 = np.zeros((nl, D, 32), np.float32)
    v2 = np.zeros((nl, 32, D), np.float32)
    for li in range(nl):
        L = L0 + li
        if L > 0:
            v1[li] = inp["vres_v1"][L - 1]
            v2[li] = inp["vres_v2"][L - 1]
    return {
        "w_in": f(inp["w_in"][L0:L1]), "pool_lin": f(inp["pool_lin"][L0:L1]),
        "w_pool_proj": f(inp["w_pool_proj"][L0:L1]), "mu_shift": f(inp["mu_shift"][L0:L1]),
        "decay_w2": f(inp["decay_w2"][L0:L1]), "a2": f(inp["a2"][L0:L1]),
        "vres_v1": v1, "vres_v2": v2,
        "w_rwkv_proj": f(inp["w_rwkv_proj"][L0:L1]), "w_out": f(inp["w_out"][L0:L1]),
        "norm_g": f(inp["norm_g"][L0:L1]), "lnx_w": f(inp["lnx_w"][L0:L1]), "lnx_b": f(inp["lnx_b"][L0:L1]),
        "final_g": f(inp["final_g"]).reshape(1, D), "pp": _pack_pp(inp, L0, L1),
    }


NO_BG = False
_PROG_CACHE = {}
STOP = None


class _Stop(Exception):
    pass


_CNT = [0]


_TILE = [0]


def _chk(name):
    if name == "T":
        _TILE[0] += 1
    if STOP == name or STOP == "%s@%d" % (name, _TILE[0]):
        raise _Stop()
    if name == "cnt" and STOP is not None and STOP.startswith("cnt"):
        _CNT[0] += 1
        if _CNT[0] >= int(STOP[3:]):
            raise _Stop()


def run_layers(inp, xs, L0, L1, NT, vfirst=None, cores=None):
    key = (L0, L1, NT)
    if key not in _PROG_CACHE:
        _PROG_CACHE[key] = build_program(L0, L1, NT, True)
    nc = _PROG_CACHE[key]
    shared = _layer_inputs(inp, L0, L1)
    in_maps = []
    for i, xc in enumerate(xs):
        m = dict(shared)
        m["x"] = np.ascontiguousarray(xc)
        if L0 > 0:
            m["vfirst_in"] = vfirst[i]
        in_maps.append(m)
    cores = list(range(len(xs))) if cores is None else cores
    import os
    if os.environ.get("KTRACE"):
        res = run_bass_kernel_spmd(nc, in_maps, core_ids=cores, trace=True)
        print("EXEC_TIME_NS", res.exec_time_ns, flush=True)
        try:
            print("TRACE", res.instructions_and_trace[1] if res.instructions_and_trace else None, flush=True)
        except Exception as ex:
            print("TRACE?", ex)
    else:
        res = run_bass_kernel_spmd(nc, in_maps, core_ids=cores)
    outs = [r["out"] for r in res.results]
    vf = [r["vfirst_out"] for r in res.results] if (L0 == 0 and L1 < DEPTH) else None
    return outs, vf


FUSED = True


def kernel(**inputs):
    x = np.asarray(inputs["x"], np.float32)
    xs = [np.ascontiguousarray(x[2 * i:2 * i + 2].reshape(NSEQ * SEQ, D)) for i in range(8)]
    if FUSED:
        outs, _ = run_layers(inputs, xs, 0, DEPTH, SEQ // T)
    else:
        outs, vf = run_layers(inputs, xs, 0, 1, SEQ // T)
        for L in range(1, DEPTH):
            outs, _ = run_layers(inputs, outs, L, L + 1, SEQ // T, vfirst=vf)
    return np.stack([o.reshape(NSEQ, SEQ, D) for o in outs], 0).reshape(16, SEQ, D).astype(np.float32)
```
